# Optimizing a Trainium2 kernel written in Bass

```python
import jax
import jax.numpy as jnp
from jax import lax
import numpy as np

D_MODEL = 1024
BATCH = 8
SEQ = 4096
DEPTH = 1

GRID_W = 64
CTX_LEN = 256
N_HEADS = 8
N_KV_HEADS = 2
Q_PER_KV = N_HEADS // N_KV_HEADS
HEAD_DIM = 64
ATTN_WIDTH = N_HEADS * HEAD_DIM
KV_WIDTH = N_KV_HEADS * HEAD_DIM
WINDOW = 128
ATTN_BLOCK = 128
SGU_HEADS = 8
SGU_HEAD_DIM = 64
SGU_WIDTH = SGU_HEADS * SGU_HEAD_DIM
SGU_CHUNK = 128
MIX_WIDTH = ATTN_WIDTH + SGU_WIDTH
KV_START = ATTN_WIDTH
KV_END = ATTN_WIDTH + 2 * KV_WIDTH
IN_WIDTH = KV_END + 2 * SGU_WIDTH
N_EXPERTS = 32
TOP_K = 4
D_FF_EXPERT = 1024
SWIGLU_LIMIT = 7.0
SWIGLU_ALPHA = 1.702
MOE_BLOCK = 256
ROPE_THETA = 10000.0
EPS = 1e-6
N_MOD = 6

kernel_name = "hybrid_swa_sgu_moe_dit_block"


def rms_norm(x, g):
    xf = x.astype(jnp.float32)
    y = xf * lax.rsqrt(jnp.mean(xf * xf, axis=-1, keepdims=True) + EPS)
    return (y * g.astype(jnp.float32)).astype(x.dtype)


def layer_norm(x, g, b):
    xf = x.astype(jnp.float32)
    mu = jnp.mean(xf, axis=-1, keepdims=True)
    var = jnp.mean(jnp.square(xf - mu), axis=-1, keepdims=True)
    y = (xf - mu) * lax.rsqrt(var + EPS) * g.astype(jnp.float32) + b.astype(jnp.float32)
    return y.astype(x.dtype)


def modulate(x, shift, scale):
    return x * (1 + scale) + shift


def axial_rope_tables(n_tokens, dtype):
    rows = n_tokens // GRID_W
    pos_row = jnp.repeat(jnp.arange(rows, dtype=jnp.float32), GRID_W)
    pos_col = jnp.tile(jnp.arange(GRID_W, dtype=jnp.float32), rows)
    n_freq = HEAD_DIM // 4
    inv_freq = ROPE_THETA ** (-jnp.arange(n_freq, dtype=jnp.float32) / n_freq)
    ang_r = pos_row[:, None, None] * inv_freq
    ang_c = pos_col[:, None, None] * inv_freq
    return (jnp.cos(ang_r).astype(dtype), jnp.sin(ang_r).astype(dtype),
            jnp.cos(ang_c).astype(dtype), jnp.sin(ang_c).astype(dtype))


def rope_axis(x, cos, sin):
    x1, x2 = jnp.split(x, 2, axis=-1)
    return jnp.concatenate([x1 * cos - x2 * sin, x2 * cos + x1 * sin], axis=-1)


def apply_axial_rope(x, tables):
    cos_r, sin_r, cos_c, sin_c = tables
    half = HEAD_DIM // 2
    return jnp.concatenate([rope_axis(x[..., :half], cos_r, sin_r),
                            rope_axis(x[..., half:], cos_c, sin_c)], axis=-1)


def split_projection(p):
    return jnp.split(p, [ATTN_WIDTH, ATTN_WIDTH + KV_WIDTH, KV_END, KV_END + SGU_WIDTH], axis=-1)


def window_attention(q, k, v, k_ctx, v_ctx, sink):
    B, S = q.shape[0], q.shape[1]
    L = k_ctx.shape[1]
    nb = S // ATTN_BLOCK
    scale = HEAD_DIM ** -0.5
    qb = q.reshape(B, nb, ATTN_BLOCK, N_KV_HEADS, Q_PER_KV, HEAD_DIM)

    def band(t):
        tp = jnp.pad(t, ((0, 0), (ATTN_BLOCK, ATTN_BLOCK), (0, 0), (0, 0)))
        tp = tp.reshape(B, nb + 2, ATTN_BLOCK, N_KV_HEADS, HEAD_DIM)
        return jnp.concatenate([tp[:, :-2], tp[:, 1:-1], tp[:, 2:]], axis=2)

    kb, vb = band(k), band(v)
    s_loc = jnp.einsum('bnqkgd,bnjkd->bnkgqj', qb, kb).astype(jnp.float32) * scale
    qi = jnp.arange(ATTN_BLOCK)[:, None]
    kj = jnp.arange(3 * ATTN_BLOCK)[None, :] - ATTN_BLOCK
    key_pos = jnp.arange(nb)[:, None, None] * ATTN_BLOCK + kj[None]
    valid = (jnp.abs(kj - qi) <= WINDOW)[None] & (key_pos >= 0) & (key_pos < S)
    s_loc = jnp.where(valid[None, :, None, None], s_loc, -jnp.inf)
    s_ctx = jnp.einsum('bnqkgd,bjkd->bnkgqj', qb, k_ctx).astype(jnp.float32) * scale
    s_sink = jnp.broadcast_to(sink.astype(jnp.float32).reshape(N_KV_HEADS, Q_PER_KV, 1, 1),
                              s_loc.shape[:-1] + (1,))
    p = jax.nn.softmax(jnp.concatenate([s_loc, s_ctx, s_sink], axis=-1), axis=-1)
    n_loc = 3 * ATTN_BLOCK
    p_loc = p[..., :n_loc].astype(v.dtype)
    p_ctx = p[..., n_loc:n_loc + L].astype(v.dtype)
    o = (jnp.einsum('bnkgqj,bnjkd->bnqkgd', p_loc, vb)
         + jnp.einsum('bnkgqj,bjkd->bnqkgd', p_ctx, v_ctx))
    return o.reshape(B, S, ATTN_WIDTH)


def context_attention(q, k, v, sink):
    B, L = q.shape[0], q.shape[1]
    qg = q.reshape(B, L, N_KV_HEADS, Q_PER_KV, HEAD_DIM)
    s = jnp.einsum('bqkgd,bjkd->bkgqj', qg, k).astype(jnp.float32) * (HEAD_DIM ** -0.5)
    s_sink = jnp.broadcast_to(sink.astype(jnp.float32).reshape(N_KV_HEADS, Q_PER_KV, 1, 1),
                              s.shape[:-1] + (1,))
    p = jax.nn.softmax(jnp.concatenate([s, s_sink], axis=-1), axis=-1)
    o = jnp.einsum('bkgqj,bjkd->bqkgd', p[..., :L].astype(v.dtype), v)
    return o.reshape(B, L, ATTN_WIDTH)


def spatial_gating(u, v, ln_g, ln_b, w_s, b_s):
    B, N = u.shape[0], u.shape[1]
    nc = N // SGU_CHUNK
    u = jax.nn.gelu(u)
    v = layer_norm(jax.nn.gelu(v), ln_g, ln_b)
    vc = v.reshape(B, nc, SGU_CHUNK, SGU_HEADS, SGU_HEAD_DIM)
    mixed = jnp.einsum('hij,bnjhc->bnihc', w_s, vc) + b_s.T[None, None, :, :, None]
    return u * mixed.reshape(B, N, SGU_WIDTH)


def merge_heads(attn_o, sgu_o, g_attn_out, g_sgu_out, w_out, b_out):
    o = jnp.concatenate([rms_norm(attn_o, g_attn_out), rms_norm(sgu_o, g_sgu_out)], axis=-1)
    return o @ w_out + b_out


def moe_ffn(h, w_router, b_router, w_gate_up, b_gate_up, w_down, b_down):
    shape = h.shape
    xt = h.reshape(-1, D_MODEL)
    T = xt.shape[0]
    logits = (xt @ w_router + b_router).astype(jnp.float32)
    top_logits, top_idx = lax.top_k(logits, TOP_K)
    gates = jax.nn.softmax(top_logits, axis=-1)
    M = T * TOP_K
    expert_flat = top_idx.reshape(-1)
    order = jnp.argsort(expert_flat)
    e_sorted = expert_flat[order]
    tok_sorted = order // TOP_K
    gate_sorted = gates.reshape(-1)[order]
    counts = jnp.zeros((N_EXPERTS,), jnp.int32).at[expert_flat].add(1)
    start = jnp.cumsum(counts) - counts
    padded = (counts + MOE_BLOCK - 1) // MOE_BLOCK * MOE_BLOCK
    pad_end = jnp.cumsum(padded)
    pad_start = pad_end - padded
    dest = pad_start[e_sorted] + jnp.arange(M) - start[e_sorted]
    n_blocks = -(-M // MOE_BLOCK) + N_EXPERTS
    P = n_blocks * MOE_BLOCK
    row_tok = jnp.full((P,), T, jnp.int32).at[dest].set(tok_sorted.astype(jnp.int32))
    row_gate = jnp.zeros((P,), jnp.float32).at[dest].set(gate_sorted)
    block_start = jnp.arange(n_blocks) * MOE_BLOCK
    block_expert = jnp.minimum(jnp.sum(pad_end[None, :] <= block_start[:, None], axis=1), N_EXPERTS - 1)
    x_pad = jnp.concatenate([xt, jnp.zeros((1, D_MODEL), xt.dtype)], axis=0)

    def expert_block(args):
        rows, e = args
        xb = x_pad[rows]
        gu = xb @ w_gate_up[e] + b_gate_up[e]
        gate, up = jnp.split(gu, 2, axis=-1)
        gate = jnp.minimum(gate, SWIGLU_LIMIT)
        up = jnp.clip(up, -SWIGLU_LIMIT, SWIGLU_LIMIT)
        act = (up + 1) * gate * jax.nn.sigmoid(SWIGLU_ALPHA * gate)
        return act @ w_down[e] + b_down[e]

    y_rows = lax.map(expert_block, (row_tok.reshape(n_blocks, MOE_BLOCK), block_expert))
    y_rows = y_rows.reshape(P, D_MODEL) * row_gate[:, None].astype(y_rows.dtype)
    y = jax.ops.segment_sum(y_rows, row_tok, num_segments=T + 1)[:T]
    return y.reshape(shape)


def setup_inputs(seed: int = 0) -> dict:
    key = jax.random.key(seed)
    ks = jax.random.split(key, 27)
    f32 = jnp.float32

    def nrm(k, shape, scale):
        return jax.random.normal(k, shape, f32) * scale

    def gain(k, shape):
        return 1.0 + 0.02 * jax.random.normal(k, shape, f32)

    L = DEPTH
    return {
        "x": nrm(ks[0], (BATCH, SEQ, D_MODEL), 1.0),
        "c": nrm(ks[1], (BATCH, D_MODEL), 1.0),
        "ctx": nrm(ks[2], (BATCH, CTX_LEN, D_MODEL), 1.0),
        "c_ctx": nrm(ks[3], (D_MODEL,), 1.0),
        "w_ada": nrm(ks[4], (L, D_MODEL, N_MOD * D_MODEL), 0.5 * D_MODEL ** -0.5),
        "b_ada": nrm(ks[5], (L, N_MOD * D_MODEL), 0.02),
        "g_pre_mix": gain(ks[6], (L, D_MODEL)),
        "g_post_mix": gain(ks[7], (L, D_MODEL)),
        "g_pre_ffn": gain(ks[8], (L, D_MODEL)),
        "g_post_ffn": gain(ks[9], (L, D_MODEL)),
        "w_in": nrm(ks[10], (L, D_MODEL, IN_WIDTH), D_MODEL ** -0.5),
        "b_in": nrm(ks[11], (L, IN_WIDTH), 0.02),
        "attn_sink": nrm(ks[12], (L, N_HEADS), 0.5),
        "sgu_ln_g": gain(ks[13], (L, SGU_WIDTH)),
        "sgu_ln_b": nrm(ks[14], (L, SGU_WIDTH), 0.02),
        "sgu_w": nrm(ks[15], (L, SGU_HEADS, SGU_CHUNK, SGU_CHUNK), SGU_CHUNK ** -0.5),
        "sgu_b": gain(ks[16], (L, SGU_HEADS, SGU_CHUNK)),
        "g_attn_out": gain(ks[17], (L, ATTN_WIDTH)),
        "g_sgu_out": gain(ks[18], (L, SGU_WIDTH)),
        "w_out": nrm(ks[19], (L, MIX_WIDTH, D_MODEL), MIX_WIDTH ** -0.5),
        "b_out": nrm(ks[20], (L, D_MODEL), 0.02),
        "w_router": nrm(ks[21], (L, D_MODEL, N_EXPERTS), D_MODEL ** -0.5),
        "b_router": nrm(ks[22], (L, N_EXPERTS), 0.01),
        "w_gate_up": nrm(ks[23], (L, N_EXPERTS, D_MODEL, 2 * D_FF_EXPERT), D_MODEL ** -0.5),
        "b_gate_up": nrm(ks[24], (L, N_EXPERTS, 2 * D_FF_EXPERT), 0.02),
        "w_down": nrm(ks[25], (L, N_EXPERTS, D_FF_EXPERT, D_MODEL), D_FF_EXPERT ** -0.5),
        "b_down": nrm(ks[26], (L, N_EXPERTS, D_MODEL), 0.02),
    }


def reference(x, c, ctx, c_ctx, w_ada, b_ada, g_pre_mix, g_post_mix, g_pre_ffn, g_post_ffn,
              w_in, b_in, attn_sink, sgu_ln_g, sgu_ln_b, sgu_w, sgu_b, g_attn_out, g_sgu_out,
              w_out, b_out, w_router, b_router, w_gate_up, b_gate_up, w_down, b_down):
    B, S = x.shape[0], x.shape[1]
    Lc = ctx.shape[1]
    rope = axial_rope_tables(S, x.dtype)
    for l in range(DEPTH):
        mod = jax.nn.silu(c) @ w_ada[l] + b_ada[l]
        sh1, sc1, gt1, sh2, sc2, gt2 = jnp.split(mod[:, None, :], N_MOD, axis=-1)
        mod_ctx = jax.nn.silu(c_ctx) @ w_ada[l] + b_ada[l]
        csh1, csc1, cgt1, csh2, csc2, cgt2 = jnp.split(mod_ctx, N_MOD)

        h = modulate(rms_norm(x, g_pre_mix[l]), sh1, sc1)
        q, k, v, su, sv = split_projection(h @ w_in[l] + b_in[l])
        q = apply_axial_rope(q.reshape(B, S, N_HEADS, HEAD_DIM), rope)
        k = apply_axial_rope(k.reshape(B, S, N_KV_HEADS, HEAD_DIM), rope)
        v = v.reshape(B, S, N_KV_HEADS, HEAD_DIM)

        hc = modulate(rms_norm(ctx, g_pre_mix[l]), csh1, csc1)
        if l < DEPTH - 1:
            qc, kc, vc, suc, svc = split_projection(hc @ w_in[l] + b_in[l])
        else:
            kc, vc = jnp.split(hc @ w_in[l][:, KV_START:KV_END] + b_in[l][KV_START:KV_END], 2, axis=-1)
        kc = kc.reshape(B, Lc, N_KV_HEADS, HEAD_DIM)
        vc = vc.reshape(B, Lc, N_KV_HEADS, HEAD_DIM)

        attn_o = window_attention(q, k, v, kc, vc, attn_sink[l])
        sgu_o = spatial_gating(su, sv, sgu_ln_g[l], sgu_ln_b[l], sgu_w[l], sgu_b[l])
        mix = merge_heads(attn_o, sgu_o, g_attn_out[l], g_sgu_out[l], w_out[l], b_out[l])
        x_mid = x + gt1 * rms_norm(mix, g_post_mix[l])

        h2 = modulate(rms_norm(x_mid, g_pre_ffn[l]), sh2, sc2)
        ffn = moe_ffn(h2, w_router[l], b_router[l], w_gate_up[l], b_gate_up[l], w_down[l], b_down[l])
        x_new = x_mid + gt2 * rms_norm(ffn, g_post_ffn[l])

        if l < DEPTH - 1:
            qc = qc.reshape(B, Lc, N_HEADS, HEAD_DIM)
            attn_c = context_attention(qc, kc, vc, attn_sink[l])
            sgu_c = spatial_gating(suc, svc, sgu_ln_g[l], sgu_ln_b[l], sgu_w[l], sgu_b[l])
            mix_c = merge_heads(attn_c, sgu_c, g_attn_out[l], g_sgu_out[l], w_out[l], b_out[l])
            ctx_mid = ctx + cgt1 * rms_norm(mix_c, g_post_mix[l])
            hc2 = modulate(rms_norm(ctx_mid, g_pre_ffn[l]), csh2, csc2)
            ffn_c = moe_ffn(hc2, w_router[l], b_router[l], w_gate_up[l], b_gate_up[l], w_down[l], b_down[l])
            ctx = ctx_mid + cgt2 * rms_norm(ffn_c, g_post_ffn[l])
        x = x_new
    return x
```

```python
import sys
import numpy as np
from contextlib import ExitStack
import concourse.bass as bass
import concourse.mybir as mybir
from concourse.bass_utils import run_bass_kernel_spmd

F32 = mybir.dt.float32
BF16 = mybir.dt.bfloat16
I32 = mybir.dt.int32
ALU = mybir.AluOpType
AF = mybir.ActivationFunctionType
AX = mybir.AxisListType

NT = 32
R = 512
NB = 64
RG = R // 128
EPS = 1e-6
SUB = 0


class Reg:
    __slots__ = ("name", "w", "rs")

    def __init__(self, name=""):
        self.name = name
        self.w = None
        self.rs = []


class DSem:
    def __init__(self, sem):
        self.sem = sem
        self.count = 0
        self.ops = []

    def seal(self):
        for o in self.ops:
            o.dcount = self.count


class Op:
    __slots__ = ("eng", "fn", "deps", "sig", "sigcount", "dsem", "dcount", "idx", "tag")


class Sched:
    ENGS = ("pe", "act", "dve", "pool", "sp")

    def __init__(self, nc, stack):
        self.nc = nc
        self.stack = stack
        self.ops = []
        self.esem = {e: stack.enter_context(nc.semaphore("es_" + e)) for e in ("pe", "act", "dve", "pool")}

    def dsem(self, name):
        return DSem(self.stack.enter_context(self.nc.semaphore("ds_" + name)))

    def op(self, eng, fn, reads=(), writes=(), dsem=None):
        o = Op()
        o.eng = eng
        o.fn = fn
        o.dsem = dsem
        o.sig = False
        o.sigcount = 0
        o.dcount = 0
        o.idx = len(self.ops)
        fr = sys._getframe(1)
        tags = []
        while fr is not None and len(tags) < 4:
            if fr.f_code.co_filename == __file__:
                tags.append(str(fr.f_lineno))
            fr = fr.f_back
        o.tag = "L" + "<".join(tags)
        deps = set()
        for r in reads:
            if r.w is not None:
                deps.add(r.w)
        for r in writes:
            if r.w is not None:
                deps.add(r.w)
            deps.update(r.rs)
        deps.discard(o.idx)
        if fn is not None:
            for r in reads:
                r.rs.append(o.idx)
            for r in writes:
                r.w = o.idx
                r.rs = []
        o.deps = deps
        if dsem is not None:
            dsem.count += 16
            o.dcount = dsem.count
            dsem.ops.append(o)
        self.ops.append(o)
        return o

    def finalize(self):
        ops = self.ops
        for o in ops:
            for d in o.deps:
                t = ops[d]
                if t.dsem is not None:
                    continue
                if t.eng == o.eng and o.eng in ("pe", "sp"):
                    continue
                t.sig = True
        cnt = {e: 0 for e in self.ENGS}
        for o in ops:
            if o.dsem is None and o.sig:
                cnt[o.eng] += 1
                o.sigcount = cnt[o.eng]

    def emit(self, engname, engobj):
        ops = self.ops
        waited = {}
        for o in ops:
            if o.eng != engname:
                continue
            need = {}
            for d in o.deps:
                t = ops[d]
                if t.dsem is not None:
                    key = ("d", id(t.dsem))
                    sem = t.dsem.sem
                    val = t.dcount
                else:
                    if t.eng == o.eng and o.eng in ("pe", "sp"):
                        continue
                    key = ("e", t.eng)
                    sem = self.esem[t.eng]
                    val = t.sigcount
                if key not in need or need[key][1] < val:
                    need[key] = (sem, val)
            for key, (sem, val) in need.items():
                if waited.get(key, 0) < val:
                    engobj.wait_ge(sem, val)
                    waited[key] = val
            if o.fn is None:
                continue
            ins = o.fn(engobj)
            try:
                ins.annotate(o.tag)
            except Exception:
                pass
            if o.dsem is not None:
                ins.then_inc(o.dsem.sem, 16)
            elif o.sig:
                ins.then_inc(self.esem[o.eng], 1)

    def run(self):
        self.finalize()
        nc = self.nc
        with nc.Block() as block:
            @block.tensor
            def _(e):
                self.emit("pe", e)

            @block.scalar
            def _(e):
                self.emit("act", e)

            @block.vector
            def _(e):
                self.emit("dve", e)

            @block.gpsimd
            def _(e):
                self.emit("pool", e)

            @block.sync
            def _(e):
                self.emit("sp", e)


class Ring:
    def __init__(self, items):
        self.items = items
        self.i = 0

    def next(self):
        it = self.items[self.i % len(self.items)]
        self.i += 1
        return it


def build_program(stop_after=None):
    nc = bass.Bass("TRN2", target_bir_lowering=False)

    def din(name, shape, dt=F32):
        return nc.dram_tensor(name, shape, dt, kind="ExternalInput").ap()

    x = din("x", [4096, 1024])
    ctx = din("ctx", [256, 1024])
    cc = din("cc", [128, 16])
    w_ada = din("w_ada", [1024, 6144])
    b_ada = din("b_ada", [1, 6144])
    colpack = din("colpack", [128, 16])
    rowpack = din("rowpack", [1, 4104])
    browf = din("browf", [1, 3872])
    w_in = din("w_in", [1024, 1792])
    w_out = din("w_out", [1024, 1024])
    w_r = din("w_r", [1024, 32])
    sguwT = din("sguwT", [128, 1024])
    b_down = din("b_down", [32, 1024])
    if stop_after is None:
        wgu = [din("wgu%d" % k, [4096, 2048]) for k in range(8)]
        wd = [din("wd%d" % k, [4096, 1024]) for k in range(8)]
        bgu = din("bgu", [4096, 16])
    cbf_d = din("cbf", [128, 640])
    cf32_d = din("cf32", [128, 32 + NB + 9])
    rope_d = din("rope", [4096, 128])
    out = nc.dram_tensor("out", [4096, 1024], F32, kind="ExternalOutput").ap()
    ik = "Internal" if stop_after is None else "ExternalOutput"
    xs_d = nc.dram_tensor("xs_d", [NB * R, 1024], BF16, kind=ik).ap()
    ys_d = nc.dram_tensor("ys_d", [NB * R, 1024], F32, kind=ik).ap()
    xmid_d = nc.dram_tensor("xmid_d", [4096, 1024], F32, kind=ik).ap()
    h2_d = nc.dram_tensor("h2_d", [4096, 1024], BF16, kind=ik).ap()
    if stop_after is not None:
        dbg_route = nc.dram_tensor("dbg_route", [128, 3 * NT * 4 + 32], F32, kind="ExternalOutput").ap()
        dbg_book = nc.dram_tensor("dbg_book", [128, NT * 4 + NB], I32, kind="ExternalOutput").ap()

    with ExitStack() as st_all:
        S = Sched(nc, st_all)

        def sbuf(st, name, shape, dt):
            return st.enter_context(nc.sbuf_tensor("s_" + name, shape, dt))

        def mkring(st, name, shape, dt, n):
            return Ring([(sbuf(st, "%s%d" % (name, k), shape, dt), Reg(name)) for k in range(n)])

        def op1(eng, meth, reads, writes, *a, **k):
            S.op(eng, (lambda e: getattr(e, meth)(*a, **k)), reads, writes)

        def dma(eng, out_ap, in_ap, reads, writes, ds, **k):
            S.op(eng, (lambda e: e.dma_start(out=out_ap, in_=in_ap, **k)), reads, writes, dsem=ds)

        def pe_mm(mms, reads, writes):
            mms = list(mms)

            def fn(e):
                ins = None
                for (o, l, r, a, b) in mms:
                    ins = e.matmul(o, l, r, start=a, stop=b)
                return ins
            S.op("pe", fn, reads, writes)

        def pe_tr(trs, reads, writes):
            trs = list(trs)

            def fn(e):
                ins = None
                for (o, i_) in trs:
                    ins = e.transpose(o, i_, ident)
                return ins
            S.op("pe", fn, reads, writes)

        pbig = [st_all.enter_context(nc.psum_tensor("pb%d" % k, [128, 1024], F32)) for k in range(3)]
        pslots = []
        for k in range(3):
            for h in range(2):
                pslots.append((pbig[k], h, Reg("ps%d%d" % (k, h))))
        pstate = {"i": 0}

        def ps_next():
            t, h, r = pslots[pstate["i"] % 6]
            pstate["i"] += 1
            return t[:, h * 512:(h + 1) * 512], r

        def ps_pair():
            if pstate["i"] % 2 == 1:
                pstate["i"] += 1
            t, _, r0 = pslots[pstate["i"] % 6]
            _, _, r1 = pslots[(pstate["i"] + 1) % 6]
            pstate["i"] += 2
            return t, [r0, r1]

        ptr = Ring([(st_all.enter_context(nc.psum_tensor("pt%d" % k, [128, 8, 128], BF16)), Reg("pt")) for k in range(2)])

        cbf = sbuf(st_all, "cbf", [128, 640], BF16)
        ident = cbf[:, 0:128]
        Utri = cbf[:, 128:256]
        ones_bf = cbf[:, 256:384]
        mprev = cbf[:, 384:512]
        mnext = cbf[:, 512:640]
        cf32 = sbuf(st_all, "cf32", [128, 32 + NB + 9], F32)
        iota_e = cf32[:, 0:32]
        bstart = cf32[:, 32:32 + NB]
        pidx = cf32[:, 32 + NB:33 + NB]
        thr8 = cf32[:, 33 + NB:41 + NB]
        MR = sbuf(st_all, "MR", [128, 4, 1024], F32)
        bdown_bf = sbuf(st_all, "bdown_bf", [32, 1024], BF16)
        rank_all = sbuf(st_all, "rank_all", [128, NT * 4], F32)
        ek_all = sbuf(st_all, "ek_all", [128, NT * 4], F32)
        gk_all = sbuf(st_all, "gk_all", [128, NT * 4], F32)
        dest_i = sbuf(st_all, "dest_i", [128, NT * 4], I32)
        runc = sbuf(st_all, "runc", [128, 32], F32)
        eps_t = sbuf(st_all, "eps_t", [128, 1], F32)
        esink = sbuf(st_all, "esink", [128, 8], F32)
        A1 = sbuf(st_all, "A1", [128, 8, 2], F32)
        B1 = sbuf(st_all, "B1", [128, 8, 2], F32)
        colp = sbuf(st_all, "colp", [128, 16], F32)
        ones_f = sbuf(st_all, "ones_f", [1, 128], F32)
        widx = sbuf(st_all, "widx", [128, NB], I32)
        r_const = Reg("const")
        r_MR = Reg("MR")
        r_AB = Reg("AB")
        r_route = Reg("route")
        r_run = Reg("run")
        r_bdown = Reg("bdown")
        r_dest = Reg("dest")
        r_eblk = Reg("eblk")

        d_const = S.dsem("const")
        d_out = [S.dsem("out0"), S.dsem("out1")]

        st_mix = ExitStack()
        w_in_bf = sbuf(st_mix, "w_in_bf", [128, 8, 1792], BF16)
        w_out_bf = sbuf(st_mix, "w_out_bf", [128, 8, 1024], BF16)
        wr_bf = sbuf(st_mix, "wr_bf", [128, 8, 32], BF16)
        sguw_bf = sbuf(st_mix, "sguw_bf", [128, 1024], BF16)
        brow_bf = sbuf(st_mix, "brow_bf", [1, 3872], BF16)
        lngb = sbuf(st_mix, "lngb", [128, 1024], F32)
        kT2r = [(sbuf(st_mix, "kT2_%d" % k, [128, 2, 128], BF16), Reg("kT2")) for k in range(4)]
        vaugr = [(sbuf(st_mix, "vaug_%d" % k, [128, 2, 66], BF16), Reg("vaug")) for k in range(4)]
        kcT2 = sbuf(st_mix, "kcT2", [128, 2, 256], BF16)
        vcaug = sbuf(st_mix, "vcaug", [128, 2, 2, 66], BF16)
        r_kc = Reg("kc")
        r_vc = Reg("vc")
        r_w = Reg("wts")
        r_brow = Reg("brow")
        r_lngb = Reg("lngb")

        st0 = ExitStack()
        cstage = sbuf(st0, "cstage", [128, 640], F32)
        slab = mkring(st0, "slab", [128, 8, 512], F32, 2)
        bada_sb = sbuf(st0, "bada_sb", [1, 6144], F32)
        stg = mkring(st0, "stg", [128, 1792], F32, 2)
        browf_sb = sbuf(st0, "browf_sb", [1, 3872], F32)
        cc_sb = sbuf(st0, "cc_sb", [128, 8, 2], F32)
        sc_f = sbuf(st0, "sc_f", [128, 8, 2], F32)
        rep_f = sbuf(st0, "rep_f", [128, 8, 128], F32)
        modcol = sbuf(st0, "modcol", [128, 16, 2], F32)
        rows_bc = sbuf(st0, "rows_bc", [128, 3, 1024], F32)
        tmp8 = sbuf(st0, "tmp8", [128, 8, 2], F32)
        bdown_f = sbuf(st0, "bdown_f", [32, 1024], F32)
        wr_f = sbuf(st0, "wr_f", [128, 8, 32], F32)
        sguw_f = sbuf(st0, "sguw_f", [128, 1024], F32)
        sink_bc = sbuf(st0, "sink_bc", [128, 8], F32)
        r0 = {k: Reg(k) for k in ("cstage", "bada", "browf", "cc", "sc", "rep", "modcol", "rows", "tmp8",
                                  "bdown_f", "wr_f", "sguw_f", "sink")}

        dma("sp", cstage[:], cbf_d, [], [r0["cstage"]], d_const)
        dma("sp", cf32[:], cf32_d, [], [r_const], d_const)
        dma("sp", cc_sb[:].rearrange("p j s -> p (j s)"), cc, [], [r0["cc"]], d_const)
        dma("sp", bada_sb[:], b_ada, [], [r0["bada"]], d_const)
        dma("sp", browf_sb[:], browf, [], [r0["browf"]], d_const)
        dma("sp", rows_bc[:].rearrange("p a n -> p (a n)"), rowpack[0:1, 0:3072].partition_broadcast(128)[:, 0, :],
            [], [r0["rows"]], d_const)
        dma("sp", lngb[:], rowpack[0:1, 3072:4096].partition_broadcast(128)[:, 0, :], [], [r_lngb], d_const)
        dma("sp", sink_bc[:], rowpack[0:1, 4096:4104].partition_broadcast(128)[:, 0, :], [], [r0["sink"]], d_const)
        dma("sp", bdown_f[:], b_down, [], [r0["bdown_f"]], d_const)
        dma("sp", wr_f[:], w_r.rearrange("(j p) n -> p j n", p=128), [], [r0["wr_f"]], d_const)
        dma("sp", sguw_f[:], sguwT, [], [r0["sguw_f"]], d_const)
        d_const.seal()
        d_const2 = S.dsem("const2")
        dma("sp", colp[:], colpack, [], [r_const], d_const2)

        op1("pool", "memset", [], [r_const], eps_t[:], EPS)
        op1("pool", "memset", [], [r_const], ones_f[:], 1.0)
        op1("pool", "memset", [], [r_run], runc[:], 0.0)
        op1("act", "activation", [r0["cstage"]], [r_const], cbf[:], cstage[:], AF.Copy)
        op1("act", "activation", [r0["sink"]], [r_const], esink[:], sink_bc[:], AF.Exp)
        op1("act", "activation", [r0["cc"]], [r0["sc"]], sc_f[:], cc_sb[:], AF.Silu)
        op1("dve", "tensor_copy", [r0["sc"]], [r0["rep"]], rep_f[:], sc_f[:, :, 0:1].to_broadcast([128, 8, 128]))
        op1("dve", "tensor_copy", [r0["bdown_f"]], [r_bdown], bdown_bf[:], bdown_f[:])
        op1("dve", "tensor_copy", [r0["wr_f"]], [r_w], wr_bf[:], wr_f[:])
        op1("dve", "tensor_copy", [r0["sguw_f"]], [r_w], sguw_bf[:], sguw_f[:])
        op1("dve", "tensor_copy", [r0["browf"]], [r_brow], brow_bf[:], browf_sb[:])

        zt = sbuf(st0, "zt", [128, 4096], BF16)
        r_zt = Reg("zt")
        r_xs = Reg("xs_d")
        d_zero = S.dsem("zero")
        op1("pool", "memset", [], [r_zt], zt[:], 0.0)
        nz = NB * R // 512
        for k in range(nz):
            dma("sp", xs_d[k * 512:(k + 1) * 512, :].rearrange("(p a) n -> p (a n)", p=128), zt[:],
                [r_zt], [Reg("xsz")], d_zero)
        zero_ops = list(d_zero.ops)
        d_zero.seal()

        d_slab = [S.dsem("slab0"), S.dsem("slab1")]
        pcol, r_pcol = ps_next()
        for n in range(12):
            sl, r_sl = slab.next()
            dma("sp", sl[:], w_ada[:, n * 512:(n + 1) * 512].rearrange("(j p) n -> p j n", p=128),
                [], [r_sl], d_slab[n % 2])
            if n < 4:
                mms = []
                for fc in range(4):
                    ci = n * 4 + fc
                    o = pcol[:, ci * 2:ci * 2 + 2]
                    for j in range(8):
                        mms.append((o, sl[:, j, fc * 128:(fc + 1) * 128], sc_f[:, j, :], j == 0, False))
                    mms.append((o, bada_sb[0:1, ci * 128:(ci + 1) * 128], ones_f[0:1, 0:2], False, True))
                pe_mm(mms, [r_sl, r0["sc"], r0["bada"], r_const], [r_pcol])
                if n == 3:
                    op1("dve", "tensor_copy", [r_pcol], [r0["modcol"]],
                        modcol[:].rearrange("p a s -> p (a s)"), pcol[:, 0:32])
            else:
                pr, r_pr = ps_next()
                mms = [(pr, rep_f[:, j, :], sl[:, j, :], j == 0, False) for j in range(8)]
                mms.append((pr, ones_f[0:1, 0:128], bada_sb[0:1, n * 512:(n + 1) * 512], False, True))
                pe_mm(mms, [r_sl, r0["rep"], r0["bada"], r_const], [r_pr])
                q4 = (n - 4) // 2
                h4 = (n - 4) % 2
                op1("act", "activation", [r_pr], [r_MR], MR[:, q4, h4 * 512:(h4 + 1) * 512], pr, AF.Copy)
        op1("dve", "tensor_scalar", [r0["modcol"]], [r0["tmp8"]], tmp8[:], modcol[:, 8:16, :], 1.0, None, ALU.add)
        op1("dve", "tensor_tensor", [r0["tmp8"], r_const], [r_AB], A1[:], tmp8[:],
            colp[:, 0:8].unsqueeze(2).to_broadcast([128, 8, 2]), ALU.mult)
        op1("dve", "tensor_copy", [r0["modcol"]], [r_AB], B1[:], modcol[:, 0:8, :])
        op1("pool", "tensor_tensor", [r_MR, r0["rows"]], [r_MR], MR[:, 0, :], MR[:, 0, :], rows_bc[:, 0, :], ALU.mult)
        op1("dve", "scalar_tensor_tensor", [r_MR, r0["rows"]], [r_MR], MR[:, 2, :], MR[:, 2, :], 1.0,
            rows_bc[:, 1, :], ALU.add, ALU.mult)
        op1("pool", "tensor_tensor", [r_MR, r0["rows"]], [r_MR], MR[:, 3, :], MR[:, 3, :], rows_bc[:, 2, :], ALU.mult)

        d_stg = [S.dsem("stg0"), S.dsem("stg1")]
        for j in range(8):
            sg, r_sg = stg.next()
            dma("sp", sg[:], w_in[j * 128:(j + 1) * 128, :], [], [r_sg], d_stg[j % 2])
            op1("pool", "tensor_copy", [r_sg], [r_w], w_in_bf[:, j, :], sg[:])
        for j in range(8):
            sg, r_sg = stg.next()
            dma("sp", sg[:, 0:1024], w_out[j * 128:(j + 1) * 128, :], [], [r_sg], d_stg[j % 2])
            op1("pool", "tensor_copy", [r_sg], [r_w], w_out_bf[:, j, :], sg[:, 0:1024])

        allr0 = list(r0.values()) + [r_zt] + [r for (_, r) in slab.items] + [r for (_, r) in stg.items]
        for e in Sched.ENGS:
            S.op(e, None, reads=allr0)
        st0.close()

        st1 = ExitStack()
        xt_r = mkring(st1, "xt", [128, 1024], F32, 4)
        d_xt = [S.dsem("xt%d" % k) for k in range(4)]
        rp_r = mkring(st1, "rp", [128, 128], F32, 3)
        d_rp = [S.dsem("rp%d" % k) for k in range(3)]
        junk = sbuf(st1, "junk", [128, 1024], BF16)
        r_junk = Reg("junk")
        xs_r = mkring(st1, "xsb", [128, 1024], BF16, 2)
        hT_r = mkring(st1, "hT", [128, 8, 128], BF16, 2)
        qk_r = mkring(st1, "qk", [128, 768], BF16, 2)
        rA = sbuf(st1, "rA", [128, 512], F32)
        rB = sbuf(st1, "rB", [128, 512], F32)
        r_rA = Reg("rA")
        r_rB = Reg("rB")
        qT_r = mkring(st1, "qT", [128, 4, 128], BF16, 3)
        u_r = mkring(st1, "u", [128, 512], BF16, 2)
        gv_r = mkring(st1, "gv", [128, 512], F32, 2)
        z_r = mkring(st1, "z", [128, 512], F32, 2)
        vln_r = mkring(st1, "vln", [128, 512], BF16, 2)
        sgo_r = mkring(st1, "sgo", [128, 512], F32, 3)
        p_r = mkring(st1, "p", [128, 512], BF16, 7)
        ao_r = mkring(st1, "ao", [128, 512], F32, 2)
        on_r = mkring(st1, "on", [128, 1024], BF16, 2)
        oT_r = mkring(st1, "oT", [128, 8, 128], BF16, 2)
        tm_r = mkring(st1, "tm", [128, 1024], F32, 1)
        xm_r = mkring(st1, "xm", [128, 1024], F32, 2)
        d_xm = [S.dsem("xm0"), S.dsem("xm1")]
        h2f_r = mkring(st1, "h2f", [128, 1024], F32, 1)
        h2b_r = mkring(st1, "h2b", [128, 1024], BF16, 2)
        d_h2 = [S.dsem("h2b0"), S.dsem("h2b1")]
        h2T_r = mkring(st1, "h2T", [128, 8, 128], BF16, 2)
        st_r = mkring(st1, "stat", [128, 16], F32, 6)
        bn_r = mkring(st1, "bn", [128, 8], F32, 2)
        lg_r = mkring(st1, "lg", [128, 32], F32, 2)
        t8_r = mkring(st1, "t8", [128, 8], F32, 2)
        mk_r = mkring(st1, "mk", [128, 32], F32, 2)
        mkb_r = mkring(st1, "mkb", [128, 32], BF16, 2)
        ex_r = mkring(st1, "ex", [128, 32], F32, 2)
        oh_r = mkring(st1, "oh", [128, 4, 32], F32, 2)
        Dm_r = mkring(st1, "Dm", [128, 32], F32, 2)
        tq_r = mkring(st1, "tq", [128, 4, 32], F32, 2)
        r_h2d = Reg("h2_d")
        r_xmd = Reg("xmid_d")

        BIN, BOUT, BSGU, BRT = 0, 1792, 2816, 3840

        def rstd_from(ss_ap, out_ap, scale, r_in, r_out):
            op1("act", "activation", [r_in, r_const], [r_out], out_ap, ss_ap, AF.Sqrt, bias=eps_t[:], scale=scale)
            op1("dve", "reciprocal", [r_out], [r_out], out_ap, out_ap)

        def norm_T(src, r_src, colA, colB, s_idx, hT, r_hT, xsb, r_xsb, stt, r_stt):
            op1("act", "activation", [r_src], [r_junk, r_stt], junk[:], src, AF.Square, accum_out=stt[:, 0:1])
            rstd_from(stt[:, 0:1], stt[:, 1:2], 1.0 / 1024, r_stt, r_stt)
            op1("dve", "tensor_scalar", [r_src, r_stt], [r_xsb], xsb[:], src, stt[:, 1:2], None, ALU.mult)
            pt, r_pt = ptr.next()
            pe_tr([(pt[:, j, :], xsb[:, j * 128:(j + 1) * 128]) for j in range(8)], [r_xsb, r_const], [r_pt])
            for j in range(8):
                S.op("act", (lambda e, j=j: e.activation(hT[:, j, :], pt[:, j, :], AF.Identity,
                                                         bias=colB[:, j, s_idx:s_idx + 1],
                                                         scale=colA[:, j, s_idx:s_idx + 1])),
                     [r_pt, r_AB], [r_hT])

        op1("pool", "memset", [], [r_vc], vcaug[:], 1.0)
        for k in range(4):
            op1("pool", "memset", [], [vaugr[k][1]], vaugr[k][0][:], 1.0)
        for ci in range(2):
            xt, r_xt = xt_r.next()
            dma("sp", xt[:], ctx[ci * 128:(ci + 1) * 128, :], [], [r_xt], d_xt[(xt_r.i - 1) % 4])
            xsb, r_xsb = xs_r.next()
            hT, r_hT = hT_r.next()
            stt, r_stt = st_r.next()
            norm_T(xt[:], r_xt, A1, B1, 1, hT, r_hT, xsb, r_xsb, stt, r_stt)
            pk, r_pk = ps_next()
            mms = [(pk[:, 0:256], hT[:, j, :], w_in_bf[:, j, 1536:1792], j == 0, False) for j in range(8)]
            mms.append((pk[:, 0:256], ones_bf[0:1, :], brow_bf[0:1, BIN + 1536:BIN + 1792], False, True))
            pe_mm(mms, [r_hT, r_w, r_brow, r_const], [r_pk])
            qk, r_qk = qk_r.next()
            for dup in range(2):
                S.op("act", (lambda e, dup=dup, qk=qk, pk=pk: e.activation(
                    qk[:, 0:256].rearrange("p (g d c) -> p g d c", g=2, d=2)[:, :, dup, :],
                    pk[:, 0:128].rearrange("p (g c) -> p g c", g=2), AF.Copy)), [r_pk], [r_qk])
            op1("act", "activation", [r_pk], [r_vc], vcaug[:, ci, :, 0:64],
                pk[:, 128:256].rearrange("p (g c) -> p g c", g=2), AF.Copy)
            pt, r_pt = ptr.next()
            pe_tr([(pt[:, g, :], qk[:, g * 128:(g + 1) * 128]) for g in range(2)], [r_qk, r_const], [r_pt])
            op1("dve", "tensor_copy", [r_pt], [r_kc], kcT2[:, :, ci * 128:(ci + 1) * 128], pt[:, 0:2, :])

        if stop_after == 0:
            dbg_kc = nc.dram_tensor("dbg_kc", [128, 512], BF16, kind="ExternalOutput").ap()
            d_dbg = S.dsem("dbg")
            rr = Reg("dbgo")
            dma("sp", dbg_kc, kcT2[:].rearrange("p g k -> p (g k)"), [r_kc], [rr], d_dbg)
            o = S.op("sp", None, reads=[rr])
            o.deps.update(z.idx for z in zero_ops)
            S.run()
            st1.close()
            st_mix.close()
            return nc
        tiles = {}

        def rope(src, H, dst3, rp, reads, r_dst):
            X = src.rearrange("p (h c) -> p h c", h=H)
            n = H * 64
            Av = rA[:, 0:n].rearrange("p (h c) -> p h c", h=H)
            Bv = rB[:, 0:n].rearrange("p (h c) -> p h c", h=H)
            op1("dve", "tensor_tensor", reads, [r_rA], Av, X, rp[:, 0:64].unsqueeze(1).to_broadcast([128, H, 64]),
                ALU.mult)
            for ax in range(2):
                for hf in range(2):
                    o0 = ax * 32 + hf * 16
                    i0 = ax * 32 + (1 - hf) * 16
                    op1("dve", "tensor_tensor", reads, [r_rB], Bv[:, :, o0:o0 + 16], X[:, :, i0:i0 + 16],
                        rp[:, 64 + o0:64 + o0 + 16].unsqueeze(1).to_broadcast([128, H, 16]), ALU.mult)
            for d3 in dst3:
                op1("pool", "tensor_tensor", [r_rA, r_rB], [r_dst], d3, Av, Bv, ALU.add)

        def stageA(i):
            T = {}
            xt, r_xt = xt_r.next()
            dma("sp", xt[:], x[i * 128:(i + 1) * 128, :], [], [r_xt], d_xt[(xt_r.i - 1) % 4])
            rp, r_rp = rp_r.next()
            dma("sp", rp[:], rope_d[i * 128:(i + 1) * 128, :], [], [r_rp], d_rp[(rp_r.i - 1) % 3])
            T["xt"] = (xt, r_xt)
            xsb, r_xsb = xs_r.next()
            hT, r_hT = hT_r.next()
            stt, r_stt = st_r.next()
            norm_T(xt[:], r_xt, A1, B1, 0, hT, r_hT, xsb, r_xsb, stt, r_stt)
            chunks = []
            for (c0, cw) in ((0, 512), (512, 512), (1024, 512), (1536, 128), (1664, 128)):
                pc, r_pc = ps_next()
                mms = [(pc[:, 0:cw], hT[:, j, :], w_in_bf[:, j, c0:c0 + cw], j == 0, False) for j in range(8)]
                mms.append((pc[:, 0:cw], ones_bf[0:1, :], brow_bf[0:1, BIN + c0:BIN + c0 + cw], False, True))
                pe_mm(mms, [r_hT, r_w, r_brow, r_const], [r_pc])
                chunks.append((pc, r_pc))
            (pq, r_pq), (psu, r_psu), (psv, r_psv), (pkv, r_pkv), (pvv, r_pvv) = chunks
            qk, r_qk = qk_r.next()
            rope(pq, 8, [qk[:, 0:512].rearrange("p (h c) -> p h c", h=8)], rp, [r_pq, r_rp], r_qk)
            kd = qk[:, 512:768].rearrange("p (g d c) -> p g d c", g=2, d=2)
            rope(pkv[:, 0:128], 2, [kd[:, :, 0, :], kd[:, :, 1, :]], rp, [r_pkv, r_rp], r_qk)
            if SUB == 1:
                return
            va, r_va = vaugr[i % 4]
            if SUB == 7:
                op1("dve", "tensor_copy", [r_pkv], [r_va], va[:, :, 0:64],
                    pkv[:, 128:256].rearrange("p (g c) -> p g c", g=2))
                return
            if SUB == 71:
                op1("dve", "tensor_copy", [r_pkv], [r_va], va[:, :, 0:64],
                    pkv[:, 0:128].rearrange("p (g c) -> p g c", g=2))
                return
            if SUB == 72:
                op1("dve", "tensor_copy", [r_qk], [r_va], va[:, :, 0:64],
                    qk[:, 0:128].rearrange("p (g c) -> p g c", g=2))
                return
            if SUB == 73:
                op1("dve", "tensor_copy", [r_pkv], [r_va], va[:, 0, 0:64], pkv[:, 128:192])
                return
            if SUB == 74:
                op1("dve", "tensor_copy", [r_pkv], [r_va], va[:, :, 0:64],
                    pkv[:, 256:384].rearrange("p (g c) -> p g c", g=2))
                return
            if SUB == 75:
                op1("dve", "tensor_copy", [r_pkv], [r_va], va[:, :, 0:64],
                    pkv[:, 64:192].rearrange("p (g c) -> p g c", g=2))
                return
            if SUB == 76:
                op1("dve", "tensor_copy", [r_pkv], [r_va], va[:, 0, 0:64], pkv[:, 192:256])
                return
            if SUB == 77:
                op1("dve", "tensor_copy", [r_pq], [r_va], va[:, 0, 0:64], pq[:, 192:256])
                return
            if SUB == 78:
                op1("dve", "tensor_copy", [r_pq], [r_va], va[:, 0, 0:64], pq[:, 448:512])
                return
            if SUB == 8:
                op1("act", "activation", [r_pkv], [r_junk], junk[:, 0:128].rearrange("p (g c) -> p g c", g=2),
                    pkv[:, 128:256].rearrange("p (g c) -> p g c", g=2), AF.Copy)
                return
            if SUB == 9:
                op1("act", "activation", [r_pkv], [r_va], va[:, :, 0:64],
                    pkv[:, 128:256].rearrange("p (g c) -> p g c", g=2), AF.Identity)
                return
            op1("act", "activation", [r_pvv], [r_va], va[:, :, 0:64],
                pvv[:, 0:128].rearrange("p (g c) -> p g c", g=2), AF.Copy)
            if SUB == 4:
                return
            pt, r_pt = ptr.next()
            pe_tr([(pt[:, a, :], qk[:, a * 128:(a + 1) * 128]) for a in range(6)], [r_qk, r_const], [r_pt])
            if SUB == 5:
                return
            qT, r_qT = qT_r.next()
            kT, r_kT = kT2r[i % 4]
            op1("dve", "tensor_copy", [r_pt], [r_qT], qT[:], pt[:, 0:4, :])
            if SUB == 6:
                return
            op1("dve", "tensor_copy", [r_pt], [r_kT], kT[:], pt[:, 4:6, :])
            T["qT"] = (qT, r_qT)
            if SUB == 2:
                return
            u, r_u = u_r.next()
            op1("act", "activation", [r_psu], [r_u], u[:], psu, AF.Gelu_apprx_tanh)
            gv, r_gv = gv_r.next()
            op1("act", "activation", [r_psv], [r_gv], gv[:], psv, AF.Gelu_apprx_tanh)
            bn, r_bn = bn_r.next()
            op1("dve", "bn_stats", [r_gv], [r_bn], bn[:, 0:6], gv[:])
            op1("dve", "bn_aggr", [r_bn], [r_bn], bn[:, 6:8], bn[:, 0:6])
            rstd_from(bn[:, 7:8], bn[:, 7:8], 1.0, r_bn, r_bn)
            z, r_z = z_r.next()
            op1("dve", "tensor_scalar", [r_gv, r_bn], [r_z], z[:], gv[:], bn[:, 6:7], bn[:, 7:8], ALU.subtract, ALU.mult)
            op1("pool", "tensor_tensor", [r_z, r_lngb], [r_z], z[:], z[:], lngb[:, 0:512], ALU.mult)
            vln, r_vln = vln_r.next()
            op1("pool", "tensor_tensor", [r_z, r_lngb], [r_vln], vln[:], z[:], lngb[:, 512:1024], ALU.add)
            if SUB == 3:
                return
            pm, r_pm = ps_next()
            mms = []
            for h in range(8):
                o = pm[:, h * 64:(h + 1) * 64]
                mms.append((o, sguw_bf[:, h * 128:(h + 1) * 128], vln[:, h * 64:(h + 1) * 64], True, False))
                mms.append((o, brow_bf[0:1, BSGU + h * 128:BSGU + (h + 1) * 128], ones_bf[0:1, 0:64], False, True))
            pe_mm(mms, [r_vln, r_w, r_brow, r_const], [r_pm])
            sgo, r_sgo = sgo_r.next()
            op1("dve", "tensor_tensor", [r_pm, r_u], [r_sgo], sgo[:], pm, u[:], ALU.mult)
            T["sgo"] = (sgo, r_sgo)
            tiles[i] = T

        SCALE = 64 ** -0.5

        def stageB(i):
            T = tiles.pop(i)
            xt, r_xt = T["xt"]
            qT, r_qT = T["qT"]
            sgo, r_sgo = T["sgo"]
            ao, r_ao = ao_r.next()
            for g in range(2):
                klist = []
                if i > 0:
                    klist.append(("p", kT2r[(i - 1) % 4], vaugr[(i - 1) % 4], None))
                klist.append(("o", kT2r[i % 4], vaugr[i % 4], None))
                if i < NT - 1:
                    klist.append(("n", kT2r[(i + 1) % 4], vaugr[(i + 1) % 4], None))
                klist.append(("c", (kcT2, r_kc), (vcaug, r_vc), 0))
                klist.append(("c", (kcT2, r_kc), (vcaug, r_vc), 1))
                plist = []
                for (kind, (kt, r_kt), (vt, r_vt), ci) in klist:
                    pss, r_pss = ps_pair()
                    mms = []
                    for h4 in range(4):
                        h = 4 * g + h4
                        a, hf = h // 2, h % 2
                        if kind == "c":
                            ks = kt[hf * 64:(hf + 1) * 64, g, ci * 128:(ci + 1) * 128]
                        else:
                            ks = kt[hf * 64:(hf + 1) * 64, g, :]
                        oc = hf * 512 + (h4 // 2) * 128
                        mms.append((pss[:, oc:oc + 128], ks, qT[hf * 64:(hf + 1) * 64, a, :], True, True))
                    pe_mm(mms, [r_kt, r_qT], r_pss)
                    pp, r_pp = p_r.next()
                    op1("act", "activation", r_pss, [r_pp], pp[:].rearrange("p (b c) -> p b c", b=2),
                        pss[:].rearrange("p (b c) -> p b c", b=2)[:, :, 0:256], AF.Exp, scale=SCALE)
                    if kind in ("p", "n"):
                        mk = mprev if kind == "p" else mnext
                        op1("pool", "tensor_tensor", [r_pp, r_const], [r_pp],
                            pp[:].rearrange("p (h q) -> p h q", h=4), pp[:].rearrange("p (h q) -> p h q", h=4),
                            mk.unsqueeze(1).to_broadcast([128, 4, 128]), ALU.mult)
                    if kind == "c":
                        vs = vt[:, ci, g, 0:65]
                    else:
                        vs = vt[:, g, 0:65]
                    plist.append((pp, r_pp, vs, r_vt))
                if SUB == 21:
                    return
                po, r_po = ps_next()
                mms = []
                rd = []
                for h4 in range(4):
                    for k, (pp, r_pp, vs, r_vt) in enumerate(plist):
                        pc_ = (h4 % 2) * 256 + (h4 // 2) * 128
                        mms.append((po[:, h4 * 66:h4 * 66 + 65], pp[:, pc_:pc_ + 128], vs,
                                    k == 0, k == len(plist) - 1))
                        rd += [r_pp, r_vt]
                pe_mm(mms, rd, [r_po])
                stt, r_stt = st_r.next()
                po3 = po[:, 0:264].rearrange("p (h c) -> p h c", h=4)
                op1("dve", "tensor_tensor", [r_po, r_const], [r_stt], stt[:, 0:4], po3[:, :, 64], esink[:, 4 * g:4 * g + 4],
                    ALU.add)
                op1("dve", "reciprocal", [r_stt], [r_stt], stt[:, 4:8], stt[:, 0:4])
                op1("dve", "tensor_tensor", [r_po, r_stt], [r_ao],
                    ao[:, g * 256:(g + 1) * 256].rearrange("p (h c) -> p h c", h=4), po3[:, :, 0:64],
                    stt[:, 4:8].unsqueeze(2).to_broadcast([128, 4, 64]), ALU.mult)
                if SUB == 22:
                    return
            if SUB == 23:
                return
            stt, r_stt = st_r.next()
            op1("act", "activation", [r_ao], [r_junk, r_stt], junk[:, 0:512], ao[:], AF.Square, accum_out=stt[:, 0:1])
            op1("act", "activation", [r_sgo], [r_junk, r_stt], junk[:, 512:1024], sgo[:], AF.Square,
                accum_out=stt[:, 1:2])
            rstd_from(stt[:, 0:2], stt[:, 2:4], 1.0 / 512, r_stt, r_stt)
            on, r_on = on_r.next()
            op1("dve", "tensor_scalar", [r_ao, r_stt], [r_on], on[:, 0:512], ao[:], stt[:, 2:3], None, ALU.mult)
            op1("pool", "tensor_scalar", [r_sgo, r_stt], [r_on], on[:, 512:1024], sgo[:], stt[:, 3:4], None, ALU.mult)
            pt, r_pt = ptr.next()
            pe_tr([(pt[:, j, :], on[:, j * 128:(j + 1) * 128]) for j in range(8)], [r_on, r_const], [r_pt])
            oT, r_oT = oT_r.next()
            op1("dve", "tensor_tensor", [r_pt, r_const], [r_oT], oT[:], pt[:],
                colp[:, 8:16].unsqueeze(2).to_broadcast([128, 8, 128]), ALU.mult)
            if SUB == 24:
                return
            pmix, r_pmix = ps_pair()
            mms = []
            for hh in range(2):
                o = pmix[:, hh * 512:(hh + 1) * 512]
                for j in range(8):
                    mms.append((o, oT[:, j, :], w_out_bf[:, j, hh * 512:(hh + 1) * 512], j == 0, False))
                mms.append((o, ones_bf[0:1, :], brow_bf[0:1, BOUT + hh * 512:BOUT + (hh + 1) * 512], False, True))
            pe_mm(mms, [r_oT, r_w, r_brow, r_const], r_pmix)
            stt, r_stt = st_r.next()
            op1("act", "activation", r_pmix, [r_junk, r_stt], junk[:], pmix[:], AF.Square, accum_out=stt[:, 0:1])
            rstd_from(stt[:, 0:1], stt[:, 1:2], 1.0 / 1024, r_stt, r_stt)
            tm, r_tm = tm_r.next()
            op1("dve", "scalar_tensor_tensor", r_pmix + [r_stt, r_MR], [r_tm], tm[:], pmix[:], stt[:, 1:2], MR[:, 0, :],
                ALU.mult, ALU.mult)
            xm, r_xm = xm_r.next()
            kx = (xm_r.i - 1) % 2
            op1("pool", "tensor_tensor", [r_tm, r_xt], [r_xm], xm[:], tm[:], xt[:], ALU.add)
            dma("sp", xmid_d[i * 128:(i + 1) * 128, :], xm[:], [r_xm], [r_xmd], d_xm[kx])
            if SUB == 25:
                return
            op1("act", "activation", [r_xm], [r_junk, r_stt], junk[:], xm[:], AF.Square, accum_out=stt[:, 2:3])
            rstd_from(stt[:, 2:3], stt[:, 3:4], 1.0 / 1024, r_stt, r_stt)
            h2f, r_h2f = h2f_r.next()
            op1("dve", "scalar_tensor_tensor", [r_xm, r_stt, r_MR], [r_h2f], h2f[:], xm[:], stt[:, 3:4], MR[:, 2, :],
                ALU.mult, ALU.mult)
            h2b, r_h2b = h2b_r.next()
            kh = (h2b_r.i - 1) % 2
            op1("pool", "tensor_tensor", [r_h2f, r_MR], [r_h2b], h2b[:], h2f[:], MR[:, 1, :], ALU.add)
            dma("sp", h2_d[i * 128:(i + 1) * 128, :], h2b[:], [r_h2b], [r_h2d], d_h2[kh])
            if SUB == 26:
                return
            pt, r_pt = ptr.next()
            pe_tr([(pt[:, j, :], h2b[:, j * 128:(j + 1) * 128]) for j in range(8)], [r_h2b, r_const], [r_pt])
            h2T, r_h2T = h2T_r.next()
            op1("act", "activation", [r_pt], [r_h2T], h2T[:], pt[:], AF.Copy)
            pl, r_pl = ps_next()
            mms = [(pl[:, 0:32], h2T[:, j, :], wr_bf[:, j, :], j == 0, False) for j in range(8)]
            mms.append((pl[:, 0:32], ones_bf[0:1, :], brow_bf[0:1, BRT:BRT + 32], False, True))
            pe_mm(mms, [r_h2T, r_w, r_brow, r_const], [r_pl])
            lg, r_lg = lg_r.next()
            op1("dve", "tensor_copy", [r_pl], [r_lg], lg[:], pl[:, 0:32])
            t8, r_t8 = t8_r.next()
            op1("dve", "max", [r_lg], [r_t8], t8[:], lg[:])
            if SUB == 27:
                return
            mk, r_mk = mk_r.next()
            op1("dve", "tensor_scalar", [r_lg, r_t8], [r_mk], mk[:], lg[:], t8[:, 3:4], None, ALU.is_ge)
            mkb, r_mkb = mkb_r.next()
            op1("dve", "tensor_copy", [r_mk], [r_mkb], mkb[:], mk[:])
            ex, r_ex = ex_r.next()
            op1("dve", "tensor_scalar", [r_t8], [r_ex], ex[:, 0:4], t8[:, 0:4], t8[:, 0:1], None, ALU.subtract)
            op1("act", "activation", [r_ex], [r_ex], ex[:, 4:8], ex[:, 0:4], AF.Exp)
            op1("dve", "tensor_reduce", [r_ex], [r_ex], ex[:, 8:9], ex[:, 4:8], AX.X, ALU.add)
            op1("dve", "reciprocal", [r_ex], [r_ex], ex[:, 9:10], ex[:, 8:9])
            op1("dve", "tensor_scalar", [r_ex], [r_route], gk_all[:, i * 4:(i + 1) * 4], ex[:, 4:8], ex[:, 9:10], None,
                ALU.mult)
            oh, r_oh = oh_r.next()
            for k in range(4):
                op1("dve", "tensor_scalar", [r_lg, r_t8], [r_oh], oh[:, k, :], lg[:], t8[:, k:k + 1], None, ALU.is_equal)
            pc2, r_pc2 = ps_next()
            pe_mm([(pc2[:, 0:32], Utri, mkb[:], True, True), (pc2[:, 32:64], ones_bf, mkb[:], True, True)],
                  [r_mkb, r_const], [r_pc2])
            Dm, r_Dm = Dm_r.next()
            op1("dve", "tensor_tensor", [r_pc2, r_run], [r_Dm], Dm[:], pc2[:, 0:32], runc[:], ALU.add)
            op1("dve", "tensor_tensor", [r_pc2, r_run], [r_run], runc[:], runc[:], pc2[:, 32:64], ALU.add)
            tq, r_tq = tq_r.next()
            op1("dve", "tensor_tensor", [r_oh, r_Dm], [r_tq], tq[:], oh[:], Dm[:].unsqueeze(1).to_broadcast([128, 4, 32]),
                ALU.mult)
            op1("dve", "tensor_reduce", [r_tq], [r_route], rank_all[:, i * 4:(i + 1) * 4], tq[:], AX.X, ALU.add)
            op1("dve", "tensor_tensor", [r_oh, r_const], [r_tq], tq[:], oh[:],
                iota_e.unsqueeze(1).to_broadcast([128, 4, 32]), ALU.mult)
            op1("dve", "tensor_reduce", [r_tq], [r_route], ek_all[:, i * 4:(i + 1) * 4], tq[:], AX.X, ALU.add)

        def all_p1_regs():
            rr_ = [r_junk, r_rA, r_rB, r_kc, r_vc, r_w, r_brow, r_lngb, r_AB, r_route, r_run, r_xmd, r_h2d]
            for ring in (xt_r, rp_r, xs_r, hT_r, qk_r, qT_r, u_r, gv_r, z_r, vln_r, sgo_r, p_r, ao_r, on_r, oT_r, tm_r,
                         xm_r, h2f_r, h2b_r, h2T_r, st_r, bn_r, lg_r, t8_r, mk_r, mkb_r, ex_r, oh_r, Dm_r, tq_r, ptr):
                rr_ += [r for (_, r) in ring.items]
            rr_ += [r for (_, r) in kT2r] + [r for (_, r) in vaugr] + [r for (_, _, r) in pslots]
            return rr_

        def finish_dbg():
            o = S.op("sp", None, reads=all_p1_regs())
            o.deps.update(z.idx for z in zero_ops)
            S.run()
            st1.close()
            st_mix.close()
            return nc

        stageA(0)
        if stop_after == 10:
            return finish_dbg()
        for i in range(1, NT):
            stageA(i)
            if stop_after == 11:
                return finish_dbg()
            stageB(i - 1)
            if stop_after == 12:
                return finish_dbg()
        stageB(NT - 1)

        allr1 = [r_junk, r_rA, r_rB, r_kc, r_vc, r_w, r_brow, r_lngb, r_AB]
        for ring in (xt_r, rp_r, xs_r, hT_r, qk_r, qT_r, u_r, gv_r, z_r, vln_r, sgo_r, p_r, ao_r, on_r, oT_r, tm_r,
                     xm_r, h2f_r, h2b_r, h2T_r, st_r, bn_r, lg_r, t8_r, mk_r, mkb_r, ex_r, oh_r, Dm_r, tq_r, ptr):
            allr1 += [r for (_, r) in ring.items]
        allr1 += [r for (_, r) in kT2r] + [r for (_, r) in vaugr] + [r for (_, _, r) in pslots]
        for e in Sched.ENGS:
            S.op(e, None, reads=allr1)
        if stop_after == 1:
            d_dbg = S.dsem("dbg")
            rr = [Reg("dbgo") for _ in range(4)]
            n4 = NT * 4
            dma("sp", dbg_route[:, 0:n4], rank_all[:], [r_route], [rr[0]], d_dbg)
            dma("sp", dbg_route[:, n4:2 * n4], ek_all[:], [r_route], [rr[1]], d_dbg)
            dma("sp", dbg_route[:, 2 * n4:3 * n4], gk_all[:], [r_route], [rr[2]], d_dbg)
            dma("sp", dbg_route[:, 3 * n4:3 * n4 + 32], runc[:], [r_run], [rr[3]], d_dbg)
            d_dbg.seal()
            o = S.op("sp", None, reads=rr + [r_xmd, r_h2d])
            o.deps.update(z.idx for z in zero_ops)
            S.run()
            st1.close()
            st_mix.close()
            return nc
        st1.close()
        st_mix.close()

        st2 = ExitStack()
        ci_ = sbuf(st2, "ci_", [128, 32], I32)
        pf = sbuf(st2, "pf", [128, 32], F32)
        cs0 = sbuf(st2, "cs0", [128, 32], F32)
        cs1 = sbuf(st2, "cs1", [128, 32], F32)
        pst = sbuf(st2, "pst", [128, 32], F32)
        cmp8 = sbuf(st2, "cmp8", [128, 32, 8], F32)
        cmpb = sbuf(st2, "cmpb", [128, NB, 32], F32)
        ebf = sbuf(st2, "ebf", [128, NB], F32)
        ohb = sbuf(st2, "ohb", [128, NT * 4, 32], F32)
        psel = sbuf(st2, "psel", [128, NT * 4], F32)
        r_b = Reg("book")
        LOG2R = R.bit_length() - 1
        op1("dve", "tensor_tensor", [r_run, r_const], [r_b], cmp8[:], runc[:].unsqueeze(2).to_broadcast([128, 32, 8]),
            thr8.unsqueeze(1).to_broadcast([128, 32, 8]), ALU.is_gt)
        op1("dve", "tensor_reduce", [r_b], [r_b], pf[:], cmp8[:], AX.X, ALU.add)
        op1("dve", "tensor_scalar", [r_b], [r_b], pf[:], pf[:], float(R), None, ALU.mult)
        op1("dve", "tensor_copy", [r_b], [r_b], cs0[:], pf[:])
        cur, nxt = cs0, cs1
        for s in (1, 2, 4, 8, 16):
            op1("dve", "tensor_copy", [r_b], [r_b], nxt[:, 0:s], cur[:, 0:s])
            op1("dve", "tensor_tensor", [r_b], [r_b], nxt[:, s:32], cur[:, s:32], cur[:, 0:32 - s], ALU.add)
            cur, nxt = nxt, cur
        pend = cur
        op1("dve", "tensor_tensor", [r_b], [r_b], pst[:], pend[:], pf[:], ALU.subtract)
        op1("dve", "tensor_tensor", [r_b, r_const], [r_b], cmpb[:], pend[:].unsqueeze(1).to_broadcast([128, NB, 32]),
            bstart.unsqueeze(2).to_broadcast([128, NB, 32]), ALU.is_le)
        op1("dve", "tensor_reduce", [r_b], [r_b], ebf[:], cmpb[:], AX.X, ALU.add)
        op1("dve", "tensor_scalar", [r_b], [r_b], ebf[:], ebf[:], 31.0, None, ALU.min)
        op1("dve", "tensor_scalar", [r_b, r_const], [r_b], ebf[:], ebf[:], 128.0, pidx, ALU.mult, ALU.add)
        op1("dve", "tensor_copy", [r_b], [r_eblk], widx[:], ebf[:])
        op1("dve", "tensor_tensor", [r_route, r_const], [r_b], ohb[:],
            ek_all[:].unsqueeze(2).to_broadcast([128, NT * 4, 32]),
            iota_e.unsqueeze(1).to_broadcast([128, NT * 4, 32]), ALU.is_equal)
        op1("dve", "tensor_tensor", [r_b], [r_b], ohb[:], ohb[:], pst[:].unsqueeze(1).to_broadcast([128, NT * 4, 32]),
            ALU.mult)
        op1("dve", "tensor_reduce", [r_b], [r_b], psel[:], ohb[:], AX.X, ALU.add)
        op1("dve", "tensor_tensor", [r_b, r_route], [r_b], psel[:], psel[:], rank_all[:], ALU.add)
        op1("dve", "tensor_copy", [r_b], [r_dest], dest_i[:], psel[:])

        h2l_r = mkring(st2, "h2l", [128, 1024], BF16, 3)
        d_h2l = [S.dsem("h2l%d" % k) for k in range(3)]
        d_sc = [S.dsem("scatter%d" % k) for k in range(3)]
        for i in range(NT):
            hl, r_hl = h2l_r.next()
            dma("sp", hl[:], h2_d[i * 128:(i + 1) * 128, :], [r_h2d], [r_hl], d_h2l[i % 3])
            for k in range(4):
                c = i * 4 + k
                o = S.op("pool", (lambda e, hl=hl, c=c: e.indirect_dma_start(
                    out=xs_d, out_offset=bass.IndirectOffsetOnAxis(ap=dest_i[:, c:c + 1], axis=0),
                    in_=hl[:], in_offset=None)),
                    [r_hl, r_dest], [Reg("xs_sc")], dsem=d_sc[i % 3])
                o.deps.update(z.idx for z in zero_ops)
        scatter_ops = []
        for d in d_sc:
            scatter_ops += d.ops
        if stop_after == 2:
            d_dbg = S.dsem("dbg")
            rr = [Reg("dbgo") for _ in range(2)]
            dma("sp", dbg_book[:, 0:NT * 4], dest_i[:], [r_dest], [rr[0]], d_dbg)
            dma("sp", dbg_book[:, NT * 4:NT * 4 + NB], widx[:], [r_eblk], [rr[1]], d_dbg)
            d_dbg.seal()
            o = S.op("sp", None, reads=rr)
            o.deps.update(z.idx for z in scatter_ops)
            S.run()
            st2.close()
            return nc
        for e in Sched.ENGS:
            S.op(e, None, reads=[r_b] + [r for (_, r) in h2l_r.items])
        st2.close()

        st3 = ExitStack()
        wgu_r = mkring(st3, "wgu", [128, 8, 2048], BF16, 2)
        wd_r = mkring(st3, "wd", [128, 8, 1024], BF16, 2)
        bg_r = mkring(st3, "bg", [128, 16], F32, 2)
        d_wgu = [S.dsem("wgu0"), S.dsem("wgu1")]
        d_wd = [S.dsem("wd0"), S.dsem("wd1")]
        d_bg = [S.dsem("bg0"), S.dsem("bg1")]
        xb_r = mkring(st3, "xb", [128, RG, 1024], BF16, 2)
        d_xb = [S.dsem("xb0"), S.dsem("xb1")]
        xbT_r = mkring(st3, "xbT", [128, 8, R], BF16, 2)
        aT_r = mkring(st3, "aT", [128, 8, R], BF16, 2)
        gc_r = mkring(st3, "gc", [128, R], F32, 2)
        uc_r = mkring(st3, "uc", [128, R], F32, 2)
        sg_r = mkring(st3, "sg", [128, R], F32, 2)
        yo_r = mkring(st3, "yo", [128, 1024], F32, 3)
        d_yo = [S.dsem("yo%d" % k) for k in range(3)]
        r_ys = Reg("ys_d")
        wg_regs = [[Reg("wgk") for _ in range(8)] for _ in range(2)]
        wd_regs = [[Reg("wdk") for _ in range(8)] for _ in range(2)]

        for b in range(NB):
            kb = b % 2
            wg, r_wg = wgu_r.next()
            wdn, r_wdn = wd_r.next()
            bg, r_bg = bg_r.next()
            r_wgk = wg_regs[kb]
            r_wdk = wd_regs[kb]

            for k in range(8):
                S.op("pool", (lambda e, b=b, wg=wg, k=k: e.indirect_dma_start(
                    out=wg[:, k, :], out_offset=None, in_=wgu[k],
                    in_offset=bass.IndirectOffsetOnAxis(ap=widx[:, b:b + 1], axis=0))), [r_eblk], [r_wgk[k]], dsem=d_wgu[kb])
            for k in range(8):
                S.op("pool", (lambda e, b=b, wdn=wdn, k=k: e.indirect_dma_start(
                    out=wdn[:, k, :], out_offset=None, in_=wd[k],
                    in_offset=bass.IndirectOffsetOnAxis(ap=widx[:, b:b + 1], axis=0))), [r_eblk], [r_wdk[k]], dsem=d_wd[kb])
            S.op("pool", (lambda e, b=b, bg=bg: e.indirect_dma_start(
                out=bg[:], out_offset=None, in_=bgu,
                in_offset=bass.IndirectOffsetOnAxis(ap=widx[:, b:b + 1], axis=0))), [r_eblk], [r_bg], dsem=d_bg[kb])
            xb, r_xb = xb_r.next()
            o = S.op("sp", (lambda e, xb=xb, b=b: e.dma_start(
                out=xb[:], in_=xs_d[b * R:(b + 1) * R, :].rearrange("(g p) n -> p g n", p=128))),
                [], [r_xb], dsem=d_xb[kb])
            o.deps.update(z.idx for z in scatter_ops)
            xbT, r_xbT = xbT_r.next()
            for rg in range(RG):
                pt, r_pt = ptr.next()
                pe_tr([(pt[:, j, :], xb[:, rg, j * 128:(j + 1) * 128]) for j in range(8)], [r_xb, r_const], [r_pt])
                op1("act", "activation", [r_pt], [r_xbT], xbT[:, :, rg * 128:(rg + 1) * 128], pt[:], AF.Copy)
            aT, r_aT = aT_r.next()
            for j in range(8):
                pg, r_pg = ps_next()
                pu, r_pu = ps_next()
                pe_mm([(pg[:, 0:R], wg[:, k, j * 128:(j + 1) * 128], xbT[:, k, :], k == 0, k == 7) for k in range(8)],
                      r_wgk + [r_xbT], [r_pg])
                pe_mm([(pu[:, 0:R], wg[:, k, 1024 + j * 128:1024 + (j + 1) * 128], xbT[:, k, :], k == 0, k == 7)
                       for k in range(8)], r_wgk + [r_xbT], [r_pu])
                gc, r_gc = gc_r.next()
                uc, r_uc = uc_r.next()
                sg, r_sg = sg_r.next()
                op1("dve", "tensor_scalar", [r_pg, r_bg], [r_gc], gc[:], pg[:, 0:R], bg[:, j:j + 1], 7.0, ALU.add, ALU.min)
                op1("dve", "tensor_scalar", [r_pu, r_bg], [r_uc], uc[:], pu[:, 0:R], bg[:, 8 + j:9 + j], 7.0, ALU.add,
                    ALU.min)
                op1("act", "activation", [r_gc], [r_sg], sg[:], gc[:], AF.Sigmoid, scale=1.702)
                op1("pool", "tensor_scalar", [r_uc], [r_uc], uc[:], uc[:], -7.0, 1.0, ALU.max, ALU.add)
                op1("pool", "tensor_tensor", [r_gc, r_sg], [r_sg], sg[:], gc[:], sg[:], ALU.mult)
                op1("dve", "tensor_tensor", [r_uc, r_sg], [r_aT], aT[:, j, :], uc[:], sg[:], ALU.mult)
            for rg in range(RG):
                yo, r_yo = yo_r.next()
                ky = (yo_r.i - 1) % 3
                for hh in range(2):
                    py, r_py = ps_next()
                    pe_mm([(py, aT[:, k, rg * 128:(rg + 1) * 128], wdn[:, k, hh * 512:(hh + 1) * 512], k == 0, k == 7)
                           for k in range(8)], [r_aT] + r_wdk, [r_py])
                    op1("act", "activation", [r_py], [r_yo], yo[:, hh * 512:(hh + 1) * 512], py, AF.Copy)
                dma("sp", ys_d[b * R + rg * 128:b * R + (rg + 1) * 128, :], yo[:], [r_yo], [Reg("ys_w")], d_yo[ky])
        ys_ops = []
        for d in d_yo:
            ys_ops += d.ops
        allr3 = []
        for ring in (wgu_r, wd_r, bg_r, xb_r, xbT_r, aT_r, gc_r, uc_r, sg_r, yo_r, ptr):
            allr3 += [r for (_, r) in ring.items]
        allr3 += [r for (_, _, r) in pslots]
        for kk in range(2):
            allr3 += wg_regs[kk] + wd_regs[kk]
        for e in Sched.ENGS:
            o = S.op(e, None, reads=allr3)
            o.deps.update(z.idx for z in ys_ops)
        st3.close()

        st4 = ExitStack()
        yg_r = mkring(st4, "yg", [128, 4, 1024], F32, 2)
        d_yg = [S.dsem("yg0"), S.dsem("yg1")]
        xl_r = mkring(st4, "xl", [128, 1024], F32, 2)
        d_xl = [S.dsem("xl0"), S.dsem("xl1")]
        acc_r = mkring(st4, "acc", [128, 1024], F32, 2)
        ob_r = mkring(st4, "ob", [128, 1024], F32, 2)
        G_r = mkring(st4, "G", [128, 32], F32, 2)
        Gb_r = mkring(st4, "Gb", [128, 32], BF16, 2)
        GT_r = mkring(st4, "GT", [32, 128], BF16, 2)
        toh_r = mkring(st4, "toh", [128, 32], F32, 2)
        st4_r = mkring(st4, "st4", [128, 4], F32, 2)
        junk4 = sbuf(st4, "junk4", [128, 1024], BF16)
        r_junk4 = Reg("junk4")
        out_regs = []
        yg_regs = [[Reg("ygk") for _ in range(4)] for _ in range(2)]
        for i in range(NT):
            yg, r_yg0 = yg_r.next()
            ky = i % 2
            r_ygk = yg_regs[ky]
            r_yg = r_ygk[0]
            for k in range(4):
                c = i * 4 + k
                S.op("pool", (lambda e, yg=yg, k=k, c=c: e.indirect_dma_start(
                    out=yg[:, k, :], out_offset=None, in_=ys_d,
                    in_offset=bass.IndirectOffsetOnAxis(ap=dest_i[:, c:c + 1], axis=0))), [r_dest, r_ys], [r_ygk[k]],
                    dsem=d_yg[ky])
            xl, r_xl = xl_r.next()
            dma("sp", xl[:], xmid_d[i * 128:(i + 1) * 128, :], [r_xmd], [r_xl], d_xl[ky])
            G, r_G = G_r.next()
            toh, r_toh = toh_r.next()
            for k in range(4):
                c = i * 4 + k
                op1("dve", "tensor_scalar", [r_route, r_const], [r_toh], toh[:], iota_e, ek_all[:, c:c + 1],
                    None, ALU.is_equal)
                op1("dve", "tensor_scalar", [r_route, r_toh], [r_toh], toh[:], toh[:], gk_all[:, c:c + 1],
                    None, ALU.mult)
                if k == 0:
                    op1("dve", "tensor_copy", [r_toh], [r_G], G[:], toh[:])
                else:
                    op1("dve", "tensor_tensor", [r_toh, r_G], [r_G], G[:], G[:], toh[:], ALU.add)
            Gb, r_Gb = Gb_r.next()
            op1("dve", "tensor_copy", [r_G], [r_Gb], Gb[:], G[:])
            pt, r_pt = ptr.next()
            pe_tr([(pt[0:32, 0, :], Gb[:])], [r_Gb, r_const], [r_pt])
            GT, r_GT = GT_r.next()
            op1("act", "activation", [r_pt], [r_GT], GT[:], pt[0:32, 0, :], AF.Copy)
            pbd, r_pbd = ps_pair()
            pe_mm([(pbd[:, hh * 512:(hh + 1) * 512], GT[:], bdown_bf[:, hh * 512:(hh + 1) * 512], True, True)
                   for hh in range(2)], [r_GT, r_bdown], r_pbd)
            acc, r_acc = acc_r.next()
            c0 = i * 4
            op1("dve", "scalar_tensor_tensor", [r_ygk[0], r_route] + r_pbd, [r_acc], acc[:], yg[:, 0, :],
                gk_all[:, c0:c0 + 1], pbd[:], ALU.mult, ALU.add)
            for k in (1, 2, 3):
                eng = "dve"
                op1(eng, "scalar_tensor_tensor", [r_ygk[k], r_route, r_acc], [r_acc], acc[:], yg[:, k, :],
                    gk_all[:, c0 + k:c0 + k + 1], acc[:], ALU.mult, ALU.add)
            s4, r_s4 = st4_r.next()
            op1("act", "activation", [r_acc], [r_junk4, r_s4], junk4[:], acc[:], AF.Square, accum_out=s4[:, 0:1])
            rstd_from(s4[:, 0:1], s4[:, 1:2], 1.0 / 1024, r_s4, r_s4)
            ob, r_ob = ob_r.next()
            op1("dve", "scalar_tensor_tensor", [r_acc, r_s4, r_MR], [r_ob], ob[:], acc[:], s4[:, 1:2], MR[:, 3, :],
                ALU.mult, ALU.mult)
            op1("pool", "tensor_tensor", [r_ob, r_xl], [r_ob], ob[:], ob[:], xl[:], ALU.add)
            ro = Reg("out")
            dma("sp", out[i * 128:(i + 1) * 128, :], ob[:], [r_ob], [ro], d_out[i % 2])
            out_regs.append(ro)
        S.op("sp", None, reads=out_regs)
        S.run()
        st4.close()
    return nc


def _host_consts():
    ident = np.eye(128, dtype=np.float32)
    U = np.triu(np.ones((128, 128), np.float32), k=1)
    ones = np.ones((128, 128), np.float32)
    jj = np.arange(128)[:, None]
    ii = np.arange(128)[None, :]
    mprev = (jj >= ii).astype(np.float32)
    mnext = (jj <= ii).astype(np.float32)
    cbf = np.concatenate([ident, U, ones, mprev, mnext], axis=1)
    cf32 = np.concatenate([np.tile(np.arange(32, dtype=np.float32), (128, 1)),
                           np.tile(np.arange(NB, dtype=np.float32) * R, (128, 1)),
                           np.arange(128, dtype=np.float32)[:, None],
                           np.tile(np.arange(8, dtype=np.float32) * R, (128, 1))], axis=1)
    t = np.arange(4096)
    pos_row = (t // 64).astype(np.float32)
    pos_col = (t % 64).astype(np.float32)
    inv_freq = (np.float32(10000.0) ** (-np.arange(16, dtype=np.float32) / np.float32(16))).astype(np.float32)
    ar = pos_row[:, None] * inv_freq
    ac = pos_col[:, None] * inv_freq
    cr, sr, cc_, sc_ = np.cos(ar), np.sin(ar), np.cos(ac), np.sin(ac)
    rope = np.concatenate([cr, cr, cc_, cc_, -sr, sr, -sc_, sc_], axis=1).astype(np.float32)
    return np.ascontiguousarray(cbf), np.ascontiguousarray(cf32), np.ascontiguousarray(rope)


_CACHE = {}


def _prep(x, c, ctx, c_ctx, w_ada, b_ada, g_pre_mix, g_post_mix, g_pre_ffn, g_post_ffn,
          w_in, b_in, attn_sink, sgu_ln_g, sgu_ln_b, sgu_w, sgu_b, g_attn_out, g_sgu_out,
          w_out, b_out, w_router, b_router, w_gate_up, b_gate_up, w_down, b_down):
    f = lambda a: np.ascontiguousarray(np.asarray(a, dtype=np.float32))
    x, c, ctx, c_ctx = f(x), f(c), f(ctx), f(c_ctx)
    cbf, cf32, rope = _host_consts()
    perm = np.concatenate([np.arange(0, 512), np.arange(768, 1280), np.arange(1280, 1792), np.arange(512, 768)])
    w_in_p = f(f(w_in)[0][:, perm])
    b_in_p = f(b_in)[0][perm]
    col = lambda v: f(v).reshape(8, 128).T
    colpack = f(np.concatenate([col(f(g_pre_mix)[0]),
                                col(np.concatenate([f(g_attn_out)[0], f(g_sgu_out)[0]]))], axis=1))
    rowpack = f(np.concatenate([f(g_post_mix)[0], f(g_pre_ffn)[0], f(g_post_ffn)[0], f(sgu_ln_g)[0],
                                f(sgu_ln_b)[0], f(attn_sink)[0]])[None, :])
    browf = f(np.concatenate([b_in_p, f(b_out)[0], f(sgu_b)[0].reshape(-1), f(b_router)[0]])[None, :])
    sguwT = f(np.transpose(f(sgu_w)[0], (2, 0, 1)).reshape(128, 1024))
    wgu3 = f(w_gate_up)[0]
    wd3 = f(w_down)[0]
    bgu = f(f(b_gate_up)[0].reshape(32, 2, 8, 128).transpose(0, 3, 1, 2).reshape(32 * 128, 16))
    shared = {
        "w_ada": f(w_ada)[0], "b_ada": f(b_ada), "colpack": colpack, "rowpack": rowpack, "browf": browf,
        "w_in": w_in_p, "w_out": f(w_out)[0], "w_r": f(w_router)[0], "sguwT": sguwT, "b_down": f(b_down)[0],
        "bgu": bgu, "cbf": cbf, "cf32": cf32, "rope": rope,
    }
    for k in range(8):
        shared["wgu%d" % k] = f(wgu3[:, k * 128:(k + 1) * 128, :].reshape(32 * 128, 2048))
        shared["wd%d" % k] = f(wd3[:, k * 128:(k + 1) * 128, :].reshape(32 * 128, 1024))
    in_maps = []
    for b in range(8):
        m = dict(shared)
        m["x"] = x[b]
        m["ctx"] = ctx[b]
        m["cc"] = f(np.stack([c[b].reshape(8, 128).T, c_ctx.reshape(8, 128).T], axis=2).reshape(128, 16))
        in_maps.append(m)
    return in_maps


def kernel(**inputs):
    if "nc" not in _CACHE:
        _CACHE["nc"] = build_program()
    nc = _CACHE["nc"]
    in_maps = _prep(**inputs)
    res = run_bass_kernel_spmd(nc, in_maps, core_ids=list(range(8)))
    return np.stack([np.asarray(r["out"], dtype=np.float32) for r in res.results], axis=0)
```

```python
import sys
import numpy as np
from contextlib import ExitStack
import concourse.bass as bass
import concourse.mybir as mybir
from concourse.bass_utils import run_bass_kernel_spmd

F32 = mybir.dt.float32
BF16 = mybir.dt.bfloat16
I32 = mybir.dt.int32
ALU = mybir.AluOpType
AF = mybir.ActivationFunctionType
AX = mybir.AxisListType

NT = 32
R = 512
NB = 64
RG = R // 128
EPS = 1e-6
SUB = 0


class Reg:
    __slots__ = ("name", "w", "rs")

    def __init__(self, name=""):
        self.name = name
        self.w = None
        self.rs = []


class DSem:
    def __init__(self, sem):
        self.sem = sem
        self.count = 0
        self.ops = []

    def seal(self):
        for o in self.ops:
            o.dcount = self.count


class Op:
    __slots__ = ("eng", "fn", "deps", "sig", "sigcount", "dsem", "dcount", "idx", "tag")


class Sched:
    ENGS = ("pe", "act", "dve", "pool", "sp")

    def __init__(self, nc, stack):
        self.nc = nc
        self.stack = stack
        self.ops = []
        self.esem = {e: stack.enter_context(nc.semaphore("es_" + e)) for e in ("pe", "act", "dve", "pool")}

    def dsem(self, name):
        return DSem(self.stack.enter_context(self.nc.semaphore("ds_" + name)))

    def op(self, eng, fn, reads=(), writes=(), dsem=None):
        o = Op()
        o.eng = eng
        o.fn = fn
        o.dsem = dsem
        o.sig = False
        o.sigcount = 0
        o.dcount = 0
        o.idx = len(self.ops)
        fr = sys._getframe(1)
        tags = []
        while fr is not None and len(tags) < 4:
            if fr.f_code.co_filename == __file__:
                tags.append(str(fr.f_lineno))
            fr = fr.f_back
        o.tag = "L" + "<".join(tags)
        deps = set()
        for r in reads:
            if r.w is not None:
                deps.add(r.w)
        for r in writes:
            if r.w is not None:
                deps.add(r.w)
            deps.update(r.rs)
        deps.discard(o.idx)
        if fn is not None:
            for r in reads:
                r.rs.append(o.idx)
            for r in writes:
                r.w = o.idx
                r.rs = []
        o.deps = deps
        if dsem is not None:
            dsem.count += 16
            o.dcount = dsem.count
            dsem.ops.append(o)
        self.ops.append(o)
        return o

    def finalize(self):
        ops = self.ops
        for o in ops:
            for d in o.deps:
                t = ops[d]
                if t.dsem is not None:
                    continue
                if t.eng == o.eng and o.eng in ("pe", "sp"):
                    continue
                t.sig = True
        cnt = {e: 0 for e in self.ENGS}
        for o in ops:
            if o.dsem is None and o.sig:
                cnt[o.eng] += 1
                o.sigcount = cnt[o.eng]

    def emit(self, engname, engobj):
        ops = self.ops
        waited = {}
        for o in ops:
            if o.eng != engname:
                continue
            need = {}
            for d in o.deps:
                t = ops[d]
                if t.dsem is not None:
                    key = ("d", id(t.dsem))
                    sem = t.dsem.sem
                    val = t.dcount
                else:
                    if t.eng == o.eng and o.eng in ("pe", "sp"):
                        continue
                    key = ("e", t.eng)
                    sem = self.esem[t.eng]
                    val = t.sigcount
                if key not in need or need[key][1] < val:
                    need[key] = (sem, val)
            for key, (sem, val) in need.items():
                if waited.get(key, 0) < val:
                    engobj.wait_ge(sem, val)
                    waited[key] = val
            if o.fn is None:
                continue
            ins = o.fn(engobj)
            try:
                ins.annotate(o.tag)
            except Exception:
                pass
            if o.dsem is not None:
                ins.then_inc(o.dsem.sem, 16)
            elif o.sig:
                ins.then_inc(self.esem[o.eng], 1)

    def run(self):
        self.finalize()
        nc = self.nc
        with nc.Block() as block:
            @block.tensor
            def _(e):
                self.emit("pe", e)

            @block.scalar
            def _(e):
                self.emit("act", e)

            @block.vector
            def _(e):
                self.emit("dve", e)

            @block.gpsimd
            def _(e):
                self.emit("pool", e)

            @block.sync
            def _(e):
                self.emit("sp", e)


class Ring:
    def __init__(self, items):
        self.items = items
        self.i = 0

    def next(self):
        it = self.items[self.i % len(self.items)]
        self.i += 1
        return it


def build_program(stop_after=None):
    nc = bass.Bass("TRN2", target_bir_lowering=False)

    def din(name, shape, dt=F32):
        return nc.dram_tensor(name, shape, dt, kind="ExternalInput").ap()

    x = din("x", [4096, 1024])
    ctx = din("ctx", [256, 1024])
    cc = din("cc", [128, 16])
    w_ada = din("w_ada", [1024, 6144])
    b_ada = din("b_ada", [1, 6144])
    colpack = din("colpack", [128, 16])
    rowpack = din("rowpack", [1, 4104])
    browf = din("browf", [1, 3872])
    w_in = din("w_in", [1024, 1792])
    w_out = din("w_out", [1024, 1024])
    w_r = din("w_r", [1024, 32])
    sguwT = din("sguwT", [128, 1024])
    b_down = din("b_down", [32, 1024])
    if stop_after is None:
        wgu = [din("wgu%d" % k, [4096, 2048]) for k in range(8)]
        wd = [din("wd%d" % k, [4096, 1024]) for k in range(8)]
        bgu = din("bgu", [4096, 16])
    cbf_d = din("cbf", [128, 640])
    cf32_d = din("cf32", [128, 32 + NB + 9])
    rope_d = din("rope", [4096, 128])
    out = nc.dram_tensor("out", [4096, 1024], F32, kind="ExternalOutput").ap()
    ik = "Internal" if stop_after is None else "ExternalOutput"
    xs_d = nc.dram_tensor("xs_d", [NB * R, 1024], BF16, kind=ik).ap()
    ys_d = nc.dram_tensor("ys_d", [NB * R, 1024], F32, kind=ik).ap()
    xmid_d = nc.dram_tensor("xmid_d", [4096, 1024], F32, kind=ik).ap()
    h2_d = nc.dram_tensor("h2_d", [4096, 1024], BF16, kind=ik).ap()
    if stop_after is not None:
        dbg_route = nc.dram_tensor("dbg_route", [128, 3 * NT * 4 + 32], F32, kind="ExternalOutput").ap()
        dbg_book = nc.dram_tensor("dbg_book", [128, NT * 4 + NB], I32, kind="ExternalOutput").ap()

    with ExitStack() as st_all:
        S = Sched(nc, st_all)

        def sbuf(st, name, shape, dt):
            return st.enter_context(nc.sbuf_tensor("s_" + name, shape, dt))

        def mkring(st, name, shape, dt, n):
            return Ring([(sbuf(st, "%s%d" % (name, k), shape, dt), Reg(name)) for k in range(n)])

        def op1(eng, meth, reads, writes, *a, **k):
            S.op(eng, (lambda e: getattr(e, meth)(*a, **k)), reads, writes)

        def dma(eng, out_ap, in_ap, reads, writes, ds, **k):
            S.op(eng, (lambda e: e.dma_start(out=out_ap, in_=in_ap, **k)), reads, writes, dsem=ds)

        def pe_mm(mms, reads, writes):
            mms = list(mms)

            def fn(e):
                ins = None
                for (o, l, r, a, b) in mms:
                    ins = e.matmul(o, l, r, start=a, stop=b)
                return ins
            S.op("pe", fn, reads, writes)

        def pe_tr(trs, reads, writes):
            trs = list(trs)

            def fn(e):
                ins = None
                for (o, i_) in trs:
                    ins = e.transpose(o, i_, ident)
                return ins
            S.op("pe", fn, reads, writes)

        pbig = [st_all.enter_context(nc.psum_tensor("pb%d" % k, [128, 1024], F32)) for k in range(3)]
        pslots = []
        for k in range(3):
            for h in range(2):
                pslots.append((pbig[k], h, Reg("ps%d%d" % (k, h))))
        pstate = {"i": 0}

        def ps_next():
            t, h, r = pslots[pstate["i"] % 6]
            pstate["i"] += 1
            return t[:, h * 512:(h + 1) * 512], r

        def ps_pair():
            if pstate["i"] % 2 == 1:
                pstate["i"] += 1
            t, _, r0 = pslots[pstate["i"] % 6]
            _, _, r1 = pslots[(pstate["i"] + 1) % 6]
            pstate["i"] += 2
            return t, [r0, r1]

        ptr = Ring([(st_all.enter_context(nc.psum_tensor("pt%d" % k, [128, 8, 128], BF16)), Reg("pt")) for k in range(2)])

        cbf = sbuf(st_all, "cbf", [128, 640], BF16)
        ident = cbf[:, 0:128]
        Utri = cbf[:, 128:256]
        ones_bf = cbf[:, 256:384]
        mprev = cbf[:, 384:512]
        mnext = cbf[:, 512:640]
        cf32 = sbuf(st_all, "cf32", [128, 32 + NB + 9], F32)
        iota_e = cf32[:, 0:32]
        bstart = cf32[:, 32:32 + NB]
        pidx = cf32[:, 32 + NB:33 + NB]
        thr8 = cf32[:, 33 + NB:41 + NB]
        MR = sbuf(st_all, "MR", [128, 4, 1024], F32)
        bdown_bf = sbuf(st_all, "bdown_bf", [32, 1024], BF16)
        rank_all = sbuf(st_all, "rank_all", [128, NT * 4], F32)
        ek_all = sbuf(st_all, "ek_all", [128, NT * 4], F32)
        gk_all = sbuf(st_all, "gk_all", [128, NT * 4], F32)
        dest_i = sbuf(st_all, "dest_i", [128, NT * 4], I32)
        runc = sbuf(st_all, "runc", [128, 32], F32)
        eps_t = sbuf(st_all, "eps_t", [128, 1], F32)
        esink = sbuf(st_all, "esink", [128, 8], F32)
        A1 = sbuf(st_all, "A1", [128, 8, 2], F32)
        B1 = sbuf(st_all, "B1", [128, 8, 2], F32)
        colp = sbuf(st_all, "colp", [128, 16], F32)
        ones_f = sbuf(st_all, "ones_f", [1, 128], F32)
        widx = sbuf(st_all, "widx", [128, NB], I32)
        r_const = Reg("const")
        r_MR = Reg("MR")
        r_AB = Reg("AB")
        r_route = Reg("route")
        r_run = Reg("run")
        r_bdown = Reg("bdown")
        r_dest = Reg("dest")
        r_eblk = Reg("eblk")

        d_const = S.dsem("const")
        d_out = [S.dsem("out0"), S.dsem("out1")]

        st_mix = ExitStack()
        w_in_bf = sbuf(st_mix, "w_in_bf", [128, 8, 1792], BF16)
        w_out_bf = sbuf(st_mix, "w_out_bf", [128, 8, 1024], BF16)
        wr_bf = sbuf(st_mix, "wr_bf", [128, 8, 32], BF16)
        sguw_bf = sbuf(st_mix, "sguw_bf", [128, 1024], BF16)
        brow_bf = sbuf(st_mix, "brow_bf", [1, 3872], BF16)
        lngb = sbuf(st_mix, "lngb", [128, 1024], F32)
        kT2r = [(sbuf(st_mix, "kT2_%d" % k, [128, 2, 128], BF16), Reg("kT2")) for k in range(4)]
        vaugr = [(sbuf(st_mix, "vaug_%d" % k, [128, 2, 66], BF16), Reg("vaug")) for k in range(4)]
        kcT2 = sbuf(st_mix, "kcT2", [128, 2, 256], BF16)
        vcaug = sbuf(st_mix, "vcaug", [128, 2, 2, 66], BF16)
        r_kc = Reg("kc")
        r_vc = Reg("vc")
        r_w = Reg("wts")
        r_brow = Reg("brow")
        r_lngb = Reg("lngb")

        st0 = ExitStack()
        cstage = sbuf(st0, "cstage", [128, 640], F32)
        slab = mkring(st0, "slab", [128, 8, 512], F32, 2)
        bada_sb = sbuf(st0, "bada_sb", [1, 6144], F32)
        stg = mkring(st0, "stg", [128, 1792], F32, 2)
        browf_sb = sbuf(st0, "browf_sb", [1, 3872], F32)
        cc_sb = sbuf(st0, "cc_sb", [128, 8, 2], F32)
        sc_f = sbuf(st0, "sc_f", [128, 8, 2], F32)
        rep_f = sbuf(st0, "rep_f", [128, 8, 128], F32)
        modcol = sbuf(st0, "modcol", [128, 16, 2], F32)
        rows_bc = sbuf(st0, "rows_bc", [128, 3, 1024], F32)
        tmp8 = sbuf(st0, "tmp8", [128, 8, 2], F32)
        bdown_f = sbuf(st0, "bdown_f", [32, 1024], F32)
        wr_f = sbuf(st0, "wr_f", [128, 8, 32], F32)
        sguw_f = sbuf(st0, "sguw_f", [128, 1024], F32)
        sink_bc = sbuf(st0, "sink_bc", [128, 8], F32)
        r0 = {k: Reg(k) for k in ("cstage", "bada", "browf", "cc", "sc", "rep", "modcol", "rows", "tmp8",
                                  "bdown_f", "wr_f", "sguw_f", "sink")}

        dma("sp", cstage[:], cbf_d, [], [r0["cstage"]], d_const)
        dma("sp", cf32[:], cf32_d, [], [r_const], d_const)
        dma("sp", cc_sb[:].rearrange("p j s -> p (j s)"), cc, [], [r0["cc"]], d_const)
        dma("sp", bada_sb[:], b_ada, [], [r0["bada"]], d_const)
        dma("sp", browf_sb[:], browf, [], [r0["browf"]], d_const)
        dma("sp", rows_bc[:].rearrange("p a n -> p (a n)"), rowpack[0:1, 0:3072].partition_broadcast(128)[:, 0, :],
            [], [r0["rows"]], d_const)
        dma("sp", lngb[:], rowpack[0:1, 3072:4096].partition_broadcast(128)[:, 0, :], [], [r_lngb], d_const)
        dma("sp", sink_bc[:], rowpack[0:1, 4096:4104].partition_broadcast(128)[:, 0, :], [], [r0["sink"]], d_const)
        dma("sp", bdown_f[:], b_down, [], [r0["bdown_f"]], d_const)
        dma("sp", wr_f[:], w_r.rearrange("(j p) n -> p j n", p=128), [], [r0["wr_f"]], d_const)
        dma("sp", sguw_f[:], sguwT, [], [r0["sguw_f"]], d_const)
        d_const.seal()
        d_const2 = S.dsem("const2")
        dma("sp", colp[:], colpack, [], [r_const], d_const2)

        op1("pool", "memset", [], [r_const], eps_t[:], EPS)
        op1("pool", "memset", [], [r_const], ones_f[:], 1.0)
        op1("pool", "memset", [], [r_run], runc[:], 0.0)
        op1("act", "activation", [r0["cstage"]], [r_const], cbf[:], cstage[:], AF.Copy)
        op1("act", "activation", [r0["sink"]], [r_const], esink[:], sink_bc[:], AF.Exp)
        op1("act", "activation", [r0["cc"]], [r0["sc"]], sc_f[:], cc_sb[:], AF.Silu)
        op1("dve", "tensor_copy", [r0["sc"]], [r0["rep"]], rep_f[:], sc_f[:, :, 0:1].to_broadcast([128, 8, 128]))
        op1("dve", "tensor_copy", [r0["bdown_f"]], [r_bdown], bdown_bf[:], bdown_f[:])
        op1("dve", "tensor_copy", [r0["wr_f"]], [r_w], wr_bf[:], wr_f[:])
        op1("dve", "tensor_copy", [r0["sguw_f"]], [r_w], sguw_bf[:], sguw_f[:])
        op1("dve", "tensor_copy", [r0["browf"]], [r_brow], brow_bf[:], browf_sb[:])

        zt = sbuf(st0, "zt", [128, 4096], BF16)
        r_zt = Reg("zt")
        r_xs = Reg("xs_d")
        d_zero = S.dsem("zero")
        op1("pool", "memset", [], [r_zt], zt[:], 0.0)
        nz = NB * R // 512
        for k in range(nz):
            dma("sp", xs_d[k * 512:(k + 1) * 512, :].rearrange("(p a) n -> p (a n)", p=128), zt[:],
                [r_zt], [Reg("xsz")], d_zero)
        zero_ops = list(d_zero.ops)
        d_zero.seal()

        d_slab = [S.dsem("slab0"), S.dsem("slab1")]
        pcol, r_pcol = ps_next()
        for n in range(12):
            sl, r_sl = slab.next()
            dma("sp", sl[:], w_ada[:, n * 512:(n + 1) * 512].rearrange("(j p) n -> p j n", p=128),
                [], [r_sl], d_slab[n % 2])
            if n < 4:
                mms = []
                for fc in range(4):
                    ci = n * 4 + fc
                    o = pcol[:, ci * 2:ci * 2 + 2]
                    for j in range(8):
                        mms.append((o, sl[:, j, fc * 128:(fc + 1) * 128], sc_f[:, j, :], j == 0, False))
                    mms.append((o, bada_sb[0:1, ci * 128:(ci + 1) * 128], ones_f[0:1, 0:2], False, True))
                pe_mm(mms, [r_sl, r0["sc"], r0["bada"], r_const], [r_pcol])
                if n == 3:
                    op1("dve", "tensor_copy", [r_pcol], [r0["modcol"]],
                        modcol[:].rearrange("p a s -> p (a s)"), pcol[:, 0:32])
            else:
                pr, r_pr = ps_next()
                mms = [(pr, rep_f[:, j, :], sl[:, j, :], j == 0, False) for j in range(8)]
                mms.append((pr, ones_f[0:1, 0:128], bada_sb[0:1, n * 512:(n + 1) * 512], False, True))
                pe_mm(mms, [r_sl, r0["rep"], r0["bada"], r_const], [r_pr])
                q4 = (n - 4) // 2
                h4 = (n - 4) % 2
                op1("act", "activation", [r_pr], [r_MR], MR[:, q4, h4 * 512:(h4 + 1) * 512], pr, AF.Copy)
        op1("dve", "tensor_scalar", [r0["modcol"]], [r0["tmp8"]], tmp8[:], modcol[:, 8:16, :], 1.0, None, ALU.add)
        op1("dve", "tensor_tensor", [r0["tmp8"], r_const], [r_AB], A1[:], tmp8[:],
            colp[:, 0:8].unsqueeze(2).to_broadcast([128, 8, 2]), ALU.mult)
        op1("dve", "tensor_copy", [r0["modcol"]], [r_AB], B1[:], modcol[:, 0:8, :])
        op1("pool", "tensor_tensor", [r_MR, r0["rows"]], [r_MR], MR[:, 0, :], MR[:, 0, :], rows_bc[:, 0, :], ALU.mult)
        op1("dve", "scalar_tensor_tensor", [r_MR, r0["rows"]], [r_MR], MR[:, 2, :], MR[:, 2, :], 1.0,
            rows_bc[:, 1, :], ALU.add, ALU.mult)
        op1("pool", "tensor_tensor", [r_MR, r0["rows"]], [r_MR], MR[:, 3, :], MR[:, 3, :], rows_bc[:, 2, :], ALU.mult)

        d_stg = [S.dsem("stg0"), S.dsem("stg1")]
        for j in range(8):
            sg, r_sg = stg.next()
            dma("sp", sg[:], w_in[j * 128:(j + 1) * 128, :], [], [r_sg], d_stg[j % 2])
            op1("pool", "tensor_copy", [r_sg], [r_w], w_in_bf[:, j, :], sg[:])
        for j in range(8):
            sg, r_sg = stg.next()
            dma("sp", sg[:, 0:1024], w_out[j * 128:(j + 1) * 128, :], [], [r_sg], d_stg[j % 2])
            op1("pool", "tensor_copy", [r_sg], [r_w], w_out_bf[:, j, :], sg[:, 0:1024])

        allr0 = list(r0.values()) + [r_zt] + [r for (_, r) in slab.items] + [r for (_, r) in stg.items]
        for e in Sched.ENGS:
            S.op(e, None, reads=allr0)
        st0.close()

        st1 = ExitStack()
        xt_r = mkring(st1, "xt", [128, 1024], F32, 4)
        d_xt = [S.dsem("xt%d" % k) for k in range(4)]
        rp_r = mkring(st1, "rp", [128, 128], F32, 3)
        d_rp = [S.dsem("rp%d" % k) for k in range(3)]
        junk = sbuf(st1, "junk", [128, 1024], BF16)
        r_junk = Reg("junk")
        xs_r = mkring(st1, "xsb", [128, 1024], BF16, 2)
        hT_r = mkring(st1, "hT", [128, 8, 128], BF16, 2)
        qk_r = mkring(st1, "qk", [128, 768], BF16, 2)
        rA = sbuf(st1, "rA", [128, 512], F32)
        rB = sbuf(st1, "rB", [128, 512], F32)
        r_rA = Reg("rA")
        r_rB = Reg("rB")
        qT_r = mkring(st1, "qT", [128, 4, 128], BF16, 3)
        u_r = mkring(st1, "u", [128, 512], BF16, 2)
        gv_r = mkring(st1, "gv", [128, 512], F32, 2)
        z_r = mkring(st1, "z", [128, 512], F32, 2)
        vln_r = mkring(st1, "vln", [128, 512], BF16, 2)
        sgo_r = mkring(st1, "sgo", [128, 512], F32, 3)
        p_r = mkring(st1, "p", [128, 512], BF16, 7)
        ao_r = mkring(st1, "ao", [128, 512], F32, 2)
        on_r = mkring(st1, "on", [128, 1024], BF16, 2)
        oT_r = mkring(st1, "oT", [128, 8, 128], BF16, 2)
        tm_r = mkring(st1, "tm", [128, 1024], F32, 1)
        xm_r = mkring(st1, "xm", [128, 1024], F32, 2)
        d_xm = [S.dsem("xm0"), S.dsem("xm1")]
        h2f_r = mkring(st1, "h2f", [128, 1024], F32, 1)
        h2b_r = mkring(st1, "h2b", [128, 1024], BF16, 2)
        d_h2 = [S.dsem("h2b0"), S.dsem("h2b1")]
        h2T_r = mkring(st1, "h2T", [128, 8, 128], BF16, 2)
        st_r = mkring(st1, "stat", [128, 16], F32, 6)
        bn_r = mkring(st1, "bn", [128, 8], F32, 2)
        lg_r = mkring(st1, "lg", [128, 32], F32, 2)
        t8_r = mkring(st1, "t8", [128, 8], F32, 2)
        mk_r = mkring(st1, "mk", [128, 32], F32, 2)
        mkb_r = mkring(st1, "mkb", [128, 32], BF16, 2)
        ex_r = mkring(st1, "ex", [128, 32], F32, 2)
        oh_r = mkring(st1, "oh", [128, 4, 32], F32, 2)
        Dm_r = mkring(st1, "Dm", [128, 32], F32, 2)
        tq_r = mkring(st1, "tq", [128, 4, 32], F32, 2)
        r_h2d = Reg("h2_d")
        r_xmd = Reg("xmid_d")

        BIN, BOUT, BSGU, BRT = 0, 1792, 2816, 3840

        def rstd_from(ss_ap, out_ap, scale, r_in, r_out):
            op1("act", "activation", [r_in, r_const], [r_out], out_ap, ss_ap, AF.Sqrt, bias=eps_t[:], scale=scale)
            op1("dve", "reciprocal", [r_out], [r_out], out_ap, out_ap)

        def norm_T(src, r_src, colA, colB, s_idx, hT, r_hT, xsb, r_xsb, stt, r_stt):
            op1("act", "activation", [r_src], [r_junk, r_stt], junk[:], src, AF.Square, accum_out=stt[:, 0:1])
            rstd_from(stt[:, 0:1], stt[:, 1:2], 1.0 / 1024, r_stt, r_stt)
            op1("dve", "tensor_scalar", [r_src, r_stt], [r_xsb], xsb[:], src, stt[:, 1:2], None, ALU.mult)
            pt, r_pt = ptr.next()
            pe_tr([(pt[:, j, :], xsb[:, j * 128:(j + 1) * 128]) for j in range(8)], [r_xsb, r_const], [r_pt])
            for j in range(8):
                S.op("act", (lambda e, j=j: e.activation(hT[:, j, :], pt[:, j, :], AF.Identity,
                                                         bias=colB[:, j, s_idx:s_idx + 1],
                                                         scale=colA[:, j, s_idx:s_idx + 1])),
                     [r_pt, r_AB], [r_hT])

        op1("pool", "memset", [], [r_vc], vcaug[:], 1.0)
        for k in range(4):
            op1("pool", "memset", [], [vaugr[k][1]], vaugr[k][0][:], 1.0)
        for ci in range(2):
            xt, r_xt = xt_r.next()
            dma("sp", xt[:], ctx[ci * 128:(ci + 1) * 128, :], [], [r_xt], d_xt[(xt_r.i - 1) % 4])
            xsb, r_xsb = xs_r.next()
            hT, r_hT = hT_r.next()
            stt, r_stt = st_r.next()
            norm_T(xt[:], r_xt, A1, B1, 1, hT, r_hT, xsb, r_xsb, stt, r_stt)
            pk, r_pk = ps_next()
            mms = [(pk[:, 0:256], hT[:, j, :], w_in_bf[:, j, 1536:1792], j == 0, False) for j in range(8)]
            mms.append((pk[:, 0:256], ones_bf[0:1, :], brow_bf[0:1, BIN + 1536:BIN + 1792], False, True))
            pe_mm(mms, [r_hT, r_w, r_brow, r_const], [r_pk])
            qk, r_qk = qk_r.next()
            for dup in range(2):
                S.op("act", (lambda e, dup=dup, qk=qk, pk=pk: e.activation(
                    qk[:, 0:256].rearrange("p (g d c) -> p g d c", g=2, d=2)[:, :, dup, :],
                    pk[:, 0:128].rearrange("p (g c) -> p g c", g=2), AF.Copy)), [r_pk], [r_qk])
            op1("act", "activation", [r_pk], [r_vc], vcaug[:, ci, :, 0:64],
                pk[:, 128:256].rearrange("p (g c) -> p g c", g=2), AF.Copy)
            pt, r_pt = ptr.next()
            pe_tr([(pt[:, g, :], qk[:, g * 128:(g + 1) * 128]) for g in range(2)], [r_qk, r_const], [r_pt])
            op1("dve", "tensor_copy", [r_pt], [r_kc], kcT2[:, :, ci * 128:(ci + 1) * 128], pt[:, 0:2, :])

        if stop_after == 0:
            dbg_kc = nc.dram_tensor("dbg_kc", [128, 512], BF16, kind="ExternalOutput").ap()
            d_dbg = S.dsem("dbg")
            rr = Reg("dbgo")
            dma("sp", dbg_kc, kcT2[:].rearrange("p g k -> p (g k)"), [r_kc], [rr], d_dbg)
            o = S.op("sp", None, reads=[rr])
            o.deps.update(z.idx for z in zero_ops)
            S.run()
            st1.close()
            st_mix.close()
            return nc
        tiles = {}

        def rope(src, H, dst3, rp, reads, r_dst):
            X = src.rearrange("p (h c) -> p h c", h=H)
            n = H * 64
            Av = rA[:, 0:n].rearrange("p (h c) -> p h c", h=H)
            Bv = rB[:, 0:n].rearrange("p (h c) -> p h c", h=H)
            op1("dve", "tensor_tensor", reads, [r_rA], Av, X, rp[:, 0:64].unsqueeze(1).to_broadcast([128, H, 64]),
                ALU.mult)
            for ax in range(2):
                for hf in range(2):
                    o0 = ax * 32 + hf * 16
                    i0 = ax * 32 + (1 - hf) * 16
                    op1("dve", "tensor_tensor", reads, [r_rB], Bv[:, :, o0:o0 + 16], X[:, :, i0:i0 + 16],
                        rp[:, 64 + o0:64 + o0 + 16].unsqueeze(1).to_broadcast([128, H, 16]), ALU.mult)
            for d3 in dst3:
                op1("pool", "tensor_tensor", [r_rA, r_rB], [r_dst], d3, Av, Bv, ALU.add)

        def stageA(i):
            T = {}
            xt, r_xt = xt_r.next()
            dma("sp", xt[:], x[i * 128:(i + 1) * 128, :], [], [r_xt], d_xt[(xt_r.i - 1) % 4])
            rp, r_rp = rp_r.next()
            dma("sp", rp[:], rope_d[i * 128:(i + 1) * 128, :], [], [r_rp], d_rp[(rp_r.i - 1) % 3])
            T["xt"] = (xt, r_xt)
            xsb, r_xsb = xs_r.next()
            hT, r_hT = hT_r.next()
            stt, r_stt = st_r.next()
            norm_T(xt[:], r_xt, A1, B1, 0, hT, r_hT, xsb, r_xsb, stt, r_stt)
            chunks = []
            for (c0, cw) in ((0, 512), (512, 512), (1024, 512), (1536, 128), (1664, 128)):
                pc, r_pc = ps_next()
                mms = [(pc[:, 0:cw], hT[:, j, :], w_in_bf[:, j, c0:c0 + cw], j == 0, False) for j in range(8)]
                mms.append((pc[:, 0:cw], ones_bf[0:1, :], brow_bf[0:1, BIN + c0:BIN + c0 + cw], False, True))
                pe_mm(mms, [r_hT, r_w, r_brow, r_const], [r_pc])
                chunks.append((pc, r_pc))
            (pq, r_pq), (psu, r_psu), (psv, r_psv), (pkv, r_pkv), (pvv, r_pvv) = chunks
            qk, r_qk = qk_r.next()
            rope(pq, 8, [qk[:, 0:512].rearrange("p (h c) -> p h c", h=8)], rp, [r_pq, r_rp], r_qk)
            kd = qk[:, 512:768].rearrange("p (g d c) -> p g d c", g=2, d=2)
            rope(pkv[:, 0:128], 2, [kd[:, :, 0, :], kd[:, :, 1, :]], rp, [r_pkv, r_rp], r_qk)
            if SUB == 1:
                return
            va, r_va = vaugr[i % 4]
            if SUB == 7:
                op1("dve", "tensor_copy", [r_pkv], [r_va], va[:, :, 0:64],
                    pkv[:, 128:256].rearrange("p (g c) -> p g c", g=2))
                return
            if SUB == 71:
                op1("dve", "tensor_copy", [r_pkv], [r_va], va[:, :, 0:64],
                    pkv[:, 0:128].rearrange("p (g c) -> p g c", g=2))
                return
            if SUB == 72:
                op1("dve", "tensor_copy", [r_qk], [r_va], va[:, :, 0:64],
                    qk[:, 0:128].rearrange("p (g c) -> p g c", g=2))
                return
            if SUB == 73:
                op1("dve", "tensor_copy", [r_pkv], [r_va], va[:, 0, 0:64], pkv[:, 128:192])
                return
            if SUB == 74:
                op1("dve", "tensor_copy", [r_pkv], [r_va], va[:, :, 0:64],
                    pkv[:, 256:384].rearrange("p (g c) -> p g c", g=2))
                return
            if SUB == 75:
                op1("dve", "tensor_copy", [r_pkv], [r_va], va[:, :, 0:64],
                    pkv[:, 64:192].rearrange("p (g c) -> p g c", g=2))
                return
            if SUB == 76:
                op1("dve", "tensor_copy", [r_pkv], [r_va], va[:, 0, 0:64], pkv[:, 192:256])
                return
            if SUB == 77:
                op1("dve", "tensor_copy", [r_pq], [r_va], va[:, 0, 0:64], pq[:, 192:256])
                return
            if SUB == 78:
                op1("dve", "tensor_copy", [r_pq], [r_va], va[:, 0, 0:64], pq[:, 448:512])
                return
            if SUB == 8:
                op1("act", "activation", [r_pkv], [r_junk], junk[:, 0:128].rearrange("p (g c) -> p g c", g=2),
                    pkv[:, 128:256].rearrange("p (g c) -> p g c", g=2), AF.Copy)
                return
            if SUB == 9:
                op1("act", "activation", [r_pkv], [r_va], va[:, :, 0:64],
                    pkv[:, 128:256].rearrange("p (g c) -> p g c", g=2), AF.Identity)
                return
            op1("act", "activation", [r_pvv], [r_va], va[:, :, 0:64],
                pvv[:, 0:128].rearrange("p (g c) -> p g c", g=2), AF.Copy)
            if SUB == 4:
                return
            pt, r_pt = ptr.next()
            pe_tr([(pt[:, a, :], qk[:, a * 128:(a + 1) * 128]) for a in range(6)], [r_qk, r_const], [r_pt])
            if SUB == 5:
                return
            qT, r_qT = qT_r.next()
            kT, r_kT = kT2r[i % 4]
            op1("dve", "tensor_copy", [r_pt], [r_qT], qT[:], pt[:, 0:4, :])
            if SUB == 6:
                return
            op1("dve", "tensor_copy", [r_pt], [r_kT], kT[:], pt[:, 4:6, :])
            T["qT"] = (qT, r_qT)
            if SUB == 2:
                return
            u, r_u = u_r.next()
            op1("act", "activation", [r_psu], [r_u], u[:], psu, AF.Gelu_apprx_tanh)
            gv, r_gv = gv_r.next()
            op1("act", "activation", [r_psv], [r_gv], gv[:], psv, AF.Gelu_apprx_tanh)
            bn, r_bn = bn_r.next()
            op1("dve", "bn_stats", [r_gv], [r_bn], bn[:, 0:6], gv[:])
            op1("dve", "bn_aggr", [r_bn], [r_bn], bn[:, 6:8], bn[:, 0:6])
            rstd_from(bn[:, 7:8], bn[:, 7:8], 1.0, r_bn, r_bn)
            z, r_z = z_r.next()
            op1("dve", "tensor_scalar", [r_gv, r_bn], [r_z], z[:], gv[:], bn[:, 6:7], bn[:, 7:8], ALU.subtract, ALU.mult)
            op1("pool", "tensor_tensor", [r_z, r_lngb], [r_z], z[:], z[:], lngb[:, 0:512], ALU.mult)
            vln, r_vln = vln_r.next()
            op1("pool", "tensor_tensor", [r_z, r_lngb], [r_vln], vln[:], z[:], lngb[:, 512:1024], ALU.add)
            if SUB == 3:
                return
            pm, r_pm = ps_next()
            mms = []
            for h in range(8):
                o = pm[:, h * 64:(h + 1) * 64]
                mms.append((o, sguw_bf[:, h * 128:(h + 1) * 128], vln[:, h * 64:(h + 1) * 64], True, False))
                mms.append((o, brow_bf[0:1, BSGU + h * 128:BSGU + (h + 1) * 128], ones_bf[0:1, 0:64], False, True))
            pe_mm(mms, [r_vln, r_w, r_brow, r_const], [r_pm])
            sgo, r_sgo = sgo_r.next()
            op1("dve", "tensor_tensor", [r_pm, r_u], [r_sgo], sgo[:], pm, u[:], ALU.mult)
            T["sgo"] = (sgo, r_sgo)
            tiles[i] = T

        SCALE = 64 ** -0.5

        def stageB(i):
            T = tiles.pop(i)
            xt, r_xt = T["xt"]
            qT, r_qT = T["qT"]
            sgo, r_sgo = T["sgo"]
            ao, r_ao = ao_r.next()
            for g in range(2):
                klist = []
                if i > 0:
                    klist.append(("p", kT2r[(i - 1) % 4], vaugr[(i - 1) % 4], None))
                klist.append(("o", kT2r[i % 4], vaugr[i % 4], None))
                if i < NT - 1:
                    klist.append(("n", kT2r[(i + 1) % 4], vaugr[(i + 1) % 4], None))
                klist.append(("c", (kcT2, r_kc), (vcaug, r_vc), 0))
                klist.append(("c", (kcT2, r_kc), (vcaug, r_vc), 1))
                plist = []
                for (kind, (kt, r_kt), (vt, r_vt), ci) in klist:
                    pss, r_pss = ps_pair()
                    mms = []
                    for h4 in range(4):
                        h = 4 * g + h4
                        a, hf = h // 2, h % 2
                        if kind == "c":
                            ks = kt[hf * 64:(hf + 1) * 64, g, ci * 128:(ci + 1) * 128]
                        else:
                            ks = kt[hf * 64:(hf + 1) * 64, g, :]
                        oc = hf * 512 + (h4 // 2) * 128
                        mms.append((pss[:, oc:oc + 128], ks, qT[hf * 64:(hf + 1) * 64, a, :], True, True))
                    pe_mm(mms, [r_kt, r_qT], r_pss)
                    pp, r_pp = p_r.next()
                    op1("act", "activation", r_pss, [r_pp], pp[:].rearrange("p (b c) -> p b c", b=2),
                        pss[:].rearrange("p (b c) -> p b c", b=2)[:, :, 0:256], AF.Exp, scale=SCALE)
                    if kind in ("p", "n"):
                        mk = mprev if kind == "p" else mnext
                        op1("pool", "tensor_tensor", [r_pp, r_const], [r_pp],
                            pp[:].rearrange("p (h q) -> p h q", h=4), pp[:].rearrange("p (h q) -> p h q", h=4),
                            mk.unsqueeze(1).to_broadcast([128, 4, 128]), ALU.mult)
                    if kind == "c":
                        vs = vt[:, ci, g, 0:65]
                    else:
                        vs = vt[:, g, 0:65]
                    plist.append((pp, r_pp, vs, r_vt))
                if SUB == 21:
                    return
                po, r_po = ps_next()
                mms = []
                rd = []
                for h4 in range(4):
                    for k, (pp, r_pp, vs, r_vt) in enumerate(plist):
                        pc_ = (h4 % 2) * 256 + (h4 // 2) * 128
                        mms.append((po[:, h4 * 66:h4 * 66 + 65], pp[:, pc_:pc_ + 128], vs,
                                    k == 0, k == len(plist) - 1))
                        rd += [r_pp, r_vt]
                pe_mm(mms, rd, [r_po])
                stt, r_stt = st_r.next()
                po3 = po[:, 0:264].rearrange("p (h c) -> p h c", h=4)
                op1("dve", "tensor_tensor", [r_po, r_const], [r_stt], stt[:, 0:4], po3[:, :, 64], esink[:, 4 * g:4 * g + 4],
                    ALU.add)
                op1("dve", "reciprocal", [r_stt], [r_stt], stt[:, 4:8], stt[:, 0:4])
                op1("dve", "tensor_tensor", [r_po, r_stt], [r_ao],
                    ao[:, g * 256:(g + 1) * 256].rearrange("p (h c) -> p h c", h=4), po3[:, :, 0:64],
                    stt[:, 4:8].unsqueeze(2).to_broadcast([128, 4, 64]), ALU.mult)
                if SUB == 22:
                    return
            if SUB == 23:
                return
            stt, r_stt = st_r.next()
            op1("act", "activation", [r_ao], [r_junk, r_stt], junk[:, 0:512], ao[:], AF.Square, accum_out=stt[:, 0:1])
            op1("act", "activation", [r_sgo], [r_junk, r_stt], junk[:, 512:1024], sgo[:], AF.Square,
                accum_out=stt[:, 1:2])
            rstd_from(stt[:, 0:2], stt[:, 2:4], 1.0 / 512, r_stt, r_stt)
            on, r_on = on_r.next()
            op1("dve", "tensor_scalar", [r_ao, r_stt], [r_on], on[:, 0:512], ao[:], stt[:, 2:3], None, ALU.mult)
            op1("pool", "tensor_scalar", [r_sgo, r_stt], [r_on], on[:, 512:1024], sgo[:], stt[:, 3:4], None, ALU.mult)
            pt, r_pt = ptr.next()
            pe_tr([(pt[:, j, :], on[:, j * 128:(j + 1) * 128]) for j in range(8)], [r_on, r_const], [r_pt])
            oT, r_oT = oT_r.next()
            op1("dve", "tensor_tensor", [r_pt, r_const], [r_oT], oT[:], pt[:],
                colp[:, 8:16].unsqueeze(2).to_broadcast([128, 8, 128]), ALU.mult)
            if SUB == 24:
                return
            pmix, r_pmix = ps_pair()
            mms = []
            for hh in range(2):
                o = pmix[:, hh * 512:(hh + 1) * 512]
                for j in range(8):
                    mms.append((o, oT[:, j, :], w_out_bf[:, j, hh * 512:(hh + 1) * 512], j == 0, False))
                mms.append((o, ones_bf[0:1, :], brow_bf[0:1, BOUT + hh * 512:BOUT + (hh + 1) * 512], False, True))
            pe_mm(mms, [r_oT, r_w, r_brow, r_const], r_pmix)
            stt, r_stt = st_r.next()
            op1("act", "activation", r_pmix, [r_junk, r_stt], junk[:], pmix[:], AF.Square, accum_out=stt[:, 0:1])
            rstd_from(stt[:, 0:1], stt[:, 1:2], 1.0 / 1024, r_stt, r_stt)
            tm, r_tm = tm_r.next()
            op1("dve", "scalar_tensor_tensor", r_pmix + [r_stt, r_MR], [r_tm], tm[:], pmix[:], stt[:, 1:2], MR[:, 0, :],
                ALU.mult, ALU.mult)
            xm, r_xm = xm_r.next()
            kx = (xm_r.i - 1) % 2
            op1("pool", "tensor_tensor", [r_tm, r_xt], [r_xm], xm[:], tm[:], xt[:], ALU.add)
            dma("sp", xmid_d[i * 128:(i + 1) * 128, :], xm[:], [r_xm], [r_xmd], d_xm[kx])
            if SUB == 25:
                return
            op1("act", "activation", [r_xm], [r_junk, r_stt], junk[:], xm[:], AF.Square, accum_out=stt[:, 2:3])
            rstd_from(stt[:, 2:3], stt[:, 3:4], 1.0 / 1024, r_stt, r_stt)
            h2f, r_h2f = h2f_r.next()
            op1("dve", "scalar_tensor_tensor", [r_xm, r_stt, r_MR], [r_h2f], h2f[:], xm[:], stt[:, 3:4], MR[:, 2, :],
                ALU.mult, ALU.mult)
            h2b, r_h2b = h2b_r.next()
            kh = (h2b_r.i - 1) % 2
            op1("pool", "tensor_tensor", [r_h2f, r_MR], [r_h2b], h2b[:], h2f[:], MR[:, 1, :], ALU.add)
            dma("sp", h2_d[i * 128:(i + 1) * 128, :], h2b[:], [r_h2b], [r_h2d], d_h2[kh])
            if SUB == 26:
                return
            pt, r_pt = ptr.next()
            pe_tr([(pt[:, j, :], h2b[:, j * 128:(j + 1) * 128]) for j in range(8)], [r_h2b, r_const], [r_pt])
            h2T, r_h2T = h2T_r.next()
            op1("act", "activation", [r_pt], [r_h2T], h2T[:], pt[:], AF.Copy)
            pl, r_pl = ps_next()
            mms = [(pl[:, 0:32], h2T[:, j, :], wr_bf[:, j, :], j == 0, False) for j in range(8)]
            mms.append((pl[:, 0:32], ones_bf[0:1, :], brow_bf[0:1, BRT:BRT + 32], False, True))
            pe_mm(mms, [r_h2T, r_w, r_brow, r_const], [r_pl])
            lg, r_lg = lg_r.next()
            op1("dve", "tensor_copy", [r_pl], [r_lg], lg[:], pl[:, 0:32])
            t8, r_t8 = t8_r.next()
            op1("dve", "max", [r_lg], [r_t8], t8[:], lg[:])
            if SUB == 27:
                return
            mk, r_mk = mk_r.next()
            op1("dve", "tensor_scalar", [r_lg, r_t8], [r_mk], mk[:], lg[:], t8[:, 3:4], None, ALU.is_ge)
            mkb, r_mkb = mkb_r.next()
            op1("dve", "tensor_copy", [r_mk], [r_mkb], mkb[:], mk[:])
            ex, r_ex = ex_r.next()
            op1("dve", "tensor_scalar", [r_t8], [r_ex], ex[:, 0:4], t8[:, 0:4], t8[:, 0:1], None, ALU.subtract)
            op1("act", "activation", [r_ex], [r_ex], ex[:, 4:8], ex[:, 0:4], AF.Exp)
            op1("dve", "tensor_reduce", [r_ex], [r_ex], ex[:, 8:9], ex[:, 4:8], AX.X, ALU.add)
            op1("dve", "reciprocal", [r_ex], [r_ex], ex[:, 9:10], ex[:, 8:9])
            op1("dve", "tensor_scalar", [r_ex], [r_route], gk_all[:, i * 4:(i + 1) * 4], ex[:, 4:8], ex[:, 9:10], None,
                ALU.mult)
            oh, r_oh = oh_r.next()
            for k in range(4):
                op1("dve", "tensor_scalar", [r_lg, r_t8], [r_oh], oh[:, k, :], lg[:], t8[:, k:k + 1], None, ALU.is_equal)
            pc2, r_pc2 = ps_next()
            pe_mm([(pc2[:, 0:32], Utri, mkb[:], True, True), (pc2[:, 32:64], ones_bf, mkb[:], True, True)],
                  [r_mkb, r_const], [r_pc2])
            Dm, r_Dm = Dm_r.next()
            op1("dve", "tensor_tensor", [r_pc2, r_run], [r_Dm], Dm[:], pc2[:, 0:32], runc[:], ALU.add)
            op1("dve", "tensor_tensor", [r_pc2, r_run], [r_run], runc[:], runc[:], pc2[:, 32:64], ALU.add)
            tq, r_tq = tq_r.next()
            op1("dve", "tensor_tensor", [r_oh, r_Dm], [r_tq], tq[:], oh[:], Dm[:].unsqueeze(1).to_broadcast([128, 4, 32]),
                ALU.mult)
            op1("dve", "tensor_reduce", [r_tq], [r_route], rank_all[:, i * 4:(i + 1) * 4], tq[:], AX.X, ALU.add)
            op1("dve", "tensor_tensor", [r_oh, r_const], [r_tq], tq[:], oh[:],
                iota_e.unsqueeze(1).to_broadcast([128, 4, 32]), ALU.mult)
            op1("dve", "tensor_reduce", [r_tq], [r_route], ek_all[:, i * 4:(i + 1) * 4], tq[:], AX.X, ALU.add)

        def all_p1_regs():
            rr_ = [r_junk, r_rA, r_rB, r_kc, r_vc, r_w, r_brow, r_lngb, r_AB, r_route, r_run, r_xmd, r_h2d]
            for ring in (xt_r, rp_r, xs_r, hT_r, qk_r, qT_r, u_r, gv_r, z_r, vln_r, sgo_r, p_r, ao_r, on_r, oT_r, tm_r,
                         xm_r, h2f_r, h2b_r, h2T_r, st_r, bn_r, lg_r, t8_r, mk_r, mkb_r, ex_r, oh_r, Dm_r, tq_r, ptr):
                rr_ += [r for (_, r) in ring.items]
            rr_ += [r for (_, r) in kT2r] + [r for (_, r) in vaugr] + [r for (_, _, r) in pslots]
            return rr_

        def finish_dbg():
            o = S.op("sp", None, reads=all_p1_regs())
            o.deps.update(z.idx for z in zero_ops)
            S.run()
            st1.close()
            st_mix.close()
            return nc

        stageA(0)
        if stop_after == 10:
            return finish_dbg()
        for i in range(1, NT):
            stageA(i)
            if stop_after == 11:
                return finish_dbg()
            stageB(i - 1)
            if stop_after == 12:
                return finish_dbg()
        stageB(NT - 1)

        allr1 = [r_junk, r_rA, r_rB, r_kc, r_vc, r_w, r_brow, r_lngb, r_AB]
        for ring in (xt_r, rp_r, xs_r, hT_r, qk_r, qT_r, u_r, gv_r, z_r, vln_r, sgo_r, p_r, ao_r, on_r, oT_r, tm_r,
                     xm_r, h2f_r, h2b_r, h2T_r, st_r, bn_r, lg_r, t8_r, mk_r, mkb_r, ex_r, oh_r, Dm_r, tq_r, ptr):
            allr1 += [r for (_, r) in ring.items]
        allr1 += [r for (_, r) in kT2r] + [r for (_, r) in vaugr] + [r for (_, _, r) in pslots]
        for e in Sched.ENGS:
            S.op(e, None, reads=allr1)
        if stop_after == 1:
            d_dbg = S.dsem("dbg")
            rr = [Reg("dbgo") for _ in range(4)]
            n4 = NT * 4
            dma("sp", dbg_route[:, 0:n4], rank_all[:], [r_route], [rr[0]], d_dbg)
            dma("sp", dbg_route[:, n4:2 * n4], ek_all[:], [r_route], [rr[1]], d_dbg)
            dma("sp", dbg_route[:, 2 * n4:3 * n4], gk_all[:], [r_route], [rr[2]], d_dbg)
            dma("sp", dbg_route[:, 3 * n4:3 * n4 + 32], runc[:], [r_run], [rr[3]], d_dbg)
            d_dbg.seal()
            o = S.op("sp", None, reads=rr + [r_xmd, r_h2d])
            o.deps.update(z.idx for z in zero_ops)
            S.run()
            st1.close()
            st_mix.close()
            return nc
        st1.close()
        st_mix.close()

        st2 = ExitStack()
        ci_ = sbuf(st2, "ci_", [128, 32], I32)
        pf = sbuf(st2, "pf", [128, 32], F32)
        cs0 = sbuf(st2, "cs0", [128, 32], F32)
        cs1 = sbuf(st2, "cs1", [128, 32], F32)
        pst = sbuf(st2, "pst", [128, 32], F32)
        cmp8 = sbuf(st2, "cmp8", [128, 32, 8], F32)
        cmpb = sbuf(st2, "cmpb", [128, NB, 32], F32)
        ebf = sbuf(st2, "ebf", [128, NB], F32)
        ohb = sbuf(st2, "ohb", [128, NT * 4, 32], F32)
        psel = sbuf(st2, "psel", [128, NT * 4], F32)
        r_b = Reg("book")
        LOG2R = R.bit_length() - 1
        op1("dve", "tensor_tensor", [r_run, r_const], [r_b], cmp8[:], runc[:].unsqueeze(2).to_broadcast([128, 32, 8]),
            thr8.unsqueeze(1).to_broadcast([128, 32, 8]), ALU.is_gt)
        op1("dve", "tensor_reduce", [r_b], [r_b], pf[:], cmp8[:], AX.X, ALU.add)
        op1("dve", "tensor_scalar", [r_b], [r_b], pf[:], pf[:], float(R), None, ALU.mult)
        op1("dve", "tensor_copy", [r_b], [r_b], cs0[:], pf[:])
        cur, nxt = cs0, cs1
        for s in (1, 2, 4, 8, 16):
            op1("dve", "tensor_copy", [r_b], [r_b], nxt[:, 0:s], cur[:, 0:s])
            op1("dve", "tensor_tensor", [r_b], [r_b], nxt[:, s:32], cur[:, s:32], cur[:, 0:32 - s], ALU.add)
            cur, nxt = nxt, cur
        pend = cur
        op1("dve", "tensor_tensor", [r_b], [r_b], pst[:], pend[:], pf[:], ALU.subtract)
        op1("dve", "tensor_tensor", [r_b, r_const], [r_b], cmpb[:], pend[:].unsqueeze(1).to_broadcast([128, NB, 32]),
            bstart.unsqueeze(2).to_broadcast([128, NB, 32]), ALU.is_le)
        op1("dve", "tensor_reduce", [r_b], [r_b], ebf[:], cmpb[:], AX.X, ALU.add)
        op1("dve", "tensor_scalar", [r_b], [r_b], ebf[:], ebf[:], 31.0, None, ALU.min)
        op1("dve", "tensor_scalar", [r_b, r_const], [r_b], ebf[:], ebf[:], 128.0, pidx, ALU.mult, ALU.add)
        op1("dve", "tensor_copy", [r_b], [r_eblk], widx[:], ebf[:])
        op1("dve", "tensor_tensor", [r_route, r_const], [r_b], ohb[:],
            ek_all[:].unsqueeze(2).to_broadcast([128, NT * 4, 32]),
            iota_e.unsqueeze(1).to_broadcast([128, NT * 4, 32]), ALU.is_equal)
        op1("dve", "tensor_tensor", [r_b], [r_b], ohb[:], ohb[:], pst[:].unsqueeze(1).to_broadcast([128, NT * 4, 32]),
            ALU.mult)
        op1("dve", "tensor_reduce", [r_b], [r_b], psel[:], ohb[:], AX.X, ALU.add)
        op1("dve", "tensor_tensor", [r_b, r_route], [r_b], psel[:], psel[:], rank_all[:], ALU.add)
        op1("dve", "tensor_copy", [r_b], [r_dest], dest_i[:], psel[:])

        h2l_r = mkring(st2, "h2l", [128, 1024], BF16, 3)
        d_h2l = [S.dsem("h2l%d" % k) for k in range(3)]
        d_sc = [S.dsem("scatter%d" % k) for k in range(3)]
        for i in range(NT):
            hl, r_hl = h2l_r.next()
            dma("sp", hl[:], h2_d[i * 128:(i + 1) * 128, :], [r_h2d], [r_hl], d_h2l[i % 3])
            for k in range(4):
                c = i * 4 + k
                o = S.op("pool", (lambda e, hl=hl, c=c: e.indirect_dma_start(
                    out=xs_d, out_offset=bass.IndirectOffsetOnAxis(ap=dest_i[:, c:c + 1], axis=0),
                    in_=hl[:], in_offset=None)),
                    [r_hl, r_dest], [Reg("xs_sc")], dsem=d_sc[i % 3])
                o.deps.update(z.idx for z in zero_ops)
        scatter_ops = []
        for d in d_sc:
            scatter_ops += d.ops
        if stop_after == 2:
            d_dbg = S.dsem("dbg")
            rr = [Reg("dbgo") for _ in range(2)]
            dma("sp", dbg_book[:, 0:NT * 4], dest_i[:], [r_dest], [rr[0]], d_dbg)
            dma("sp", dbg_book[:, NT * 4:NT * 4 + NB], widx[:], [r_eblk], [rr[1]], d_dbg)
            d_dbg.seal()
            o = S.op("sp", None, reads=rr)
            o.deps.update(z.idx for z in scatter_ops)
            S.run()
            st2.close()
            return nc
        for e in Sched.ENGS:
            S.op(e, None, reads=[r_b] + [r for (_, r) in h2l_r.items])
        st2.close()

        st3 = ExitStack()
        wgu_r = mkring(st3, "wgu", [128, 8, 2048], BF16, 2)
        wd_r = mkring(st3, "wd", [128, 8, 1024], BF16, 2)
        bg_r = mkring(st3, "bg", [128, 16], F32, 2)
        d_wgu = [S.dsem("wgu0"), S.dsem("wgu1")]
        d_wd = [S.dsem("wd0"), S.dsem("wd1")]
        d_bg = [S.dsem("bg0"), S.dsem("bg1")]
        xb_r = mkring(st3, "xb", [128, RG, 1024], BF16, 2)
        d_xb = [S.dsem("xb0"), S.dsem("xb1")]
        xbT_r = mkring(st3, "xbT", [128, 8, R], BF16, 2)
        aT_r = mkring(st3, "aT", [128, 8, R], BF16, 2)
        gc_r = mkring(st3, "gc", [128, R], F32, 2)
        uc_r = mkring(st3, "uc", [128, R], F32, 2)
        sg_r = mkring(st3, "sg", [128, R], F32, 2)
        yo_r = mkring(st3, "yo", [128, 1024], F32, 3)
        d_yo = [S.dsem("yo%d" % k) for k in range(3)]
        r_ys = Reg("ys_d")
        wg_regs = [[Reg("wgk") for _ in range(8)] for _ in range(2)]
        wd_regs = [[Reg("wdk") for _ in range(8)] for _ in range(2)]

        for b in range(NB):
            kb = b % 2
            wg, r_wg = wgu_r.next()
            wdn, r_wdn = wd_r.next()
            bg, r_bg = bg_r.next()
            r_wgk = wg_regs[kb]
            r_wdk = wd_regs[kb]

            for k in range(8):
                S.op("pool", (lambda e, b=b, wg=wg, k=k: e.indirect_dma_start(
                    out=wg[:, k, :], out_offset=None, in_=wgu[k],
                    in_offset=bass.IndirectOffsetOnAxis(ap=widx[:, b:b + 1], axis=0))), [r_eblk], [r_wgk[k]], dsem=d_wgu[kb])
            for k in range(8):
                S.op("pool", (lambda e, b=b, wdn=wdn, k=k: e.indirect_dma_start(
                    out=wdn[:, k, :], out_offset=None, in_=wd[k],
                    in_offset=bass.IndirectOffsetOnAxis(ap=widx[:, b:b + 1], axis=0))), [r_eblk], [r_wdk[k]], dsem=d_wd[kb])
            S.op("pool", (lambda e, b=b, bg=bg: e.indirect_dma_start(
                out=bg[:], out_offset=None, in_=bgu,
                in_offset=bass.IndirectOffsetOnAxis(ap=widx[:, b:b + 1], axis=0))), [r_eblk], [r_bg], dsem=d_bg[kb])
            xb, r_xb = xb_r.next()
            o = S.op("sp", (lambda e, xb=xb, b=b: e.dma_start(
                out=xb[:], in_=xs_d[b * R:(b + 1) * R, :].rearrange("(g p) n -> p g n", p=128))),
                [], [r_xb], dsem=d_xb[kb])
            o.deps.update(z.idx for z in scatter_ops)
            xbT, r_xbT = xbT_r.next()
            for rg in range(RG):
                pt, r_pt = ptr.next()
                pe_tr([(pt[:, j, :], xb[:, rg, j * 128:(j + 1) * 128]) for j in range(8)], [r_xb, r_const], [r_pt])
                op1("act", "activation", [r_pt], [r_xbT], xbT[:, :, rg * 128:(rg + 1) * 128], pt[:], AF.Copy)
            aT, r_aT = aT_r.next()
            for j in range(8):
                pg, r_pg = ps_next()
                pu, r_pu = ps_next()
                pe_mm([(pg[:, 0:R], wg[:, k, j * 128:(j + 1) * 128], xbT[:, k, :], k == 0, k == 7) for k in range(8)],
                      r_wgk + [r_xbT], [r_pg])
                pe_mm([(pu[:, 0:R], wg[:, k, 1024 + j * 128:1024 + (j + 1) * 128], xbT[:, k, :], k == 0, k == 7)
                       for k in range(8)], r_wgk + [r_xbT], [r_pu])
                gc, r_gc = gc_r.next()
                uc, r_uc = uc_r.next()
                sg, r_sg = sg_r.next()
                op1("dve", "tensor_scalar", [r_pg, r_bg], [r_gc], gc[:], pg[:, 0:R], bg[:, j:j + 1], 7.0, ALU.add, ALU.min)
                op1("act", "activation", [r_pu, r_bg], [r_uc], uc[:], pu[:, 0:R], AF.Identity, bias=bg[:, 8 + j:9 + j])
                op1("act", "activation", [r_gc], [r_sg], sg[:], gc[:], AF.Sigmoid, scale=1.702)
                op1("dve", "tensor_scalar", [r_uc], [r_uc], uc[:], uc[:], 7.0, -7.0, ALU.min, ALU.max)
                op1("dve", "tensor_tensor", [r_gc, r_sg], [r_sg], sg[:], gc[:], sg[:], ALU.mult)
                op1("dve", "scalar_tensor_tensor", [r_uc, r_sg], [r_aT], aT[:, j, :], uc[:], 1.0, sg[:], ALU.add, ALU.mult)
            for rg in range(RG):
                yo, r_yo = yo_r.next()
                ky = (yo_r.i - 1) % 3
                for hh in range(2):
                    py, r_py = ps_next()
                    pe_mm([(py, aT[:, k, rg * 128:(rg + 1) * 128], wdn[:, k, hh * 512:(hh + 1) * 512], k == 0, k == 7)
                           for k in range(8)], [r_aT] + r_wdk, [r_py])
                    op1("act", "activation", [r_py], [r_yo], yo[:, hh * 512:(hh + 1) * 512], py, AF.Copy)
                dma("sp", ys_d[b * R + rg * 128:b * R + (rg + 1) * 128, :], yo[:], [r_yo], [Reg("ys_w")], d_yo[ky])
        ys_ops = []
        for d in d_yo:
            ys_ops += d.ops
        allr3 = []
        for ring in (wgu_r, wd_r, bg_r, xb_r, xbT_r, aT_r, gc_r, uc_r, sg_r, yo_r, ptr):
            allr3 += [r for (_, r) in ring.items]
        allr3 += [r for (_, _, r) in pslots]
        for kk in range(2):
            allr3 += wg_regs[kk] + wd_regs[kk]
        for e in Sched.ENGS:
            o = S.op(e, None, reads=allr3)
            o.deps.update(z.idx for z in ys_ops)
        st3.close()

        st4 = ExitStack()
        yg_r = mkring(st4, "yg", [128, 4, 1024], F32, 2)
        d_yg = [S.dsem("yg0"), S.dsem("yg1")]
        xl_r = mkring(st4, "xl", [128, 1024], F32, 2)
        d_xl = [S.dsem("xl0"), S.dsem("xl1")]
        acc_r = mkring(st4, "acc", [128, 1024], F32, 2)
        ob_r = mkring(st4, "ob", [128, 1024], F32, 2)
        G_r = mkring(st4, "G", [128, 32], F32, 2)
        Gb_r = mkring(st4, "Gb", [128, 32], BF16, 2)
        GT_r = mkring(st4, "GT", [32, 128], BF16, 2)
        toh_r = mkring(st4, "toh", [128, 32], F32, 2)
        st4_r = mkring(st4, "st4", [128, 4], F32, 2)
        junk4 = sbuf(st4, "junk4", [128, 1024], BF16)
        r_junk4 = Reg("junk4")
        out_regs = []
        yg_regs = [[Reg("ygk") for _ in range(4)] for _ in range(2)]
        for i in range(NT):
            yg, r_yg0 = yg_r.next()
            ky = i % 2
            r_ygk = yg_regs[ky]
            r_yg = r_ygk[0]
            for k in range(4):
                c = i * 4 + k
                S.op("pool", (lambda e, yg=yg, k=k, c=c: e.indirect_dma_start(
                    out=yg[:, k, :], out_offset=None, in_=ys_d,
                    in_offset=bass.IndirectOffsetOnAxis(ap=dest_i[:, c:c + 1], axis=0))), [r_dest, r_ys], [r_ygk[k]],
                    dsem=d_yg[ky])
            xl, r_xl = xl_r.next()
            dma("sp", xl[:], xmid_d[i * 128:(i + 1) * 128, :], [r_xmd], [r_xl], d_xl[ky])
            G, r_G = G_r.next()
            toh, r_toh = toh_r.next()
            for k in range(4):
                c = i * 4 + k
                op1("dve", "tensor_scalar", [r_route, r_const], [r_toh], toh[:], iota_e, ek_all[:, c:c + 1],
                    None, ALU.is_equal)
                op1("dve", "tensor_scalar", [r_route, r_toh], [r_toh], toh[:], toh[:], gk_all[:, c:c + 1],
                    None, ALU.mult)
                if k == 0:
                    op1("dve", "tensor_copy", [r_toh], [r_G], G[:], toh[:])
                else:
                    op1("dve", "tensor_tensor", [r_toh, r_G], [r_G], G[:], G[:], toh[:], ALU.add)
            Gb, r_Gb = Gb_r.next()
            op1("dve", "tensor_copy", [r_G], [r_Gb], Gb[:], G[:])
            pt, r_pt = ptr.next()
            pe_tr([(pt[0:32, 0, :], Gb[:])], [r_Gb, r_const], [r_pt])
            GT, r_GT = GT_r.next()
            op1("act", "activation", [r_pt], [r_GT], GT[:], pt[0:32, 0, :], AF.Copy)
            pbd, r_pbd = ps_pair()
            pe_mm([(pbd[:, hh * 512:(hh + 1) * 512], GT[:], bdown_bf[:, hh * 512:(hh + 1) * 512], True, True)
                   for hh in range(2)], [r_GT, r_bdown], r_pbd)
            acc, r_acc = acc_r.next()
            c0 = i * 4
            op1("dve", "scalar_tensor_tensor", r_ygk + [r_route] + r_pbd, [r_acc], acc[:], yg[:, 0, :],
                gk_all[:, c0:c0 + 1], pbd[:], ALU.mult, ALU.add)
            for k in (1, 2, 3):
                eng = "dve"
                op1(eng, "scalar_tensor_tensor", r_ygk + [r_route, r_acc], [r_acc], acc[:], yg[:, k, :],
                    gk_all[:, c0 + k:c0 + k + 1], acc[:], ALU.mult, ALU.add)
            s4, r_s4 = st4_r.next()
            op1("act", "activation", [r_acc], [r_junk4, r_s4], junk4[:], acc[:], AF.Square, accum_out=s4[:, 0:1])
            rstd_from(s4[:, 0:1], s4[:, 1:2], 1.0 / 1024, r_s4, r_s4)
            ob, r_ob = ob_r.next()
            op1("dve", "scalar_tensor_tensor", [r_acc, r_s4, r_MR], [r_ob], ob[:], acc[:], s4[:, 1:2], MR[:, 3, :],
                ALU.mult, ALU.mult)
            op1("pool", "tensor_tensor", [r_ob, r_xl], [r_ob], ob[:], ob[:], xl[:], ALU.add)
            ro = Reg("out")
            dma("sp", out[i * 128:(i + 1) * 128, :], ob[:], [r_ob], [ro], d_out[i % 2])
            out_regs.append(ro)
        S.op("sp", None, reads=out_regs)
        S.run()
        st4.close()
    return nc


def _host_consts():
    ident = np.eye(128, dtype=np.float32)
    U = np.triu(np.ones((128, 128), np.float32), k=1)
    ones = np.ones((128, 128), np.float32)
    jj = np.arange(128)[:, None]
    ii = np.arange(128)[None, :]
    mprev = (jj >= ii).astype(np.float32)
    mnext = (jj <= ii).astype(np.float32)
    cbf = np.concatenate([ident, U, ones, mprev, mnext], axis=1)
    cf32 = np.concatenate([np.tile(np.arange(32, dtype=np.float32), (128, 1)),
                           np.tile(np.arange(NB, dtype=np.float32) * R, (128, 1)),
                           np.arange(128, dtype=np.float32)[:, None],
                           np.tile(np.arange(8, dtype=np.float32) * R, (128, 1))], axis=1)
    t = np.arange(4096)
    pos_row = (t // 64).astype(np.float32)
    pos_col = (t % 64).astype(np.float32)
    inv_freq = (np.float32(10000.0) ** (-np.arange(16, dtype=np.float32) / np.float32(16))).astype(np.float32)
    ar = pos_row[:, None] * inv_freq
    ac = pos_col[:, None] * inv_freq
    cr, sr, cc_, sc_ = np.cos(ar), np.sin(ar), np.cos(ac), np.sin(ac)
    rope = np.concatenate([cr, cr, cc_, cc_, -sr, sr, -sc_, sc_], axis=1).astype(np.float32)
    return np.ascontiguousarray(cbf), np.ascontiguousarray(cf32), np.ascontiguousarray(rope)


_CACHE = {}


def _prep(x, c, ctx, c_ctx, w_ada, b_ada, g_pre_mix, g_post_mix, g_pre_ffn, g_post_ffn,
          w_in, b_in, attn_sink, sgu_ln_g, sgu_ln_b, sgu_w, sgu_b, g_attn_out, g_sgu_out,
          w_out, b_out, w_router, b_router, w_gate_up, b_gate_up, w_down, b_down):
    f = lambda a: np.ascontiguousarray(np.asarray(a, dtype=np.float32))
    x, c, ctx, c_ctx = f(x), f(c), f(ctx), f(c_ctx)
    cbf, cf32, rope = _host_consts()
    perm = np.concatenate([np.arange(0, 512), np.arange(768, 1280), np.arange(1280, 1792), np.arange(512, 768)])
    w_in_p = f(f(w_in)[0][:, perm])
    b_in_p = f(b_in)[0][perm]
    col = lambda v: f(v).reshape(8, 128).T
    colpack = f(np.concatenate([col(f(g_pre_mix)[0]),
                                col(np.concatenate([f(g_attn_out)[0], f(g_sgu_out)[0]]))], axis=1))
    rowpack = f(np.concatenate([f(g_post_mix)[0], f(g_pre_ffn)[0], f(g_post_ffn)[0], f(sgu_ln_g)[0],
                                f(sgu_ln_b)[0], f(attn_sink)[0]])[None, :])
    browf = f(np.concatenate([b_in_p, f(b_out)[0], f(sgu_b)[0].reshape(-1), f(b_router)[0]])[None, :])
    sguwT = f(np.transpose(f(sgu_w)[0], (2, 0, 1)).reshape(128, 1024))
    wgu3 = f(w_gate_up)[0]
    wd3 = f(w_down)[0]
    bgu = f(f(b_gate_up)[0].reshape(32, 2, 8, 128).transpose(0, 3, 1, 2).reshape(32 * 128, 16))
    shared = {
        "w_ada": f(w_ada)[0], "b_ada": f(b_ada), "colpack": colpack, "rowpack": rowpack, "browf": browf,
        "w_in": w_in_p, "w_out": f(w_out)[0], "w_r": f(w_router)[0], "sguwT": sguwT, "b_down": f(b_down)[0],
        "bgu": bgu, "cbf": cbf, "cf32": cf32, "rope": rope,
    }
    for k in range(8):
        shared["wgu%d" % k] = f(wgu3[:, k * 128:(k + 1) * 128, :].reshape(32 * 128, 2048))
        shared["wd%d" % k] = f(wd3[:, k * 128:(k + 1) * 128, :].reshape(32 * 128, 1024))
    in_maps = []
    for b in range(8):
        m = dict(shared)
        m["x"] = x[b]
        m["ctx"] = ctx[b]
        m["cc"] = f(np.stack([c[b].reshape(8, 128).T, c_ctx.reshape(8, 128).T], axis=2).reshape(128, 16))
        in_maps.append(m)
    return in_maps


def kernel(**inputs):
    if "nc" not in _CACHE:
        _CACHE["nc"] = build_program()
    nc = _CACHE["nc"]
    in_maps = _prep(**inputs)
    res = run_bass_kernel_spmd(nc, in_maps, core_ids=list(range(8)))
    return np.stack([np.asarray(r["out"], dtype=np.float32) for r in res.results], axis=0)
```

```python
import sys
import numpy as np
from contextlib import ExitStack
import concourse.bass as bass
import concourse.mybir as mybir
from concourse.bass_utils import run_bass_kernel_spmd

F32 = mybir.dt.float32
BF16 = mybir.dt.bfloat16
I32 = mybir.dt.int32
ALU = mybir.AluOpType
AF = mybir.ActivationFunctionType
AX = mybir.AxisListType

NT = 32
R = 512
NB = 64
RG = R // 128
EPS = 1e-6
SUB = 0


class Reg:
    __slots__ = ("name", "w", "rs")

    def __init__(self, name=""):
        self.name = name
        self.w = None
        self.rs = []


class DSem:
    def __init__(self, sem):
        self.sem = sem
        self.count = 0
        self.ops = []

    def seal(self):
        for o in self.ops:
            o.dcount = self.count


class Op:
    __slots__ = ("eng", "fn", "deps", "sig", "sigcount", "dsem", "dcount", "idx", "tag")


class Sched:
    ENGS = ("pe", "act", "dve", "pool", "sp")

    def __init__(self, nc, stack):
        self.nc = nc
        self.stack = stack
        self.ops = []
        self.esem = {e: stack.enter_context(nc.semaphore("es_" + e)) for e in ("pe", "act", "dve", "pool")}

    def dsem(self, name):
        return DSem(self.stack.enter_context(self.nc.semaphore("ds_" + name)))

    def op(self, eng, fn, reads=(), writes=(), dsem=None):
        o = Op()
        o.eng = eng
        o.fn = fn
        o.dsem = dsem
        o.sig = False
        o.sigcount = 0
        o.dcount = 0
        o.idx = len(self.ops)
        fr = sys._getframe(1)
        tags = []
        while fr is not None and len(tags) < 4:
            if fr.f_code.co_filename == __file__:
                tags.append(str(fr.f_lineno))
            fr = fr.f_back
        o.tag = "L" + "<".join(tags)
        deps = set()
        for r in reads:
            if r.w is not None:
                deps.add(r.w)
        for r in writes:
            if r.w is not None:
                deps.add(r.w)
            deps.update(r.rs)
        deps.discard(o.idx)
        if fn is not None:
            for r in reads:
                r.rs.append(o.idx)
            for r in writes:
                r.w = o.idx
                r.rs = []
        o.deps = deps
        if dsem is not None:
            dsem.count += 16
            o.dcount = dsem.count
            dsem.ops.append(o)
        self.ops.append(o)
        return o

    def finalize(self):
        ops = self.ops
        for o in ops:
            for d in o.deps:
                t = ops[d]
                if t.dsem is not None:
                    continue
                if t.eng == o.eng and o.eng in ("pe", "sp"):
                    continue
                t.sig = True
        cnt = {e: 0 for e in self.ENGS}
        for o in ops:
            if o.dsem is None and o.sig:
                cnt[o.eng] += 1
                o.sigcount = cnt[o.eng]

    def emit(self, engname, engobj):
        ops = self.ops
        waited = {}
        for o in ops:
            if o.eng != engname:
                continue
            need = {}
            for d in o.deps:
                t = ops[d]
                if t.dsem is not None:
                    key = ("d", id(t.dsem))
                    sem = t.dsem.sem
                    val = t.dcount
                else:
                    if t.eng == o.eng and o.eng in ("pe", "sp"):
                        continue
                    key = ("e", t.eng)
                    sem = self.esem[t.eng]
                    val = t.sigcount
                if key not in need or need[key][1] < val:
                    need[key] = (sem, val)
            for key, (sem, val) in need.items():
                if waited.get(key, 0) < val:
                    engobj.wait_ge(sem, val)
                    waited[key] = val
            if o.fn is None:
                continue
            ins = o.fn(engobj)
            try:
                ins.annotate(o.tag)
            except Exception:
                pass
            if o.dsem is not None:
                ins.then_inc(o.dsem.sem, 16)
            elif o.sig:
                ins.then_inc(self.esem[o.eng], 1)

    def run(self):
        self.finalize()
        nc = self.nc
        with nc.Block() as block:
            @block.tensor
            def _(e):
                self.emit("pe", e)

            @block.scalar
            def _(e):
                self.emit("act", e)

            @block.vector
            def _(e):
                self.emit("dve", e)

            @block.gpsimd
            def _(e):
                self.emit("pool", e)

            @block.sync
            def _(e):
                self.emit("sp", e)


class Ring:
    def __init__(self, items):
        self.items = items
        self.i = 0

    def next(self):
        it = self.items[self.i % len(self.items)]
        self.i += 1
        return it


def build_program(stop_after=None):
    nc = bass.Bass("TRN2", target_bir_lowering=False)

    def din(name, shape, dt=F32):
        return nc.dram_tensor(name, shape, dt, kind="ExternalInput").ap()

    x = din("x", [4096, 1024])
    ctx = din("ctx", [256, 1024])
    cc = din("cc", [128, 16])
    w_ada = din("w_ada", [1024, 6144])
    b_ada = din("b_ada", [1, 6144])
    colpack = din("colpack", [128, 16])
    rowpack = din("rowpack", [1, 4104])
    browf = din("browf", [1, 3872])
    w_in = din("w_in", [1024, 1792])
    w_out = din("w_out", [1024, 1024])
    w_r = din("w_r", [1024, 32])
    sguwT = din("sguwT", [128, 1024])
    b_down = din("b_down", [32, 1024])
    if stop_after is None:
        wgu = [din("wgu%d" % k, [4096, 2048]) for k in range(8)]
        wd = [din("wd%d" % k, [4096, 1024]) for k in range(8)]
        bgu = din("bgu", [4096, 16])
    cbf_d = din("cbf", [128, 640])
    cf32_d = din("cf32", [128, 32 + NB + 9])
    rope_d = din("rope", [4096, 128])
    out = nc.dram_tensor("out", [4096, 1024], F32, kind="ExternalOutput").ap()
    ik = "Internal" if stop_after is None else "ExternalOutput"
    xs_d = nc.dram_tensor("xs_d", [NB * R, 1024], BF16, kind=ik).ap()
    ys_d = nc.dram_tensor("ys_d", [NB * R, 1024], F32, kind=ik).ap()
    xmid_d = nc.dram_tensor("xmid_d", [4096, 1024], F32, kind=ik).ap()
    h2_d = nc.dram_tensor("h2_d", [4096, 1024], BF16, kind=ik).ap()
    if stop_after is not None:
        dbg_route = nc.dram_tensor("dbg_route", [128, 3 * NT * 4 + 32], F32, kind="ExternalOutput").ap()
        dbg_book = nc.dram_tensor("dbg_book", [128, NT * 4 + NB], I32, kind="ExternalOutput").ap()

    with ExitStack() as st_all:
        S = Sched(nc, st_all)

        def sbuf(st, name, shape, dt):
            return st.enter_context(nc.sbuf_tensor("s_" + name, shape, dt))

        def mkring(st, name, shape, dt, n):
            return Ring([(sbuf(st, "%s%d" % (name, k), shape, dt), Reg(name)) for k in range(n)])

        def op1(eng, meth, reads, writes, *a, **k):
            S.op(eng, (lambda e: getattr(e, meth)(*a, **k)), reads, writes)

        def dma(eng, out_ap, in_ap, reads, writes, ds, **k):
            S.op(eng, (lambda e: e.dma_start(out=out_ap, in_=in_ap, **k)), reads, writes, dsem=ds)

        def pe_mm(mms, reads, writes):
            mms = list(mms)

            def fn(e):
                ins = None
                for (o, l, r, a, b) in mms:
                    ins = e.matmul(o, l, r, start=a, stop=b)
                return ins
            S.op("pe", fn, reads, writes)

        def pe_tr(trs, reads, writes):
            trs = list(trs)

            def fn(e):
                ins = None
                for (o, i_) in trs:
                    ins = e.transpose(o, i_, ident)
                return ins
            S.op("pe", fn, reads, writes)

        pbig = [st_all.enter_context(nc.psum_tensor("pb%d" % k, [128, 1024], F32)) for k in range(3)]
        pslots = []
        for k in range(3):
            for h in range(2):
                pslots.append((pbig[k], h, Reg("ps%d%d" % (k, h))))
        pstate = {"i": 0}

        def ps_next():
            t, h, r = pslots[pstate["i"] % 6]
            pstate["i"] += 1
            return t[:, h * 512:(h + 1) * 512], r

        def ps_pair():
            if pstate["i"] % 2 == 1:
                pstate["i"] += 1
            t, _, r0 = pslots[pstate["i"] % 6]
            _, _, r1 = pslots[(pstate["i"] + 1) % 6]
            pstate["i"] += 2
            return t, [r0, r1]

        ptr = Ring([(st_all.enter_context(nc.psum_tensor("pt%d" % k, [128, 8, 128], BF16)), Reg("pt")) for k in range(2)])

        cbf = sbuf(st_all, "cbf", [128, 640], BF16)
        ident = cbf[:, 0:128]
        Utri = cbf[:, 128:256]
        ones_bf = cbf[:, 256:384]
        mprev = cbf[:, 384:512]
        mnext = cbf[:, 512:640]
        cf32 = sbuf(st_all, "cf32", [128, 32 + NB + 9], F32)
        iota_e = cf32[:, 0:32]
        bstart = cf32[:, 32:32 + NB]
        pidx = cf32[:, 32 + NB:33 + NB]
        thr8 = cf32[:, 33 + NB:41 + NB]
        MR = sbuf(st_all, "MR", [128, 4, 1024], F32)
        bdown_bf = sbuf(st_all, "bdown_bf", [32, 1024], BF16)
        rank_all = sbuf(st_all, "rank_all", [128, NT * 4], F32)
        ek_all = sbuf(st_all, "ek_all", [128, NT * 4], F32)
        gk_all = sbuf(st_all, "gk_all", [128, NT * 4], F32)
        dest_i = sbuf(st_all, "dest_i", [128, NT * 4], I32)
        runc = sbuf(st_all, "runc", [128, 32], F32)
        eps_t = sbuf(st_all, "eps_t", [128, 1], F32)
        esink = sbuf(st_all, "esink", [128, 8], F32)
        A1 = sbuf(st_all, "A1", [128, 8, 2], F32)
        B1 = sbuf(st_all, "B1", [128, 8, 2], F32)
        colp = sbuf(st_all, "colp", [128, 16], F32)
        ones_f = sbuf(st_all, "ones_f", [1, 128], F32)
        widx = sbuf(st_all, "widx", [128, NB], I32)
        r_const = Reg("const")
        r_MR = Reg("MR")
        r_AB = Reg("AB")
        r_route = Reg("route")
        r_run = Reg("run")
        r_bdown = Reg("bdown")
        r_dest = Reg("dest")
        r_eblk = Reg("eblk")

        d_const = S.dsem("const")
        d_out = [S.dsem("out0"), S.dsem("out1")]

        st_mix = ExitStack()
        w_in_bf = sbuf(st_mix, "w_in_bf", [128, 8, 1792], BF16)
        w_out_bf = sbuf(st_mix, "w_out_bf", [128, 8, 1024], BF16)
        wr_bf = sbuf(st_mix, "wr_bf", [128, 8, 32], BF16)
        sguw_bf = sbuf(st_mix, "sguw_bf", [128, 1024], BF16)
        brow_bf = sbuf(st_mix, "brow_bf", [1, 3872], BF16)
        lngb = sbuf(st_mix, "lngb", [128, 1024], F32)
        kT2r = [(sbuf(st_mix, "kT2_%d" % k, [128, 2, 128], BF16), Reg("kT2")) for k in range(4)]
        vaugr = [(sbuf(st_mix, "vaug_%d" % k, [128, 2, 66], BF16), Reg("vaug")) for k in range(4)]
        kcT2 = sbuf(st_mix, "kcT2", [128, 2, 256], BF16)
        vcaug = sbuf(st_mix, "vcaug", [128, 2, 2, 66], BF16)
        r_kc = Reg("kc")
        r_vc = Reg("vc")
        r_w = Reg("wts")
        r_brow = Reg("brow")
        r_lngb = Reg("lngb")

        st0 = ExitStack()
        cstage = sbuf(st0, "cstage", [128, 640], F32)
        slab = mkring(st0, "slab", [128, 8, 512], F32, 2)
        bada_sb = sbuf(st0, "bada_sb", [1, 6144], F32)
        stg = mkring(st0, "stg", [128, 1792], F32, 2)
        browf_sb = sbuf(st0, "browf_sb", [1, 3872], F32)
        cc_sb = sbuf(st0, "cc_sb", [128, 8, 2], F32)
        sc_f = sbuf(st0, "sc_f", [128, 8, 2], F32)
        rep_f = sbuf(st0, "rep_f", [128, 8, 128], F32)
        modcol = sbuf(st0, "modcol", [128, 16, 2], F32)
        rows_bc = sbuf(st0, "rows_bc", [128, 3, 1024], F32)
        tmp8 = sbuf(st0, "tmp8", [128, 8, 2], F32)
        bdown_f = sbuf(st0, "bdown_f", [32, 1024], F32)
        wr_f = sbuf(st0, "wr_f", [128, 8, 32], F32)
        sguw_f = sbuf(st0, "sguw_f", [128, 1024], F32)
        sink_bc = sbuf(st0, "sink_bc", [128, 8], F32)
        r0 = {k: Reg(k) for k in ("cstage", "bada", "browf", "cc", "sc", "rep", "modcol", "rows", "tmp8",
                                  "bdown_f", "wr_f", "sguw_f", "sink")}

        dma("sp", cstage[:], cbf_d, [], [r0["cstage"]], d_const)
        dma("sp", cf32[:], cf32_d, [], [r_const], d_const)
        dma("sp", cc_sb[:].rearrange("p j s -> p (j s)"), cc, [], [r0["cc"]], d_const)
        dma("sp", bada_sb[:], b_ada, [], [r0["bada"]], d_const)
        dma("sp", browf_sb[:], browf, [], [r0["browf"]], d_const)
        dma("sp", rows_bc[:].rearrange("p a n -> p (a n)"), rowpack[0:1, 0:3072].partition_broadcast(128)[:, 0, :],
            [], [r0["rows"]], d_const)
        dma("sp", lngb[:], rowpack[0:1, 3072:4096].partition_broadcast(128)[:, 0, :], [], [r_lngb], d_const)
        dma("sp", sink_bc[:], rowpack[0:1, 4096:4104].partition_broadcast(128)[:, 0, :], [], [r0["sink"]], d_const)
        dma("sp", bdown_f[:], b_down, [], [r0["bdown_f"]], d_const)
        dma("sp", wr_f[:], w_r.rearrange("(j p) n -> p j n", p=128), [], [r0["wr_f"]], d_const)
        dma("sp", sguw_f[:], sguwT, [], [r0["sguw_f"]], d_const)
        d_const.seal()
        d_const2 = S.dsem("const2")
        dma("sp", colp[:], colpack, [], [r_const], d_const2)

        op1("pool", "memset", [], [r_const], eps_t[:], EPS)
        op1("pool", "memset", [], [r_const], ones_f[:], 1.0)
        op1("pool", "memset", [], [r_run], runc[:], 0.0)
        op1("act", "activation", [r0["cstage"]], [r_const], cbf[:], cstage[:], AF.Copy)
        op1("act", "activation", [r0["sink"]], [r_const], esink[:], sink_bc[:], AF.Exp)
        op1("act", "activation", [r0["cc"]], [r0["sc"]], sc_f[:], cc_sb[:], AF.Silu)
        op1("dve", "tensor_copy", [r0["sc"]], [r0["rep"]], rep_f[:], sc_f[:, :, 0:1].to_broadcast([128, 8, 128]))
        op1("dve", "tensor_copy", [r0["bdown_f"]], [r_bdown], bdown_bf[:], bdown_f[:])
        op1("dve", "tensor_copy", [r0["wr_f"]], [r_w], wr_bf[:], wr_f[:])
        op1("dve", "tensor_copy", [r0["sguw_f"]], [r_w], sguw_bf[:], sguw_f[:])
        op1("dve", "tensor_copy", [r0["browf"]], [r_brow], brow_bf[:], browf_sb[:])

        zt = sbuf(st0, "zt", [128, 4096], BF16)
        r_zt = Reg("zt")
        r_xs = Reg("xs_d")
        d_zero = S.dsem("zero")
        op1("pool", "memset", [], [r_zt], zt[:], 0.0)
        nz = NB * R // 512
        for k in range(nz):
            dma("sp", xs_d[k * 512:(k + 1) * 512, :].rearrange("(p a) n -> p (a n)", p=128), zt[:],
                [r_zt], [Reg("xsz")], d_zero)
        zero_ops = list(d_zero.ops)
        d_zero.seal()

        d_slab = [S.dsem("slab0"), S.dsem("slab1")]
        pcol, r_pcol = ps_next()
        for n in range(12):
            sl, r_sl = slab.next()
            dma("sp", sl[:], w_ada[:, n * 512:(n + 1) * 512].rearrange("(j p) n -> p j n", p=128),
                [], [r_sl], d_slab[n % 2])
            if n < 4:
                mms = []
                for fc in range(4):
                    ci = n * 4 + fc
                    o = pcol[:, ci * 2:ci * 2 + 2]
                    for j in range(8):
                        mms.append((o, sl[:, j, fc * 128:(fc + 1) * 128], sc_f[:, j, :], j == 0, False))
                    mms.append((o, bada_sb[0:1, ci * 128:(ci + 1) * 128], ones_f[0:1, 0:2], False, True))
                pe_mm(mms, [r_sl, r0["sc"], r0["bada"], r_const], [r_pcol])
                if n == 3:
                    op1("dve", "tensor_copy", [r_pcol], [r0["modcol"]],
                        modcol[:].rearrange("p a s -> p (a s)"), pcol[:, 0:32])
            else:
                pr, r_pr = ps_next()
                mms = [(pr, rep_f[:, j, :], sl[:, j, :], j == 0, False) for j in range(8)]
                mms.append((pr, ones_f[0:1, 0:128], bada_sb[0:1, n * 512:(n + 1) * 512], False, True))
                pe_mm(mms, [r_sl, r0["rep"], r0["bada"], r_const], [r_pr])
                q4 = (n - 4) // 2
                h4 = (n - 4) % 2
                op1("act", "activation", [r_pr], [r_MR], MR[:, q4, h4 * 512:(h4 + 1) * 512], pr, AF.Copy)
        op1("dve", "tensor_scalar", [r0["modcol"]], [r0["tmp8"]], tmp8[:], modcol[:, 8:16, :], 1.0, None, ALU.add)
        op1("dve", "tensor_tensor", [r0["tmp8"], r_const], [r_AB], A1[:], tmp8[:],
            colp[:, 0:8].unsqueeze(2).to_broadcast([128, 8, 2]), ALU.mult)
        op1("dve", "tensor_copy", [r0["modcol"]], [r_AB], B1[:], modcol[:, 0:8, :])
        op1("pool", "tensor_tensor", [r_MR, r0["rows"]], [r_MR], MR[:, 0, :], MR[:, 0, :], rows_bc[:, 0, :], ALU.mult)
        op1("dve", "scalar_tensor_tensor", [r_MR, r0["rows"]], [r_MR], MR[:, 2, :], MR[:, 2, :], 1.0,
            rows_bc[:, 1, :], ALU.add, ALU.mult)
        op1("pool", "tensor_tensor", [r_MR, r0["rows"]], [r_MR], MR[:, 3, :], MR[:, 3, :], rows_bc[:, 2, :], ALU.mult)

        d_stg = [S.dsem("stg0"), S.dsem("stg1")]
        for j in range(8):
            sg, r_sg = stg.next()
            dma("sp", sg[:], w_in[j * 128:(j + 1) * 128, :], [], [r_sg], d_stg[j % 2])
            op1("pool", "tensor_copy", [r_sg], [r_w], w_in_bf[:, j, :], sg[:])
        for j in range(8):
            sg, r_sg = stg.next()
            dma("sp", sg[:, 0:1024], w_out[j * 128:(j + 1) * 128, :], [], [r_sg], d_stg[j % 2])
            op1("pool", "tensor_copy", [r_sg], [r_w], w_out_bf[:, j, :], sg[:, 0:1024])

        allr0 = list(r0.values()) + [r_zt] + [r for (_, r) in slab.items] + [r for (_, r) in stg.items]
        for e in Sched.ENGS:
            S.op(e, None, reads=allr0)
        st0.close()

        st1 = ExitStack()
        xt_r = mkring(st1, "xt", [128, 1024], F32, 4)
        d_xt = [S.dsem("xt%d" % k) for k in range(4)]
        rp_r = mkring(st1, "rp", [128, 128], F32, 3)
        d_rp = [S.dsem("rp%d" % k) for k in range(3)]
        junk = sbuf(st1, "junk", [128, 1024], BF16)
        r_junk = Reg("junk")
        xs_r = mkring(st1, "xsb", [128, 1024], BF16, 2)
        hT_r = mkring(st1, "hT", [128, 8, 128], BF16, 2)
        qk_r = mkring(st1, "qk", [128, 768], BF16, 2)
        rA = sbuf(st1, "rA", [128, 512], F32)
        rB = sbuf(st1, "rB", [128, 512], F32)
        r_rA = Reg("rA")
        r_rB = Reg("rB")
        qT_r = mkring(st1, "qT", [128, 4, 128], BF16, 3)
        u_r = mkring(st1, "u", [128, 512], BF16, 2)
        gv_r = mkring(st1, "gv", [128, 512], F32, 2)
        z_r = mkring(st1, "z", [128, 512], F32, 2)
        vln_r = mkring(st1, "vln", [128, 512], BF16, 2)
        sgo_r = mkring(st1, "sgo", [128, 512], F32, 3)
        p_r = mkring(st1, "p", [128, 512], BF16, 7)
        ao_r = mkring(st1, "ao", [128, 512], F32, 2)
        on_r = mkring(st1, "on", [128, 1024], BF16, 2)
        oT_r = mkring(st1, "oT", [128, 8, 128], BF16, 2)
        tm_r = mkring(st1, "tm", [128, 1024], F32, 1)
        xm_r = mkring(st1, "xm", [128, 1024], F32, 2)
        d_xm = [S.dsem("xm0"), S.dsem("xm1")]
        h2f_r = mkring(st1, "h2f", [128, 1024], F32, 1)
        h2b_r = mkring(st1, "h2b", [128, 1024], BF16, 2)
        d_h2 = [S.dsem("h2b0"), S.dsem("h2b1")]
        h2T_r = mkring(st1, "h2T", [128, 8, 128], BF16, 2)
        st_r = mkring(st1, "stat", [128, 16], F32, 6)
        bn_r = mkring(st1, "bn", [128, 8], F32, 2)
        lg_r = mkring(st1, "lg", [128, 32], F32, 2)
        t8_r = mkring(st1, "t8", [128, 8], F32, 2)
        mk_r = mkring(st1, "mk", [128, 32], F32, 2)
        mkb_r = mkring(st1, "mkb", [128, 32], BF16, 2)
        ex_r = mkring(st1, "ex", [128, 32], F32, 2)
        oh_r = mkring(st1, "oh", [128, 4, 32], F32, 2)
        Dm_r = mkring(st1, "Dm", [128, 32], F32, 2)
        tq_r = mkring(st1, "tq", [128, 4, 32], F32, 2)
        r_h2d = Reg("h2_d")
        r_xmd = Reg("xmid_d")

        BIN, BOUT, BSGU, BRT = 0, 1792, 2816, 3840

        def rstd_from(ss_ap, out_ap, scale, r_in, r_out):
            op1("act", "activation", [r_in, r_const], [r_out], out_ap, ss_ap, AF.Sqrt, bias=eps_t[:], scale=scale)
            op1("dve", "reciprocal", [r_out], [r_out], out_ap, out_ap)

        def norm_T(src, r_src, colA, colB, s_idx, hT, r_hT, xsb, r_xsb, stt, r_stt):
            op1("act", "activation", [r_src], [r_junk, r_stt], junk[:], src, AF.Square, accum_out=stt[:, 0:1])
            rstd_from(stt[:, 0:1], stt[:, 1:2], 1.0 / 1024, r_stt, r_stt)
            op1("dve", "tensor_scalar", [r_src, r_stt], [r_xsb], xsb[:], src, stt[:, 1:2], None, ALU.mult)
            pt, r_pt = ptr.next()
            pe_tr([(pt[:, j, :], xsb[:, j * 128:(j + 1) * 128]) for j in range(8)], [r_xsb, r_const], [r_pt])
            for j in range(8):
                S.op("act", (lambda e, j=j: e.activation(hT[:, j, :], pt[:, j, :], AF.Identity,
                                                         bias=colB[:, j, s_idx:s_idx + 1],
                                                         scale=colA[:, j, s_idx:s_idx + 1])),
                     [r_pt, r_AB], [r_hT])

        op1("pool", "memset", [], [r_vc], vcaug[:], 1.0)
        for k in range(4):
            op1("pool", "memset", [], [vaugr[k][1]], vaugr[k][0][:], 1.0)
        for ci in range(2):
            xt, r_xt = xt_r.next()
            dma("sp", xt[:], ctx[ci * 128:(ci + 1) * 128, :], [], [r_xt], d_xt[(xt_r.i - 1) % 4])
            xsb, r_xsb = xs_r.next()
            hT, r_hT = hT_r.next()
            stt, r_stt = st_r.next()
            norm_T(xt[:], r_xt, A1, B1, 1, hT, r_hT, xsb, r_xsb, stt, r_stt)
            pk, r_pk = ps_next()
            mms = [(pk[:, 0:256], hT[:, j, :], w_in_bf[:, j, 1536:1792], j == 0, False) for j in range(8)]
            mms.append((pk[:, 0:256], ones_bf[0:1, :], brow_bf[0:1, BIN + 1536:BIN + 1792], False, True))
            pe_mm(mms, [r_hT, r_w, r_brow, r_const], [r_pk])
            qk, r_qk = qk_r.next()
            for dup in range(2):
                S.op("act", (lambda e, dup=dup, qk=qk, pk=pk: e.activation(
                    qk[:, 0:256].rearrange("p (g d c) -> p g d c", g=2, d=2)[:, :, dup, :],
                    pk[:, 0:128].rearrange("p (g c) -> p g c", g=2), AF.Copy)), [r_pk], [r_qk])
            op1("act", "activation", [r_pk], [r_vc], vcaug[:, ci, :, 0:64],
                pk[:, 128:256].rearrange("p (g c) -> p g c", g=2), AF.Copy)
            pt, r_pt = ptr.next()
            pe_tr([(pt[:, g, :], qk[:, g * 128:(g + 1) * 128]) for g in range(2)], [r_qk, r_const], [r_pt])
            op1("dve", "tensor_copy", [r_pt], [r_kc], kcT2[:, :, ci * 128:(ci + 1) * 128], pt[:, 0:2, :])

        if stop_after == 0:
            dbg_kc = nc.dram_tensor("dbg_kc", [128, 512], BF16, kind="ExternalOutput").ap()
            d_dbg = S.dsem("dbg")
            rr = Reg("dbgo")
            dma("sp", dbg_kc, kcT2[:].rearrange("p g k -> p (g k)"), [r_kc], [rr], d_dbg)
            o = S.op("sp", None, reads=[rr])
            o.deps.update(z.idx for z in zero_ops)
            S.run()
            st1.close()
            st_mix.close()
            return nc
        tiles = {}

        def rope(src, H, dst3, rp, reads, r_dst):
            X = src.rearrange("p (h c) -> p h c", h=H)
            n = H * 64
            Av = rA[:, 0:n].rearrange("p (h c) -> p h c", h=H)
            Bv = rB[:, 0:n].rearrange("p (h c) -> p h c", h=H)
            op1("dve", "tensor_tensor", reads, [r_rA], Av, X, rp[:, 0:64].unsqueeze(1).to_broadcast([128, H, 64]),
                ALU.mult)
            for ax in range(2):
                for hf in range(2):
                    o0 = ax * 32 + hf * 16
                    i0 = ax * 32 + (1 - hf) * 16
                    op1("dve", "tensor_tensor", reads, [r_rB], Bv[:, :, o0:o0 + 16], X[:, :, i0:i0 + 16],
                        rp[:, 64 + o0:64 + o0 + 16].unsqueeze(1).to_broadcast([128, H, 16]), ALU.mult)
            for d3 in dst3:
                op1("pool", "tensor_tensor", [r_rA, r_rB], [r_dst], d3, Av, Bv, ALU.add)

        def stageA(i):
            T = {}
            xt, r_xt = xt_r.next()
            dma("sp", xt[:], x[i * 128:(i + 1) * 128, :], [], [r_xt], d_xt[(xt_r.i - 1) % 4])
            rp, r_rp = rp_r.next()
            dma("sp", rp[:], rope_d[i * 128:(i + 1) * 128, :], [], [r_rp], d_rp[(rp_r.i - 1) % 3])
            T["xt"] = (xt, r_xt)
            xsb, r_xsb = xs_r.next()
            hT, r_hT = hT_r.next()
            stt, r_stt = st_r.next()
            norm_T(xt[:], r_xt, A1, B1, 0, hT, r_hT, xsb, r_xsb, stt, r_stt)
            chunks = []
            for (c0, cw) in ((0, 512), (512, 512), (1024, 512), (1536, 128), (1664, 128)):
                pc, r_pc = ps_next()
                mms = [(pc[:, 0:cw], hT[:, j, :], w_in_bf[:, j, c0:c0 + cw], j == 0, False) for j in range(8)]
                mms.append((pc[:, 0:cw], ones_bf[0:1, :], brow_bf[0:1, BIN + c0:BIN + c0 + cw], False, True))
                pe_mm(mms, [r_hT, r_w, r_brow, r_const], [r_pc])
                chunks.append((pc, r_pc))
            (pq, r_pq), (psu, r_psu), (psv, r_psv), (pkv, r_pkv), (pvv, r_pvv) = chunks
            qk, r_qk = qk_r.next()
            rope(pq, 8, [qk[:, 0:512].rearrange("p (h c) -> p h c", h=8)], rp, [r_pq, r_rp], r_qk)
            kd = qk[:, 512:768].rearrange("p (g d c) -> p g d c", g=2, d=2)
            rope(pkv[:, 0:128], 2, [kd[:, :, 0, :], kd[:, :, 1, :]], rp, [r_pkv, r_rp], r_qk)
            if SUB == 1:
                return
            va, r_va = vaugr[i % 4]
            if SUB == 7:
                op1("dve", "tensor_copy", [r_pkv], [r_va], va[:, :, 0:64],
                    pkv[:, 128:256].rearrange("p (g c) -> p g c", g=2))
                return
            if SUB == 71:
                op1("dve", "tensor_copy", [r_pkv], [r_va], va[:, :, 0:64],
                    pkv[:, 0:128].rearrange("p (g c) -> p g c", g=2))
                return
            if SUB == 72:
                op1("dve", "tensor_copy", [r_qk], [r_va], va[:, :, 0:64],
                    qk[:, 0:128].rearrange("p (g c) -> p g c", g=2))
                return
            if SUB == 73:
                op1("dve", "tensor_copy", [r_pkv], [r_va], va[:, 0, 0:64], pkv[:, 128:192])
                return
            if SUB == 74:
                op1("dve", "tensor_copy", [r_pkv], [r_va], va[:, :, 0:64],
                    pkv[:, 256:384].rearrange("p (g c) -> p g c", g=2))
                return
            if SUB == 75:
                op1("dve", "tensor_copy", [r_pkv], [r_va], va[:, :, 0:64],
                    pkv[:, 64:192].rearrange("p (g c) -> p g c", g=2))
                return
            if SUB == 76:
                op1("dve", "tensor_copy", [r_pkv], [r_va], va[:, 0, 0:64], pkv[:, 192:256])
                return
            if SUB == 77:
                op1("dve", "tensor_copy", [r_pq], [r_va], va[:, 0, 0:64], pq[:, 192:256])
                return
            if SUB == 78:
                op1("dve", "tensor_copy", [r_pq], [r_va], va[:, 0, 0:64], pq[:, 448:512])
                return
            if SUB == 8:
                op1("act", "activation", [r_pkv], [r_junk], junk[:, 0:128].rearrange("p (g c) -> p g c", g=2),
                    pkv[:, 128:256].rearrange("p (g c) -> p g c", g=2), AF.Copy)
                return
            if SUB == 9:
                op1("act", "activation", [r_pkv], [r_va], va[:, :, 0:64],
                    pkv[:, 128:256].rearrange("p (g c) -> p g c", g=2), AF.Identity)
                return
            op1("act", "activation", [r_pvv], [r_va], va[:, :, 0:64],
                pvv[:, 0:128].rearrange("p (g c) -> p g c", g=2), AF.Copy)
            if SUB == 4:
                return
            pt, r_pt = ptr.next()
            pe_tr([(pt[:, a, :], qk[:, a * 128:(a + 1) * 128]) for a in range(6)], [r_qk, r_const], [r_pt])
            if SUB == 5:
                return
            qT, r_qT = qT_r.next()
            kT, r_kT = kT2r[i % 4]
            op1("dve", "tensor_copy", [r_pt], [r_qT], qT[:], pt[:, 0:4, :])
            if SUB == 6:
                return
            op1("dve", "tensor_copy", [r_pt], [r_kT], kT[:], pt[:, 4:6, :])
            T["qT"] = (qT, r_qT)
            if SUB == 2:
                return
            u, r_u = u_r.next()
            op1("act", "activation", [r_psu], [r_u], u[:], psu, AF.Gelu_apprx_tanh)
            gv, r_gv = gv_r.next()
            op1("act", "activation", [r_psv], [r_gv], gv[:], psv, AF.Gelu_apprx_tanh)
            bn, r_bn = bn_r.next()
            op1("dve", "bn_stats", [r_gv], [r_bn], bn[:, 0:6], gv[:])
            op1("dve", "bn_aggr", [r_bn], [r_bn], bn[:, 6:8], bn[:, 0:6])
            rstd_from(bn[:, 7:8], bn[:, 7:8], 1.0, r_bn, r_bn)
            z, r_z = z_r.next()
            op1("dve", "tensor_scalar", [r_gv, r_bn], [r_z], z[:], gv[:], bn[:, 6:7], bn[:, 7:8], ALU.subtract, ALU.mult)
            op1("pool", "tensor_tensor", [r_z, r_lngb], [r_z], z[:], z[:], lngb[:, 0:512], ALU.mult)
            vln, r_vln = vln_r.next()
            op1("pool", "tensor_tensor", [r_z, r_lngb], [r_vln], vln[:], z[:], lngb[:, 512:1024], ALU.add)
            if SUB == 3:
                return
            pm, r_pm = ps_next()
            mms = []
            for h in range(8):
                o = pm[:, h * 64:(h + 1) * 64]
                mms.append((o, sguw_bf[:, h * 128:(h + 1) * 128], vln[:, h * 64:(h + 1) * 64], True, False))
                mms.append((o, brow_bf[0:1, BSGU + h * 128:BSGU + (h + 1) * 128], ones_bf[0:1, 0:64], False, True))
            pe_mm(mms, [r_vln, r_w, r_brow, r_const], [r_pm])
            sgo, r_sgo = sgo_r.next()
            op1("dve", "tensor_tensor", [r_pm, r_u], [r_sgo], sgo[:], pm, u[:], ALU.mult)
            T["sgo"] = (sgo, r_sgo)
            tiles[i] = T

        SCALE = 64 ** -0.5

        def stageB(i):
            T = tiles.pop(i)
            xt, r_xt = T["xt"]
            qT, r_qT = T["qT"]
            sgo, r_sgo = T["sgo"]
            ao, r_ao = ao_r.next()
            for g in range(2):
                klist = []
                if i > 0:
                    klist.append(("p", kT2r[(i - 1) % 4], vaugr[(i - 1) % 4], None))
                klist.append(("o", kT2r[i % 4], vaugr[i % 4], None))
                if i < NT - 1:
                    klist.append(("n", kT2r[(i + 1) % 4], vaugr[(i + 1) % 4], None))
                klist.append(("c", (kcT2, r_kc), (vcaug, r_vc), 0))
                klist.append(("c", (kcT2, r_kc), (vcaug, r_vc), 1))
                plist = []
                for (kind, (kt, r_kt), (vt, r_vt), ci) in klist:
                    pss, r_pss = ps_pair()
                    mms = []
                    for h4 in range(4):
                        h = 4 * g + h4
                        a, hf = h // 2, h % 2
                        if kind == "c":
                            ks = kt[hf * 64:(hf + 1) * 64, g, ci * 128:(ci + 1) * 128]
                        else:
                            ks = kt[hf * 64:(hf + 1) * 64, g, :]
                        oc = hf * 512 + (h4 // 2) * 128
                        mms.append((pss[:, oc:oc + 128], ks, qT[hf * 64:(hf + 1) * 64, a, :], True, True))
                    pe_mm(mms, [r_kt, r_qT], r_pss)
                    pp, r_pp = p_r.next()
                    op1("act", "activation", r_pss, [r_pp], pp[:].rearrange("p (b c) -> p b c", b=2),
                        pss[:].rearrange("p (b c) -> p b c", b=2)[:, :, 0:256], AF.Exp, scale=SCALE)
                    if kind in ("p", "n"):
                        mk = mprev if kind == "p" else mnext
                        op1("pool", "tensor_tensor", [r_pp, r_const], [r_pp],
                            pp[:].rearrange("p (h q) -> p h q", h=4), pp[:].rearrange("p (h q) -> p h q", h=4),
                            mk.unsqueeze(1).to_broadcast([128, 4, 128]), ALU.mult)
                    if kind == "c":
                        vs = vt[:, ci, g, 0:65]
                    else:
                        vs = vt[:, g, 0:65]
                    plist.append((pp, r_pp, vs, r_vt))
                if SUB == 21:
                    return
                po, r_po = ps_next()
                mms = []
                rd = []
                for h4 in range(4):
                    for k, (pp, r_pp, vs, r_vt) in enumerate(plist):
                        pc_ = (h4 % 2) * 256 + (h4 // 2) * 128
                        mms.append((po[:, h4 * 66:h4 * 66 + 65], pp[:, pc_:pc_ + 128], vs,
                                    k == 0, k == len(plist) - 1))
                        rd += [r_pp, r_vt]
                pe_mm(mms, rd, [r_po])
                stt, r_stt = st_r.next()
                po3 = po[:, 0:264].rearrange("p (h c) -> p h c", h=4)
                op1("dve", "tensor_tensor", [r_po, r_const], [r_stt], stt[:, 0:4], po3[:, :, 64], esink[:, 4 * g:4 * g + 4],
                    ALU.add)
                op1("dve", "reciprocal", [r_stt], [r_stt], stt[:, 4:8], stt[:, 0:4])
                op1("dve", "tensor_tensor", [r_po, r_stt], [r_ao],
                    ao[:, g * 256:(g + 1) * 256].rearrange("p (h c) -> p h c", h=4), po3[:, :, 0:64],
                    stt[:, 4:8].unsqueeze(2).to_broadcast([128, 4, 64]), ALU.mult)
                if SUB == 22:
                    return
            if SUB == 23:
                return
            stt, r_stt = st_r.next()
            op1("act", "activation", [r_ao], [r_junk, r_stt], junk[:, 0:512], ao[:], AF.Square, accum_out=stt[:, 0:1])
            op1("act", "activation", [r_sgo], [r_junk, r_stt], junk[:, 512:1024], sgo[:], AF.Square,
                accum_out=stt[:, 1:2])
            rstd_from(stt[:, 0:2], stt[:, 2:4], 1.0 / 512, r_stt, r_stt)
            on, r_on = on_r.next()
            op1("dve", "tensor_scalar", [r_ao, r_stt], [r_on], on[:, 0:512], ao[:], stt[:, 2:3], None, ALU.mult)
            op1("pool", "tensor_scalar", [r_sgo, r_stt], [r_on], on[:, 512:1024], sgo[:], stt[:, 3:4], None, ALU.mult)
            pt, r_pt = ptr.next()
            pe_tr([(pt[:, j, :], on[:, j * 128:(j + 1) * 128]) for j in range(8)], [r_on, r_const], [r_pt])
            oT, r_oT = oT_r.next()
            op1("dve", "tensor_tensor", [r_pt, r_const], [r_oT], oT[:], pt[:],
                colp[:, 8:16].unsqueeze(2).to_broadcast([128, 8, 128]), ALU.mult)
            if SUB == 24:
                return
            pmix, r_pmix = ps_pair()
            mms = []
            for hh in range(2):
                o = pmix[:, hh * 512:(hh + 1) * 512]
                for j in range(8):
                    mms.append((o, oT[:, j, :], w_out_bf[:, j, hh * 512:(hh + 1) * 512], j == 0, False))
                mms.append((o, ones_bf[0:1, :], brow_bf[0:1, BOUT + hh * 512:BOUT + (hh + 1) * 512], False, True))
            pe_mm(mms, [r_oT, r_w, r_brow, r_const], r_pmix)
            stt, r_stt = st_r.next()
            op1("act", "activation", r_pmix, [r_junk, r_stt], junk[:], pmix[:], AF.Square, accum_out=stt[:, 0:1])
            rstd_from(stt[:, 0:1], stt[:, 1:2], 1.0 / 1024, r_stt, r_stt)
            tm, r_tm = tm_r.next()
            op1("dve", "scalar_tensor_tensor", r_pmix + [r_stt, r_MR], [r_tm], tm[:], pmix[:], stt[:, 1:2], MR[:, 0, :],
                ALU.mult, ALU.mult)
            xm, r_xm = xm_r.next()
            kx = (xm_r.i - 1) % 2
            op1("pool", "tensor_tensor", [r_tm, r_xt], [r_xm], xm[:], tm[:], xt[:], ALU.add)
            dma("sp", xmid_d[i * 128:(i + 1) * 128, :], xm[:], [r_xm], [r_xmd], d_xm[kx])
            if SUB == 25:
                return
            op1("act", "activation", [r_xm], [r_junk, r_stt], junk[:], xm[:], AF.Square, accum_out=stt[:, 2:3])
            rstd_from(stt[:, 2:3], stt[:, 3:4], 1.0 / 1024, r_stt, r_stt)
            h2f, r_h2f = h2f_r.next()
            op1("dve", "scalar_tensor_tensor", [r_xm, r_stt, r_MR], [r_h2f], h2f[:], xm[:], stt[:, 3:4], MR[:, 2, :],
                ALU.mult, ALU.mult)
            h2b, r_h2b = h2b_r.next()
            kh = (h2b_r.i - 1) % 2
            op1("pool", "tensor_tensor", [r_h2f, r_MR], [r_h2b], h2b[:], h2f[:], MR[:, 1, :], ALU.add)
            dma("sp", h2_d[i * 128:(i + 1) * 128, :], h2b[:], [r_h2b], [r_h2d], d_h2[kh])
            if SUB == 26:
                return
            pt, r_pt = ptr.next()
            pe_tr([(pt[:, j, :], h2b[:, j * 128:(j + 1) * 128]) for j in range(8)], [r_h2b, r_const], [r_pt])
            h2T, r_h2T = h2T_r.next()
            op1("act", "activation", [r_pt], [r_h2T], h2T[:], pt[:], AF.Copy)
            pl, r_pl = ps_next()
            mms = [(pl[:, 0:32], h2T[:, j, :], wr_bf[:, j, :], j == 0, False) for j in range(8)]
            mms.append((pl[:, 0:32], ones_bf[0:1, :], brow_bf[0:1, BRT:BRT + 32], False, True))
            pe_mm(mms, [r_h2T, r_w, r_brow, r_const], [r_pl])
            lg, r_lg = lg_r.next()
            op1("dve", "tensor_copy", [r_pl], [r_lg], lg[:], pl[:, 0:32])
            t8, r_t8 = t8_r.next()
            op1("dve", "max", [r_lg], [r_t8], t8[:], lg[:])
            if SUB == 27:
                return
            mk, r_mk = mk_r.next()
            op1("dve", "tensor_scalar", [r_lg, r_t8], [r_mk], mk[:], lg[:], t8[:, 3:4], None, ALU.is_ge)
            mkb, r_mkb = mkb_r.next()
            op1("dve", "tensor_copy", [r_mk], [r_mkb], mkb[:], mk[:])
            ex, r_ex = ex_r.next()
            op1("dve", "tensor_scalar", [r_t8], [r_ex], ex[:, 0:4], t8[:, 0:4], t8[:, 0:1], None, ALU.subtract)
            op1("act", "activation", [r_ex], [r_ex], ex[:, 4:8], ex[:, 0:4], AF.Exp)
            op1("dve", "tensor_reduce", [r_ex], [r_ex], ex[:, 8:9], ex[:, 4:8], AX.X, ALU.add)
            op1("dve", "reciprocal", [r_ex], [r_ex], ex[:, 9:10], ex[:, 8:9])
            op1("dve", "tensor_scalar", [r_ex], [r_route], gk_all[:, i * 4:(i + 1) * 4], ex[:, 4:8], ex[:, 9:10], None,
                ALU.mult)
            oh, r_oh = oh_r.next()
            for k in range(4):
                op1("dve", "tensor_scalar", [r_lg, r_t8], [r_oh], oh[:, k, :], lg[:], t8[:, k:k + 1], None, ALU.is_equal)
            pc2, r_pc2 = ps_next()
            pe_mm([(pc2[:, 0:32], Utri, mkb[:], True, True), (pc2[:, 32:64], ones_bf, mkb[:], True, True)],
                  [r_mkb, r_const], [r_pc2])
            Dm, r_Dm = Dm_r.next()
            op1("dve", "tensor_tensor", [r_pc2, r_run], [r_Dm], Dm[:], pc2[:, 0:32], runc[:], ALU.add)
            op1("dve", "tensor_tensor", [r_pc2, r_run], [r_run], runc[:], runc[:], pc2[:, 32:64], ALU.add)
            tq, r_tq = tq_r.next()
            op1("dve", "tensor_tensor", [r_oh, r_Dm], [r_tq], tq[:], oh[:], Dm[:].unsqueeze(1).to_broadcast([128, 4, 32]),
                ALU.mult)
            op1("dve", "tensor_reduce", [r_tq], [r_route], rank_all[:, i * 4:(i + 1) * 4], tq[:], AX.X, ALU.add)
            op1("dve", "tensor_tensor", [r_oh, r_const], [r_tq], tq[:], oh[:],
                iota_e.unsqueeze(1).to_broadcast([128, 4, 32]), ALU.mult)
            op1("dve", "tensor_reduce", [r_tq], [r_route], ek_all[:, i * 4:(i + 1) * 4], tq[:], AX.X, ALU.add)

        def all_p1_regs():
            rr_ = [r_junk, r_rA, r_rB, r_kc, r_vc, r_w, r_brow, r_lngb, r_AB, r_route, r_run, r_xmd, r_h2d]
            for ring in (xt_r, rp_r, xs_r, hT_r, qk_r, qT_r, u_r, gv_r, z_r, vln_r, sgo_r, p_r, ao_r, on_r, oT_r, tm_r,
                         xm_r, h2f_r, h2b_r, h2T_r, st_r, bn_r, lg_r, t8_r, mk_r, mkb_r, ex_r, oh_r, Dm_r, tq_r, ptr):
                rr_ += [r for (_, r) in ring.items]
            rr_ += [r for (_, r) in kT2r] + [r for (_, r) in vaugr] + [r for (_, _, r) in pslots]
            return rr_

        def finish_dbg():
            o = S.op("sp", None, reads=all_p1_regs())
            o.deps.update(z.idx for z in zero_ops)
            S.run()
            st1.close()
            st_mix.close()
            return nc

        stageA(0)
        if stop_after == 10:
            return finish_dbg()
        for i in range(1, NT):
            stageA(i)
            if stop_after == 11:
                return finish_dbg()
            stageB(i - 1)
            if stop_after == 12:
                return finish_dbg()
        stageB(NT - 1)

        allr1 = [r_junk, r_rA, r_rB, r_kc, r_vc, r_w, r_brow, r_lngb, r_AB]
        for ring in (xt_r, rp_r, xs_r, hT_r, qk_r, qT_r, u_r, gv_r, z_r, vln_r, sgo_r, p_r, ao_r, on_r, oT_r, tm_r,
                     xm_r, h2f_r, h2b_r, h2T_r, st_r, bn_r, lg_r, t8_r, mk_r, mkb_r, ex_r, oh_r, Dm_r, tq_r, ptr):
            allr1 += [r for (_, r) in ring.items]
        allr1 += [r for (_, r) in kT2r] + [r for (_, r) in vaugr] + [r for (_, _, r) in pslots]
        for e in Sched.ENGS:
            S.op(e, None, reads=allr1)
        if stop_after == 1:
            d_dbg = S.dsem("dbg")
            rr = [Reg("dbgo") for _ in range(4)]
            n4 = NT * 4
            dma("sp", dbg_route[:, 0:n4], rank_all[:], [r_route], [rr[0]], d_dbg)
            dma("sp", dbg_route[:, n4:2 * n4], ek_all[:], [r_route], [rr[1]], d_dbg)
            dma("sp", dbg_route[:, 2 * n4:3 * n4], gk_all[:], [r_route], [rr[2]], d_dbg)
            dma("sp", dbg_route[:, 3 * n4:3 * n4 + 32], runc[:], [r_run], [rr[3]], d_dbg)
            d_dbg.seal()
            o = S.op("sp", None, reads=rr + [r_xmd, r_h2d])
            o.deps.update(z.idx for z in zero_ops)
            S.run()
            st1.close()
            st_mix.close()
            return nc
        st1.close()
        st_mix.close()

        st2 = ExitStack()
        ci_ = sbuf(st2, "ci_", [128, 32], I32)
        pf = sbuf(st2, "pf", [128, 32], F32)
        cs0 = sbuf(st2, "cs0", [128, 32], F32)
        cs1 = sbuf(st2, "cs1", [128, 32], F32)
        pst = sbuf(st2, "pst", [128, 32], F32)
        cmp8 = sbuf(st2, "cmp8", [128, 32, 8], F32)
        cmpb = sbuf(st2, "cmpb", [128, NB, 32], F32)
        ebf = sbuf(st2, "ebf", [128, NB], F32)
        samef = sbuf(st2, "samef", [128, NB], F32)
        ohb = sbuf(st2, "ohb", [128, NT * 4, 32], F32)
        psel = sbuf(st2, "psel", [128, NT * 4], F32)
        r_b = Reg("book")
        LOG2R = R.bit_length() - 1
        op1("dve", "tensor_tensor", [r_run, r_const], [r_b], cmp8[:], runc[:].unsqueeze(2).to_broadcast([128, 32, 8]),
            thr8.unsqueeze(1).to_broadcast([128, 32, 8]), ALU.is_gt)
        op1("dve", "tensor_reduce", [r_b], [r_b], pf[:], cmp8[:], AX.X, ALU.add)
        op1("dve", "tensor_scalar", [r_b], [r_b], pf[:], pf[:], float(R), None, ALU.mult)
        op1("dve", "tensor_copy", [r_b], [r_b], cs0[:], pf[:])
        cur, nxt = cs0, cs1
        for s in (1, 2, 4, 8, 16):
            op1("dve", "tensor_copy", [r_b], [r_b], nxt[:, 0:s], cur[:, 0:s])
            op1("dve", "tensor_tensor", [r_b], [r_b], nxt[:, s:32], cur[:, s:32], cur[:, 0:32 - s], ALU.add)
            cur, nxt = nxt, cur
        pend = cur
        op1("dve", "tensor_tensor", [r_b], [r_b], pst[:], pend[:], pf[:], ALU.subtract)
        op1("dve", "tensor_tensor", [r_b, r_const], [r_b], cmpb[:], pend[:].unsqueeze(1).to_broadcast([128, NB, 32]),
            bstart.unsqueeze(2).to_broadcast([128, NB, 32]), ALU.is_le)
        op1("dve", "tensor_reduce", [r_b], [r_b], ebf[:], cmpb[:], AX.X, ALU.add)
        op1("dve", "tensor_scalar", [r_b], [r_b], ebf[:], ebf[:], 31.0, None, ALU.min)
        op1("dve", "tensor_tensor", [r_b], [r_b], samef[:, 2:NB], ebf[:, 2:NB], ebf[:, 0:NB - 2], ALU.is_equal)
        op1("dve", "tensor_scalar", [r_b, r_const], [r_b], ebf[:], ebf[:], 128.0, pidx, ALU.mult, ALU.add)
        op1("dve", "scalar_tensor_tensor", [r_b], [r_b], ebf[:, 2:NB], samef[:, 2:NB], 1.0e6, ebf[:, 2:NB],
            ALU.mult, ALU.add)
        op1("dve", "tensor_copy", [r_b], [r_eblk], widx[:], ebf[:])
        op1("dve", "tensor_tensor", [r_route, r_const], [r_b], ohb[:],
            ek_all[:].unsqueeze(2).to_broadcast([128, NT * 4, 32]),
            iota_e.unsqueeze(1).to_broadcast([128, NT * 4, 32]), ALU.is_equal)
        op1("dve", "tensor_tensor", [r_b], [r_b], ohb[:], ohb[:], pst[:].unsqueeze(1).to_broadcast([128, NT * 4, 32]),
            ALU.mult)
        op1("dve", "tensor_reduce", [r_b], [r_b], psel[:], ohb[:], AX.X, ALU.add)
        op1("dve", "tensor_tensor", [r_b, r_route], [r_b], psel[:], psel[:], rank_all[:], ALU.add)
        op1("dve", "tensor_copy", [r_b], [r_dest], dest_i[:], psel[:])

        h2l_r = mkring(st2, "h2l", [128, 1024], BF16, 3)
        d_h2l = [S.dsem("h2l%d" % k) for k in range(3)]
        d_sc = [S.dsem("scatter%d" % k) for k in range(3)]
        for i in range(NT):
            hl, r_hl = h2l_r.next()
            dma("sp", hl[:], h2_d[i * 128:(i + 1) * 128, :], [r_h2d], [r_hl], d_h2l[i % 3])
            for k in range(4):
                c = i * 4 + k
                o = S.op("pool", (lambda e, hl=hl, c=c: e.indirect_dma_start(
                    out=xs_d, out_offset=bass.IndirectOffsetOnAxis(ap=dest_i[:, c:c + 1], axis=0),
                    in_=hl[:], in_offset=None)),
                    [r_hl, r_dest], [Reg("xs_sc")], dsem=d_sc[i % 3])
                o.deps.update(z.idx for z in zero_ops)
        scatter_ops = []
        for d in d_sc:
            scatter_ops += d.ops
        if stop_after == 2:
            d_dbg = S.dsem("dbg")
            rr = [Reg("dbgo") for _ in range(2)]
            dma("sp", dbg_book[:, 0:NT * 4], dest_i[:], [r_dest], [rr[0]], d_dbg)
            dma("sp", dbg_book[:, NT * 4:NT * 4 + NB], widx[:], [r_eblk], [rr[1]], d_dbg)
            d_dbg.seal()
            o = S.op("sp", None, reads=rr)
            o.deps.update(z.idx for z in scatter_ops)
            S.run()
            st2.close()
            return nc
        for e in Sched.ENGS:
            S.op(e, None, reads=[r_b] + [r for (_, r) in h2l_r.items])
        st2.close()

        st3 = ExitStack()
        wgu_r = mkring(st3, "wgu", [128, 8, 2048], BF16, 2)
        wd_r = mkring(st3, "wd", [128, 8, 1024], BF16, 2)
        bg_r = mkring(st3, "bg", [128, 16], F32, 2)
        d_wgu = [S.dsem("wgu0"), S.dsem("wgu1")]
        d_wd = [S.dsem("wd0"), S.dsem("wd1")]
        d_bg = [S.dsem("bg0"), S.dsem("bg1")]
        xb_r = mkring(st3, "xb", [128, RG, 1024], BF16, 2)
        d_xb = [S.dsem("xb0"), S.dsem("xb1")]
        xbT_r = mkring(st3, "xbT", [128, 8, R], BF16, 2)
        aT_r = mkring(st3, "aT", [128, 8, R], BF16, 2)
        gc_r = mkring(st3, "gc", [128, R], F32, 2)
        uc_r = mkring(st3, "uc", [128, R], F32, 2)
        sg_r = mkring(st3, "sg", [128, R], F32, 2)
        yo_r = mkring(st3, "yo", [128, 1024], F32, 3)
        d_yo = [S.dsem("yo%d" % k) for k in range(3)]
        r_ys = Reg("ys_d")
        breg = {}

        def wbound(e):
            if "r" not in breg:
                breg["r"] = e.to_reg(4095)
            return breg["r"]
        wg_regs = [[Reg("wgk") for _ in range(8)] for _ in range(2)]
        wd_regs = [[Reg("wdk") for _ in range(8)] for _ in range(2)]

        for b in range(NB):
            kb = b % 2
            wg, r_wg = wgu_r.next()
            wdn, r_wdn = wd_r.next()
            bg, r_bg = bg_r.next()
            r_wgk = wg_regs[kb]
            r_wdk = wd_regs[kb]

            for k in range(8):
                S.op("pool", (lambda e, b=b, wg=wg, k=k: e.indirect_dma_start(
                    out=wg[:, k, :], out_offset=None, in_=wgu[k],
                    in_offset=bass.IndirectOffsetOnAxis(ap=widx[:, b:b + 1], axis=0),
                    bounds_check=wbound(e), oob_is_err=False)), [r_eblk], [r_wgk[k]], dsem=d_wgu[kb])
            for k in range(8):
                S.op("pool", (lambda e, b=b, wdn=wdn, k=k: e.indirect_dma_start(
                    out=wdn[:, k, :], out_offset=None, in_=wd[k],
                    in_offset=bass.IndirectOffsetOnAxis(ap=widx[:, b:b + 1], axis=0),
                    bounds_check=wbound(e), oob_is_err=False)), [r_eblk], [r_wdk[k]], dsem=d_wd[kb])
            S.op("pool", (lambda e, b=b, bg=bg: e.indirect_dma_start(
                out=bg[:], out_offset=None, in_=bgu,
                in_offset=bass.IndirectOffsetOnAxis(ap=widx[:, b:b + 1], axis=0),
                    bounds_check=wbound(e), oob_is_err=False)), [r_eblk], [r_bg], dsem=d_bg[kb])
            xb, r_xb = xb_r.next()
            o = S.op("sp", (lambda e, xb=xb, b=b: e.dma_start(
                out=xb[:], in_=xs_d[b * R:(b + 1) * R, :].rearrange("(g p) n -> p g n", p=128))),
                [], [r_xb], dsem=d_xb[kb])
            o.deps.update(z.idx for z in scatter_ops)
            xbT, r_xbT = xbT_r.next()
            for rg in range(RG):
                pt, r_pt = ptr.next()
                pe_tr([(pt[:, j, :], xb[:, rg, j * 128:(j + 1) * 128]) for j in range(8)], [r_xb, r_const], [r_pt])
                op1("act", "activation", [r_pt], [r_xbT], xbT[:, :, rg * 128:(rg + 1) * 128], pt[:], AF.Copy)
            aT, r_aT = aT_r.next()
            for j in range(8):
                pg, r_pg = ps_next()
                pu, r_pu = ps_next()
                pe_mm([(pg[:, 0:R], wg[:, k, j * 128:(j + 1) * 128], xbT[:, k, :], k == 0, k == 7) for k in range(8)],
                      r_wgk + [r_xbT], [r_pg])
                pe_mm([(pu[:, 0:R], wg[:, k, 1024 + j * 128:1024 + (j + 1) * 128], xbT[:, k, :], k == 0, k == 7)
                       for k in range(8)], r_wgk + [r_xbT], [r_pu])
                gc, r_gc = gc_r.next()
                uc, r_uc = uc_r.next()
                sg, r_sg = sg_r.next()
                op1("dve", "tensor_scalar", [r_pg, r_bg], [r_gc], gc[:], pg[:, 0:R], bg[:, j:j + 1], 7.0, ALU.add, ALU.min)
                op1("act", "activation", [r_pu, r_bg], [r_uc], uc[:], pu[:, 0:R], AF.Identity, bias=bg[:, 8 + j:9 + j])
                op1("act", "activation", [r_gc], [r_sg], sg[:], gc[:], AF.Sigmoid, scale=1.702)
                op1("dve", "tensor_scalar", [r_uc], [r_uc], uc[:], uc[:], 7.0, -7.0, ALU.min, ALU.max)
                op1("dve", "tensor_tensor", [r_gc, r_sg], [r_sg], sg[:], gc[:], sg[:], ALU.mult)
                op1("dve", "scalar_tensor_tensor", [r_uc, r_sg], [r_aT], aT[:, j, :], uc[:], 1.0, sg[:], ALU.add, ALU.mult)
            for rg in range(RG):
                yo, r_yo = yo_r.next()
                ky = (yo_r.i - 1) % 3
                for hh in range(2):
                    py, r_py = ps_next()
                    pe_mm([(py, aT[:, k, rg * 128:(rg + 1) * 128], wdn[:, k, hh * 512:(hh + 1) * 512], k == 0, k == 7)
                           for k in range(8)], [r_aT] + r_wdk, [r_py])
                    op1("act", "activation", [r_py], [r_yo], yo[:, hh * 512:(hh + 1) * 512], py, AF.Copy)
                dma("sp", ys_d[b * R + rg * 128:b * R + (rg + 1) * 128, :], yo[:], [r_yo], [Reg("ys_w")], d_yo[ky])
        ys_ops = []
        for d in d_yo:
            ys_ops += d.ops
        allr3 = []
        for ring in (wgu_r, wd_r, bg_r, xb_r, xbT_r, aT_r, gc_r, uc_r, sg_r, yo_r, ptr):
            allr3 += [r for (_, r) in ring.items]
        allr3 += [r for (_, _, r) in pslots]
        for kk in range(2):
            allr3 += wg_regs[kk] + wd_regs[kk]
        for e in Sched.ENGS:
            o = S.op(e, None, reads=allr3)
            o.deps.update(z.idx for z in ys_ops)
        st3.close()

        st4 = ExitStack()
        yg_r = mkring(st4, "yg", [128, 4, 1024], F32, 2)
        d_yg = [S.dsem("yg0"), S.dsem("yg1")]
        xl_r = mkring(st4, "xl", [128, 1024], F32, 2)
        d_xl = [S.dsem("xl0"), S.dsem("xl1")]
        acc_r = mkring(st4, "acc", [128, 1024], F32, 2)
        ob_r = mkring(st4, "ob", [128, 1024], F32, 2)
        G_r = mkring(st4, "G", [128, 32], F32, 2)
        Gb_r = mkring(st4, "Gb", [128, 32], BF16, 2)
        GT_r = mkring(st4, "GT", [32, 128], BF16, 2)
        toh_r = mkring(st4, "toh", [128, 32], F32, 2)
        st4_r = mkring(st4, "st4", [128, 4], F32, 2)
        junk4 = sbuf(st4, "junk4", [128, 1024], BF16)
        r_junk4 = Reg("junk4")
        out_regs = []
        yg_regs = [[Reg("ygk") for _ in range(4)] for _ in range(2)]
        for i in range(NT):
            yg, r_yg0 = yg_r.next()
            ky = i % 2
            r_ygk = yg_regs[ky]
            r_yg = r_ygk[0]
            for k in range(4):
                c = i * 4 + k
                S.op("pool", (lambda e, yg=yg, k=k, c=c: e.indirect_dma_start(
                    out=yg[:, k, :], out_offset=None, in_=ys_d,
                    in_offset=bass.IndirectOffsetOnAxis(ap=dest_i[:, c:c + 1], axis=0))), [r_dest, r_ys], [r_ygk[k]],
                    dsem=d_yg[ky])
            xl, r_xl = xl_r.next()
            dma("sp", xl[:], xmid_d[i * 128:(i + 1) * 128, :], [r_xmd], [r_xl], d_xl[ky])
            G, r_G = G_r.next()
            toh, r_toh = toh_r.next()
            for k in range(4):
                c = i * 4 + k
                op1("dve", "tensor_scalar", [r_route, r_const], [r_toh], toh[:], iota_e, ek_all[:, c:c + 1],
                    None, ALU.is_equal)
                op1("dve", "tensor_scalar", [r_route, r_toh], [r_toh], toh[:], toh[:], gk_all[:, c:c + 1],
                    None, ALU.mult)
                if k == 0:
                    op1("dve", "tensor_copy", [r_toh], [r_G], G[:], toh[:])
                else:
                    op1("dve", "tensor_tensor", [r_toh, r_G], [r_G], G[:], G[:], toh[:], ALU.add)
            Gb, r_Gb = Gb_r.next()
            op1("dve", "tensor_copy", [r_G], [r_Gb], Gb[:], G[:])
            pt, r_pt = ptr.next()
            pe_tr([(pt[0:32, 0, :], Gb[:])], [r_Gb, r_const], [r_pt])
            GT, r_GT = GT_r.next()
            op1("act", "activation", [r_pt], [r_GT], GT[:], pt[0:32, 0, :], AF.Copy)
            pbd, r_pbd = ps_pair()
            pe_mm([(pbd[:, hh * 512:(hh + 1) * 512], GT[:], bdown_bf[:, hh * 512:(hh + 1) * 512], True, True)
                   for hh in range(2)], [r_GT, r_bdown], r_pbd)
            acc, r_acc = acc_r.next()
            c0 = i * 4
            op1("dve", "scalar_tensor_tensor", r_ygk + [r_route] + r_pbd, [r_acc], acc[:], yg[:, 0, :],
                gk_all[:, c0:c0 + 1], pbd[:], ALU.mult, ALU.add)
            for k in (1, 2, 3):
                eng = "dve"
                op1(eng, "scalar_tensor_tensor", r_ygk + [r_route, r_acc], [r_acc], acc[:], yg[:, k, :],
                    gk_all[:, c0 + k:c0 + k + 1], acc[:], ALU.mult, ALU.add)
            s4, r_s4 = st4_r.next()
            op1("act", "activation", [r_acc], [r_junk4, r_s4], junk4[:], acc[:], AF.Square, accum_out=s4[:, 0:1])
            rstd_from(s4[:, 0:1], s4[:, 1:2], 1.0 / 1024, r_s4, r_s4)
            ob, r_ob = ob_r.next()
            op1("dve", "scalar_tensor_tensor", [r_acc, r_s4, r_MR], [r_ob], ob[:], acc[:], s4[:, 1:2], MR[:, 3, :],
                ALU.mult, ALU.mult)
            op1("pool", "tensor_tensor", [r_ob, r_xl], [r_ob], ob[:], ob[:], xl[:], ALU.add)
            ro = Reg("out")
            dma("sp", out[i * 128:(i + 1) * 128, :], ob[:], [r_ob], [ro], d_out[i % 2])
            out_regs.append(ro)
        S.op("sp", None, reads=out_regs)
        S.run()
        st4.close()
    return nc


def _host_consts():
    ident = np.eye(128, dtype=np.float32)
    U = np.triu(np.ones((128, 128), np.float32), k=1)
    ones = np.ones((128, 128), np.float32)
    jj = np.arange(128)[:, None]
    ii = np.arange(128)[None, :]
    mprev = (jj >= ii).astype(np.float32)
    mnext = (jj <= ii).astype(np.float32)
    cbf = np.concatenate([ident, U, ones, mprev, mnext], axis=1)
    cf32 = np.concatenate([np.tile(np.arange(32, dtype=np.float32), (128, 1)),
                           np.tile(np.arange(NB, dtype=np.float32) * R, (128, 1)),
                           np.arange(128, dtype=np.float32)[:, None],
                           np.tile(np.arange(8, dtype=np.float32) * R, (128, 1))], axis=1)
    t = np.arange(4096)
    pos_row = (t // 64).astype(np.float32)
    pos_col = (t % 64).astype(np.float32)
    inv_freq = (np.float32(10000.0) ** (-np.arange(16, dtype=np.float32) / np.float32(16))).astype(np.float32)
    ar = pos_row[:, None] * inv_freq
    ac = pos_col[:, None] * inv_freq
    cr, sr, cc_, sc_ = np.cos(ar), np.sin(ar), np.cos(ac), np.sin(ac)
    rope = np.concatenate([cr, cr, cc_, cc_, -sr, sr, -sc_, sc_], axis=1).astype(np.float32)
    return np.ascontiguousarray(cbf), np.ascontiguousarray(cf32), np.ascontiguousarray(rope)


_CACHE = {}


def _prep(x, c, ctx, c_ctx, w_ada, b_ada, g_pre_mix, g_post_mix, g_pre_ffn, g_post_ffn,
          w_in, b_in, attn_sink, sgu_ln_g, sgu_ln_b, sgu_w, sgu_b, g_attn_out, g_sgu_out,
          w_out, b_out, w_router, b_router, w_gate_up, b_gate_up, w_down, b_down):
    f = lambda a: np.ascontiguousarray(np.asarray(a, dtype=np.float32))
    x, c, ctx, c_ctx = f(x), f(c), f(ctx), f(c_ctx)
    cbf, cf32, rope = _host_consts()
    perm = np.concatenate([np.arange(0, 512), np.arange(768, 1280), np.arange(1280, 1792), np.arange(512, 768)])
    w_in_p = f(f(w_in)[0][:, perm])
    b_in_p = f(b_in)[0][perm]
    col = lambda v: f(v).reshape(8, 128).T
    colpack = f(np.concatenate([col(f(g_pre_mix)[0]),
                                col(np.concatenate([f(g_attn_out)[0], f(g_sgu_out)[0]]))], axis=1))
    rowpack = f(np.concatenate([f(g_post_mix)[0], f(g_pre_ffn)[0], f(g_post_ffn)[0], f(sgu_ln_g)[0],
                                f(sgu_ln_b)[0], f(attn_sink)[0]])[None, :])
    browf = f(np.concatenate([b_in_p, f(b_out)[0], f(sgu_b)[0].reshape(-1), f(b_router)[0]])[None, :])
    sguwT = f(np.transpose(f(sgu_w)[0], (2, 0, 1)).reshape(128, 1024))
    wgu3 = f(w_gate_up)[0]
    wd3 = f(w_down)[0]
    bgu = f(f(b_gate_up)[0].reshape(32, 2, 8, 128).transpose(0, 3, 1, 2).reshape(32 * 128, 16))
    shared = {
        "w_ada": f(w_ada)[0], "b_ada": f(b_ada), "colpack": colpack, "rowpack": rowpack, "browf": browf,
        "w_in": w_in_p, "w_out": f(w_out)[0], "w_r": f(w_router)[0], "sguwT": sguwT, "b_down": f(b_down)[0],
        "bgu": bgu, "cbf": cbf, "cf32": cf32, "rope": rope,
    }
    for k in range(8):
        shared["wgu%d" % k] = f(wgu3[:, k * 128:(k + 1) * 128, :].reshape(32 * 128, 2048))
        shared["wd%d" % k] = f(wd3[:, k * 128:(k + 1) * 128, :].reshape(32 * 128, 1024))
    in_maps = []
    for b in range(8):
        m = dict(shared)
        m["x"] = x[b]
        m["ctx"] = ctx[b]
        m["cc"] = f(np.stack([c[b].reshape(8, 128).T, c_ctx.reshape(8, 128).T], axis=2).reshape(128, 16))
        in_maps.append(m)
    return in_maps


def kernel(**inputs):
    if "nc" not in _CACHE:
        _CACHE["nc"] = build_program()
    nc = _CACHE["nc"]
    in_maps = _prep(**inputs)
    res = run_bass_kernel_spmd(nc, in_maps, core_ids=list(range(8)))
    return np.stack([np.asarray(r["out"], dtype=np.float32) for r in res.results], axis=0)
```

```python
import sys
import numpy as np
from contextlib import ExitStack
import concourse.bass as bass
import concourse.mybir as mybir
from concourse.bass_utils import run_bass_kernel_spmd

F32 = mybir.dt.float32
BF16 = mybir.dt.bfloat16
I32 = mybir.dt.int32
ALU = mybir.AluOpType
AF = mybir.ActivationFunctionType
AX = mybir.AxisListType

NT = 32
R = 512
NB = 64
RG = R // 128
EPS = 1e-6
SUB = 0


class Reg:
    __slots__ = ("name", "w", "rs")

    def __init__(self, name=""):
        self.name = name
        self.w = None
        self.rs = []


class DSem:
    def __init__(self, sem):
        self.sem = sem
        self.count = 0
        self.ops = []

    def seal(self):
        for o in self.ops:
            o.dcount = self.count


class Op:
    __slots__ = ("eng", "fn", "deps", "sig", "sigcount", "dsem", "dcount", "idx", "tag")


class Sched:
    ENGS = ("pe", "act", "dve", "pool", "sp")

    def __init__(self, nc, stack):
        self.nc = nc
        self.stack = stack
        self.ops = []
        self.esem = {e: stack.enter_context(nc.semaphore("es_" + e)) for e in ("pe", "act", "dve", "pool")}

    def dsem(self, name):
        return DSem(self.stack.enter_context(self.nc.semaphore("ds_" + name)))

    def op(self, eng, fn, reads=(), writes=(), dsem=None):
        o = Op()
        o.eng = eng
        o.fn = fn
        o.dsem = dsem
        o.sig = False
        o.sigcount = 0
        o.dcount = 0
        o.idx = len(self.ops)
        fr = sys._getframe(1)
        tags = []
        while fr is not None and len(tags) < 4:
            if fr.f_code.co_filename == __file__:
                tags.append(str(fr.f_lineno))
            fr = fr.f_back
        o.tag = "L" + "<".join(tags)
        deps = set()
        for r in reads:
            if r.w is not None:
                deps.add(r.w)
        rset = set(id(r) for r in reads)
        for r in writes:
            if r.w is not None:
                t = self.ops[r.w]
                if not (t.dsem is None and t.fn is not None and t.eng == eng and id(r) not in rset):
                    deps.add(r.w)
            deps.update(r.rs)
        deps.discard(o.idx)
        if fn is not None:
            for r in reads:
                r.rs.append(o.idx)
            for r in writes:
                r.w = o.idx
                r.rs = []
        o.deps = deps
        if dsem is not None:
            dsem.count += 16
            o.dcount = dsem.count
            dsem.ops.append(o)
        self.ops.append(o)
        return o

    def finalize(self):
        ops = self.ops
        for o in ops:
            for d in o.deps:
                t = ops[d]
                if t.dsem is not None:
                    continue
                if t.eng == o.eng and o.eng in ("pe", "sp"):
                    continue
                t.sig = True
        cnt = {e: 0 for e in self.ENGS}
        for o in ops:
            if o.dsem is None and o.sig:
                cnt[o.eng] += 1
                o.sigcount = cnt[o.eng]

    def emit(self, engname, engobj):
        ops = self.ops
        waited = {}
        for o in ops:
            if o.eng != engname:
                continue
            need = {}
            for d in o.deps:
                t = ops[d]
                if t.dsem is not None:
                    key = ("d", id(t.dsem))
                    sem = t.dsem.sem
                    val = t.dcount
                else:
                    if t.eng == o.eng and o.eng in ("pe", "sp"):
                        continue
                    key = ("e", t.eng)
                    sem = self.esem[t.eng]
                    val = t.sigcount
                if key not in need or need[key][1] < val:
                    need[key] = (sem, val)
            for key, (sem, val) in need.items():
                if waited.get(key, 0) < val:
                    engobj.wait_ge(sem, val)
                    waited[key] = val
            if o.fn is None:
                continue
            ins = o.fn(engobj)
            try:
                ins.annotate(o.tag)
            except Exception:
                pass
            if o.dsem is not None:
                ins.then_inc(o.dsem.sem, 16)
            elif o.sig:
                ins.then_inc(self.esem[o.eng], 1)

    def run(self):
        self.finalize()
        nc = self.nc
        with nc.Block() as block:
            @block.tensor
            def _(e):
                self.emit("pe", e)

            @block.scalar
            def _(e):
                self.emit("act", e)

            @block.vector
            def _(e):
                self.emit("dve", e)

            @block.gpsimd
            def _(e):
                self.emit("pool", e)

            @block.sync
            def _(e):
                self.emit("sp", e)


class Ring:
    def __init__(self, items):
        self.items = items
        self.i = 0

    def next(self):
        it = self.items[self.i % len(self.items)]
        self.i += 1
        return it


def build_program(stop_after=None):
    nc = bass.Bass("TRN2", target_bir_lowering=False)

    def din(name, shape, dt=F32):
        return nc.dram_tensor(name, shape, dt, kind="ExternalInput").ap()

    x = din("x", [4096, 1024])
    ctx = din("ctx", [256, 1024])
    cc = din("cc", [128, 16])
    w_ada = din("w_ada", [1024, 6144])
    b_ada = din("b_ada", [1, 6144])
    colpack = din("colpack", [128, 16])
    rowpack = din("rowpack", [1, 4104])
    browf = din("browf", [1, 3872])
    w_in = din("w_in", [1024, 1792])
    w_out = din("w_out", [1024, 1024])
    w_r = din("w_r", [1024, 32])
    sguwT = din("sguwT", [128, 1024])
    b_down = din("b_down", [32, 1024])
    if stop_after is None:
        wgu = [din("wgu%d" % k, [4096, 2048]) for k in range(8)]
        wd = [din("wd%d" % k, [4096, 1024]) for k in range(8)]
        bgu = din("bgu", [4096, 16])
    cbf_d = din("cbf", [128, 640])
    cf32_d = din("cf32", [128, 32 + NB + 9])
    rope_d = din("rope", [4096, 128])
    out = nc.dram_tensor("out", [4096, 1024], F32, kind="ExternalOutput").ap()
    ik = "Internal" if stop_after is None else "ExternalOutput"
    xs_d = nc.dram_tensor("xs_d", [NB * R, 1024], BF16, kind=ik).ap()
    ys_d = nc.dram_tensor("ys_d", [NB * R, 1024], F32, kind=ik).ap()
    xmid_d = nc.dram_tensor("xmid_d", [4096, 1024], F32, kind=ik).ap()
    h2_d = nc.dram_tensor("h2_d", [4096, 1024], BF16, kind=ik).ap()
    if stop_after is not None:
        dbg_route = nc.dram_tensor("dbg_route", [128, 3 * NT * 4 + 32], F32, kind="ExternalOutput").ap()
        dbg_book = nc.dram_tensor("dbg_book", [128, NT * 4 + NB], I32, kind="ExternalOutput").ap()

    with ExitStack() as st_all:
        S = Sched(nc, st_all)

        def sbuf(st, name, shape, dt):
            return st.enter_context(nc.sbuf_tensor("s_" + name, shape, dt))

        def mkring(st, name, shape, dt, n):
            return Ring([(sbuf(st, "%s%d" % (name, k), shape, dt), Reg(name)) for k in range(n)])

        def op1(eng, meth, reads, writes, *a, **k):
            S.op(eng, (lambda e: getattr(e, meth)(*a, **k)), reads, writes)

        def dma(eng, out_ap, in_ap, reads, writes, ds, **k):
            S.op(eng, (lambda e: e.dma_start(out=out_ap, in_=in_ap, **k)), reads, writes, dsem=ds)

        def pe_mm(mms, reads, writes):
            mms = list(mms)

            def fn(e):
                ins = None
                for (o, l, r, a, b) in mms:
                    ins = e.matmul(o, l, r, start=a, stop=b)
                return ins
            S.op("pe", fn, reads, writes)

        def pe_tr(trs, reads, writes):
            trs = list(trs)

            def fn(e):
                ins = None
                for (o, i_) in trs:
                    ins = e.transpose(o, i_, ident)
                return ins
            S.op("pe", fn, reads, writes)

        pbig = [st_all.enter_context(nc.psum_tensor("pb%d" % k, [128, 1024], F32)) for k in range(3)]
        pslots = []
        for k in range(3):
            for h in range(2):
                pslots.append((pbig[k], h, Reg("ps%d%d" % (k, h))))
        pstate = {"i": 0}

        def ps_next():
            t, h, r = pslots[pstate["i"] % 6]
            pstate["i"] += 1
            return t[:, h * 512:(h + 1) * 512], r

        def ps_pair():
            if pstate["i"] % 2 == 1:
                pstate["i"] += 1
            t, _, r0 = pslots[pstate["i"] % 6]
            _, _, r1 = pslots[(pstate["i"] + 1) % 6]
            pstate["i"] += 2
            return t, [r0, r1]

        ptr = Ring([(st_all.enter_context(nc.psum_tensor("pt%d" % k, [128, 8, 128], BF16)), Reg("pt")) for k in range(2)])

        cbf = sbuf(st_all, "cbf", [128, 640], BF16)
        ident = cbf[:, 0:128]
        Utri = cbf[:, 128:256]
        ones_bf = cbf[:, 256:384]
        mprev = cbf[:, 384:512]
        mnext = cbf[:, 512:640]
        cf32 = sbuf(st_all, "cf32", [128, 32 + NB + 9], F32)
        iota_e = cf32[:, 0:32]
        bstart = cf32[:, 32:32 + NB]
        pidx = cf32[:, 32 + NB:33 + NB]
        thr8 = cf32[:, 33 + NB:41 + NB]
        MR = sbuf(st_all, "MR", [128, 4, 1024], F32)
        bdown_bf = sbuf(st_all, "bdown_bf", [32, 1024], BF16)
        rank_all = sbuf(st_all, "rank_all", [128, NT * 4], F32)
        ek_all = sbuf(st_all, "ek_all", [128, NT * 4], F32)
        gk_all = sbuf(st_all, "gk_all", [128, NT * 4], F32)
        dest_i = sbuf(st_all, "dest_i", [128, NT * 4], I32)
        runc = sbuf(st_all, "runc", [128, 32], F32)
        eps_t = sbuf(st_all, "eps_t", [128, 1], F32)
        esink = sbuf(st_all, "esink", [128, 8], F32)
        A1 = sbuf(st_all, "A1", [128, 8, 2], F32)
        B1 = sbuf(st_all, "B1", [128, 8, 2], F32)
        colp = sbuf(st_all, "colp", [128, 16], F32)
        ones_f = sbuf(st_all, "ones_f", [1, 128], F32)
        widx = sbuf(st_all, "widx", [128, NB], I32)
        r_const = Reg("const")
        r_MR = Reg("MR")
        r_AB = Reg("AB")
        r_route = Reg("route")
        r_run = Reg("run")
        r_bdown = Reg("bdown")
        r_dest = Reg("dest")
        r_eblk = Reg("eblk")

        d_const = S.dsem("const")
        d_out = [S.dsem("out0"), S.dsem("out1")]

        st_mix = ExitStack()
        w_in_bf = sbuf(st_mix, "w_in_bf", [128, 8, 1792], BF16)
        w_out_bf = sbuf(st_mix, "w_out_bf", [128, 8, 1024], BF16)
        wr_bf = sbuf(st_mix, "wr_bf", [128, 8, 32], BF16)
        sguw_bf = sbuf(st_mix, "sguw_bf", [128, 1024], BF16)
        brow_bf = sbuf(st_mix, "brow_bf", [1, 3872], BF16)
        lngb = sbuf(st_mix, "lngb", [128, 1024], F32)
        kT2r = [(sbuf(st_mix, "kT2_%d" % k, [128, 2, 128], BF16), Reg("kT2")) for k in range(4)]
        vaugr = [(sbuf(st_mix, "vaug_%d" % k, [128, 2, 66], BF16), Reg("vaug")) for k in range(4)]
        kcT2 = sbuf(st_mix, "kcT2", [128, 2, 256], BF16)
        vcaug = sbuf(st_mix, "vcaug", [128, 2, 2, 66], BF16)
        r_kc = Reg("kc")
        r_vc = Reg("vc")
        r_w = Reg("wts")
        r_brow = Reg("brow")
        r_lngb = Reg("lngb")

        st0 = ExitStack()
        cstage = sbuf(st0, "cstage", [128, 640], F32)
        slab = mkring(st0, "slab", [128, 8, 512], F32, 2)
        bada_sb = sbuf(st0, "bada_sb", [1, 6144], F32)
        stg = mkring(st0, "stg", [128, 1792], F32, 2)
        browf_sb = sbuf(st0, "browf_sb", [1, 3872], F32)
        cc_sb = sbuf(st0, "cc_sb", [128, 8, 2], F32)
        sc_f = sbuf(st0, "sc_f", [128, 8, 2], F32)
        rep_f = sbuf(st0, "rep_f", [128, 8, 128], F32)
        modcol = sbuf(st0, "modcol", [128, 16, 2], F32)
        rows_bc = sbuf(st0, "rows_bc", [128, 3, 1024], F32)
        tmp8 = sbuf(st0, "tmp8", [128, 8, 2], F32)
        bdown_f = sbuf(st0, "bdown_f", [32, 1024], F32)
        wr_f = sbuf(st0, "wr_f", [128, 8, 32], F32)
        sguw_f = sbuf(st0, "sguw_f", [128, 1024], F32)
        sink_bc = sbuf(st0, "sink_bc", [128, 8], F32)
        r0 = {k: Reg(k) for k in ("cstage", "bada", "browf", "cc", "sc", "rep", "modcol", "rows", "tmp8",
                                  "bdown_f", "wr_f", "sguw_f", "sink")}

        dma("sp", cstage[:], cbf_d, [], [r0["cstage"]], d_const)
        dma("sp", cf32[:], cf32_d, [], [r_const], d_const)
        dma("sp", cc_sb[:].rearrange("p j s -> p (j s)"), cc, [], [r0["cc"]], d_const)
        dma("sp", bada_sb[:], b_ada, [], [r0["bada"]], d_const)
        dma("sp", browf_sb[:], browf, [], [r0["browf"]], d_const)
        dma("sp", rows_bc[:].rearrange("p a n -> p (a n)"), rowpack[0:1, 0:3072].partition_broadcast(128)[:, 0, :],
            [], [r0["rows"]], d_const)
        dma("sp", lngb[:], rowpack[0:1, 3072:4096].partition_broadcast(128)[:, 0, :], [], [r_lngb], d_const)
        dma("sp", sink_bc[:], rowpack[0:1, 4096:4104].partition_broadcast(128)[:, 0, :], [], [r0["sink"]], d_const)
        dma("sp", bdown_f[:], b_down, [], [r0["bdown_f"]], d_const)
        dma("sp", wr_f[:], w_r.rearrange("(j p) n -> p j n", p=128), [], [r0["wr_f"]], d_const)
        dma("sp", sguw_f[:], sguwT, [], [r0["sguw_f"]], d_const)
        d_const.seal()
        d_const2 = S.dsem("const2")
        dma("sp", colp[:], colpack, [], [r_const], d_const2)

        op1("pool", "memset", [], [r_const], eps_t[:], EPS)
        op1("pool", "memset", [], [r_const], ones_f[:], 1.0)
        op1("pool", "memset", [], [r_run], runc[:], 0.0)
        op1("act", "activation", [r0["cstage"]], [r_const], cbf[:], cstage[:], AF.Copy)
        op1("act", "activation", [r0["sink"]], [r_const], esink[:], sink_bc[:], AF.Exp)
        op1("act", "activation", [r0["cc"]], [r0["sc"]], sc_f[:], cc_sb[:], AF.Silu)
        op1("dve", "tensor_copy", [r0["sc"]], [r0["rep"]], rep_f[:], sc_f[:, :, 0:1].to_broadcast([128, 8, 128]))
        op1("dve", "tensor_copy", [r0["bdown_f"]], [r_bdown], bdown_bf[:], bdown_f[:])
        op1("dve", "tensor_copy", [r0["wr_f"]], [r_w], wr_bf[:], wr_f[:])
        op1("dve", "tensor_copy", [r0["sguw_f"]], [r_w], sguw_bf[:], sguw_f[:])
        op1("dve", "tensor_copy", [r0["browf"]], [r_brow], brow_bf[:], browf_sb[:])

        zt = sbuf(st0, "zt", [128, 4096], BF16)
        r_zt = Reg("zt")
        r_xs = Reg("xs_d")
        d_zero = S.dsem("zero")
        op1("pool", "memset", [], [r_zt], zt[:], 0.0)
        nz = NB * R // 512
        for k in range(nz):
            dma("sp", xs_d[k * 512:(k + 1) * 512, :].rearrange("(p a) n -> p (a n)", p=128), zt[:],
                [r_zt], [Reg("xsz")], d_zero)
        zero_ops = list(d_zero.ops)
        d_zero.seal()

        d_slab = [S.dsem("slab0"), S.dsem("slab1")]
        pcol, r_pcol = ps_next()
        for n in range(12):
            sl, r_sl = slab.next()
            dma("sp", sl[:], w_ada[:, n * 512:(n + 1) * 512].rearrange("(j p) n -> p j n", p=128),
                [], [r_sl], d_slab[n % 2])
            if n < 4:
                mms = []
                for fc in range(4):
                    ci = n * 4 + fc
                    o = pcol[:, ci * 2:ci * 2 + 2]
                    for j in range(8):
                        mms.append((o, sl[:, j, fc * 128:(fc + 1) * 128], sc_f[:, j, :], j == 0, False))
                    mms.append((o, bada_sb[0:1, ci * 128:(ci + 1) * 128], ones_f[0:1, 0:2], False, True))
                pe_mm(mms, [r_sl, r0["sc"], r0["bada"], r_const], [r_pcol])
                if n == 3:
                    op1("dve", "tensor_copy", [r_pcol], [r0["modcol"]],
                        modcol[:].rearrange("p a s -> p (a s)"), pcol[:, 0:32])
            else:
                pr, r_pr = ps_next()
                mms = [(pr, rep_f[:, j, :], sl[:, j, :], j == 0, False) for j in range(8)]
                mms.append((pr, ones_f[0:1, 0:128], bada_sb[0:1, n * 512:(n + 1) * 512], False, True))
                pe_mm(mms, [r_sl, r0["rep"], r0["bada"], r_const], [r_pr])
                q4 = (n - 4) // 2
                h4 = (n - 4) % 2
                op1("act", "activation", [r_pr], [r_MR], MR[:, q4, h4 * 512:(h4 + 1) * 512], pr, AF.Copy)
        op1("dve", "tensor_scalar", [r0["modcol"]], [r0["tmp8"]], tmp8[:], modcol[:, 8:16, :], 1.0, None, ALU.add)
        op1("dve", "tensor_tensor", [r0["tmp8"], r_const], [r_AB], A1[:], tmp8[:],
            colp[:, 0:8].unsqueeze(2).to_broadcast([128, 8, 2]), ALU.mult)
        op1("dve", "tensor_copy", [r0["modcol"]], [r_AB], B1[:], modcol[:, 0:8, :])
        op1("pool", "tensor_tensor", [r_MR, r0["rows"]], [r_MR], MR[:, 0, :], MR[:, 0, :], rows_bc[:, 0, :], ALU.mult)
        op1("dve", "scalar_tensor_tensor", [r_MR, r0["rows"]], [r_MR], MR[:, 2, :], MR[:, 2, :], 1.0,
            rows_bc[:, 1, :], ALU.add, ALU.mult)
        op1("pool", "tensor_tensor", [r_MR, r0["rows"]], [r_MR], MR[:, 3, :], MR[:, 3, :], rows_bc[:, 2, :], ALU.mult)

        d_stg = [S.dsem("stg0"), S.dsem("stg1")]
        for j in range(8):
            sg, r_sg = stg.next()
            dma("sp", sg[:], w_in[j * 128:(j + 1) * 128, :], [], [r_sg], d_stg[j % 2])
            op1("pool", "tensor_copy", [r_sg], [r_w], w_in_bf[:, j, :], sg[:])
        for j in range(8):
            sg, r_sg = stg.next()
            dma("sp", sg[:, 0:1024], w_out[j * 128:(j + 1) * 128, :], [], [r_sg], d_stg[j % 2])
            op1("pool", "tensor_copy", [r_sg], [r_w], w_out_bf[:, j, :], sg[:, 0:1024])

        allr0 = list(r0.values()) + [r_zt] + [r for (_, r) in slab.items] + [r for (_, r) in stg.items]
        for e in Sched.ENGS:
            S.op(e, None, reads=allr0)
        st0.close()

        st1 = ExitStack()
        xt_r = mkring(st1, "xt", [128, 1024], F32, 4)
        d_xt = [S.dsem("xt%d" % k) for k in range(4)]
        rp_r = mkring(st1, "rp", [128, 128], F32, 3)
        d_rp = [S.dsem("rp%d" % k) for k in range(3)]
        junk = sbuf(st1, "junk", [128, 1024], BF16)
        r_junk = Reg("junk")
        xs_r = mkring(st1, "xsb", [128, 1024], BF16, 2)
        hT_r = mkring(st1, "hT", [128, 8, 128], BF16, 2)
        qk_r = mkring(st1, "qk", [128, 768], BF16, 2)
        rA = sbuf(st1, "rA", [128, 512], F32)
        rB = sbuf(st1, "rB", [128, 512], F32)
        r_rA = Reg("rA")
        r_rB = Reg("rB")
        qT_r = mkring(st1, "qT", [128, 4, 128], BF16, 3)
        u_r = mkring(st1, "u", [128, 512], BF16, 2)
        gv_r = mkring(st1, "gv", [128, 512], F32, 2)
        z_r = mkring(st1, "z", [128, 512], F32, 2)
        vln_r = mkring(st1, "vln", [128, 512], BF16, 2)
        sgo_r = mkring(st1, "sgo", [128, 512], F32, 3)
        p_r = mkring(st1, "p", [128, 512], BF16, 7)
        ao_r = mkring(st1, "ao", [128, 512], F32, 2)
        on_r = mkring(st1, "on", [128, 1024], BF16, 2)
        oT_r = mkring(st1, "oT", [128, 8, 128], BF16, 2)
        tm_r = mkring(st1, "tm", [128, 1024], F32, 1)
        xm_r = mkring(st1, "xm", [128, 1024], F32, 2)
        d_xm = [S.dsem("xm0"), S.dsem("xm1")]
        h2f_r = mkring(st1, "h2f", [128, 1024], F32, 1)
        h2b_r = mkring(st1, "h2b", [128, 1024], BF16, 2)
        d_h2 = [S.dsem("h2b0"), S.dsem("h2b1")]
        h2T_r = mkring(st1, "h2T", [128, 8, 128], BF16, 2)
        st_r = mkring(st1, "stat", [128, 16], F32, 6)
        bn_r = mkring(st1, "bn", [128, 8], F32, 2)
        lg_r = mkring(st1, "lg", [128, 32], F32, 2)
        t8_r = mkring(st1, "t8", [128, 8], F32, 2)
        mk_r = mkring(st1, "mk", [128, 32], F32, 2)
        mkb_r = mkring(st1, "mkb", [128, 32], BF16, 2)
        ex_r = mkring(st1, "ex", [128, 32], F32, 2)
        oh_r = mkring(st1, "oh", [128, 4, 32], F32, 2)
        Dm_r = mkring(st1, "Dm", [128, 32], F32, 2)
        tq_r = mkring(st1, "tq", [128, 4, 32], F32, 2)
        r_h2d = Reg("h2_d")
        r_xmd = Reg("xmid_d")

        BIN, BOUT, BSGU, BRT = 0, 1792, 2816, 3840

        def rstd_from(ss_ap, out_ap, scale, r_in, r_out):
            op1("act", "activation", [r_in, r_const], [r_out], out_ap, ss_ap, AF.Sqrt, bias=eps_t[:], scale=scale)
            op1("dve", "reciprocal", [r_out], [r_out], out_ap, out_ap)

        def norm_T(src, r_src, colA, colB, s_idx, hT, r_hT, xsb, r_xsb, stt, r_stt):
            op1("act", "activation", [r_src], [r_junk, r_stt], junk[:], src, AF.Square, accum_out=stt[:, 0:1])
            rstd_from(stt[:, 0:1], stt[:, 1:2], 1.0 / 1024, r_stt, r_stt)
            op1("dve", "tensor_scalar", [r_src, r_stt], [r_xsb], xsb[:], src, stt[:, 1:2], None, ALU.mult)
            pt, r_pt = ptr.next()
            pe_tr([(pt[:, j, :], xsb[:, j * 128:(j + 1) * 128]) for j in range(8)], [r_xsb, r_const], [r_pt])
            for j in range(8):
                S.op("act", (lambda e, j=j: e.activation(hT[:, j, :], pt[:, j, :], AF.Identity,
                                                         bias=colB[:, j, s_idx:s_idx + 1],
                                                         scale=colA[:, j, s_idx:s_idx + 1])),
                     [r_pt, r_AB], [r_hT])

        op1("pool", "memset", [], [r_vc], vcaug[:], 1.0)
        for k in range(4):
            op1("pool", "memset", [], [vaugr[k][1]], vaugr[k][0][:], 1.0)
        for ci in range(2):
            xt, r_xt = xt_r.next()
            dma("sp", xt[:], ctx[ci * 128:(ci + 1) * 128, :], [], [r_xt], d_xt[(xt_r.i - 1) % 4])
            xsb, r_xsb = xs_r.next()
            hT, r_hT = hT_r.next()
            stt, r_stt = st_r.next()
            norm_T(xt[:], r_xt, A1, B1, 1, hT, r_hT, xsb, r_xsb, stt, r_stt)
            pk, r_pk = ps_next()
            mms = [(pk[:, 0:256], hT[:, j, :], w_in_bf[:, j, 1536:1792], j == 0, False) for j in range(8)]
            mms.append((pk[:, 0:256], ones_bf[0:1, :], brow_bf[0:1, BIN + 1536:BIN + 1792], False, True))
            pe_mm(mms, [r_hT, r_w, r_brow, r_const], [r_pk])
            qk, r_qk = qk_r.next()
            for dup in range(2):
                S.op("act", (lambda e, dup=dup, qk=qk, pk=pk: e.activation(
                    qk[:, 0:256].rearrange("p (g d c) -> p g d c", g=2, d=2)[:, :, dup, :],
                    pk[:, 0:128].rearrange("p (g c) -> p g c", g=2), AF.Copy)), [r_pk], [r_qk])
            op1("act", "activation", [r_pk], [r_vc], vcaug[:, ci, :, 0:64],
                pk[:, 128:256].rearrange("p (g c) -> p g c", g=2), AF.Copy)
            pt, r_pt = ptr.next()
            pe_tr([(pt[:, g, :], qk[:, g * 128:(g + 1) * 128]) for g in range(2)], [r_qk, r_const], [r_pt])
            op1("dve", "tensor_copy", [r_pt], [r_kc], kcT2[:, :, ci * 128:(ci + 1) * 128], pt[:, 0:2, :])

        if stop_after == 0:
            dbg_kc = nc.dram_tensor("dbg_kc", [128, 512], BF16, kind="ExternalOutput").ap()
            d_dbg = S.dsem("dbg")
            rr = Reg("dbgo")
            dma("sp", dbg_kc, kcT2[:].rearrange("p g k -> p (g k)"), [r_kc], [rr], d_dbg)
            o = S.op("sp", None, reads=[rr])
            o.deps.update(z.idx for z in zero_ops)
            S.run()
            st1.close()
            st_mix.close()
            return nc
        tiles = {}

        def rope(src, H, dst3, rp, reads, r_dst):
            X = src.rearrange("p (h c) -> p h c", h=H)
            n = H * 64
            Av = rA[:, 0:n].rearrange("p (h c) -> p h c", h=H)
            Bv = rB[:, 0:n].rearrange("p (h c) -> p h c", h=H)
            op1("dve", "tensor_tensor", reads, [r_rA], Av, X, rp[:, 0:64].unsqueeze(1).to_broadcast([128, H, 64]),
                ALU.mult)
            for ax in range(2):
                for hf in range(2):
                    o0 = ax * 32 + hf * 16
                    i0 = ax * 32 + (1 - hf) * 16
                    op1("dve", "tensor_tensor", reads, [r_rB], Bv[:, :, o0:o0 + 16], X[:, :, i0:i0 + 16],
                        rp[:, 64 + o0:64 + o0 + 16].unsqueeze(1).to_broadcast([128, H, 16]), ALU.mult)
            for d3 in dst3:
                op1("pool", "tensor_tensor", [r_rA, r_rB], [r_dst], d3, Av, Bv, ALU.add)

        def stageA(i):
            T = {}
            xt, r_xt = xt_r.next()
            dma("sp", xt[:], x[i * 128:(i + 1) * 128, :], [], [r_xt], d_xt[(xt_r.i - 1) % 4])
            rp, r_rp = rp_r.next()
            dma("sp", rp[:], rope_d[i * 128:(i + 1) * 128, :], [], [r_rp], d_rp[(rp_r.i - 1) % 3])
            T["xt"] = (xt, r_xt)
            xsb, r_xsb = xs_r.next()
            hT, r_hT = hT_r.next()
            stt, r_stt = st_r.next()
            norm_T(xt[:], r_xt, A1, B1, 0, hT, r_hT, xsb, r_xsb, stt, r_stt)
            chunks = []
            for (c0, cw) in ((0, 512), (512, 512), (1024, 512), (1536, 128), (1664, 128)):
                pc, r_pc = ps_next()
                mms = [(pc[:, 0:cw], hT[:, j, :], w_in_bf[:, j, c0:c0 + cw], j == 0, False) for j in range(8)]
                mms.append((pc[:, 0:cw], ones_bf[0:1, :], brow_bf[0:1, BIN + c0:BIN + c0 + cw], False, True))
                pe_mm(mms, [r_hT, r_w, r_brow, r_const], [r_pc])
                chunks.append((pc, r_pc))
            (pq, r_pq), (psu, r_psu), (psv, r_psv), (pkv, r_pkv), (pvv, r_pvv) = chunks
            qk, r_qk = qk_r.next()
            rope(pq, 8, [qk[:, 0:512].rearrange("p (h c) -> p h c", h=8)], rp, [r_pq, r_rp], r_qk)
            kd = qk[:, 512:768].rearrange("p (g d c) -> p g d c", g=2, d=2)
            rope(pkv[:, 0:128], 2, [kd[:, :, 0, :], kd[:, :, 1, :]], rp, [r_pkv, r_rp], r_qk)
            if SUB == 1:
                return
            va, r_va = vaugr[i % 4]
            if SUB == 7:
                op1("dve", "tensor_copy", [r_pkv], [r_va], va[:, :, 0:64],
                    pkv[:, 128:256].rearrange("p (g c) -> p g c", g=2))
                return
            if SUB == 71:
                op1("dve", "tensor_copy", [r_pkv], [r_va], va[:, :, 0:64],
                    pkv[:, 0:128].rearrange("p (g c) -> p g c", g=2))
                return
            if SUB == 72:
                op1("dve", "tensor_copy", [r_qk], [r_va], va[:, :, 0:64],
                    qk[:, 0:128].rearrange("p (g c) -> p g c", g=2))
                return
            if SUB == 73:
                op1("dve", "tensor_copy", [r_pkv], [r_va], va[:, 0, 0:64], pkv[:, 128:192])
                return
            if SUB == 74:
                op1("dve", "tensor_copy", [r_pkv], [r_va], va[:, :, 0:64],
                    pkv[:, 256:384].rearrange("p (g c) -> p g c", g=2))
                return
            if SUB == 75:
                op1("dve", "tensor_copy", [r_pkv], [r_va], va[:, :, 0:64],
                    pkv[:, 64:192].rearrange("p (g c) -> p g c", g=2))
                return
            if SUB == 76:
                op1("dve", "tensor_copy", [r_pkv], [r_va], va[:, 0, 0:64], pkv[:, 192:256])
                return
            if SUB == 77:
                op1("dve", "tensor_copy", [r_pq], [r_va], va[:, 0, 0:64], pq[:, 192:256])
                return
            if SUB == 78:
                op1("dve", "tensor_copy", [r_pq], [r_va], va[:, 0, 0:64], pq[:, 448:512])
                return
            if SUB == 8:
                op1("act", "activation", [r_pkv], [r_junk], junk[:, 0:128].rearrange("p (g c) -> p g c", g=2),
                    pkv[:, 128:256].rearrange("p (g c) -> p g c", g=2), AF.Copy)
                return
            if SUB == 9:
                op1("act", "activation", [r_pkv], [r_va], va[:, :, 0:64],
                    pkv[:, 128:256].rearrange("p (g c) -> p g c", g=2), AF.Identity)
                return
            op1("act", "activation", [r_pvv], [r_va], va[:, :, 0:64],
                pvv[:, 0:128].rearrange("p (g c) -> p g c", g=2), AF.Copy)
            if SUB == 4:
                return
            pt, r_pt = ptr.next()
            pe_tr([(pt[:, a, :], qk[:, a * 128:(a + 1) * 128]) for a in range(6)], [r_qk, r_const], [r_pt])
            if SUB == 5:
                return
            qT, r_qT = qT_r.next()
            kT, r_kT = kT2r[i % 4]
            op1("dve", "tensor_copy", [r_pt], [r_qT], qT[:], pt[:, 0:4, :])
            if SUB == 6:
                return
            op1("dve", "tensor_copy", [r_pt], [r_kT], kT[:], pt[:, 4:6, :])
            T["qT"] = (qT, r_qT)
            if SUB == 2:
                return
            u, r_u = u_r.next()
            op1("act", "activation", [r_psu], [r_u], u[:], psu, AF.Gelu_apprx_tanh)
            gv, r_gv = gv_r.next()
            op1("act", "activation", [r_psv], [r_gv], gv[:], psv, AF.Gelu_apprx_tanh)
            bn, r_bn = bn_r.next()
            op1("dve", "bn_stats", [r_gv], [r_bn], bn[:, 0:6], gv[:])
            op1("dve", "bn_aggr", [r_bn], [r_bn], bn[:, 6:8], bn[:, 0:6])
            rstd_from(bn[:, 7:8], bn[:, 7:8], 1.0, r_bn, r_bn)
            z, r_z = z_r.next()
            op1("dve", "tensor_scalar", [r_gv, r_bn], [r_z], z[:], gv[:], bn[:, 6:7], bn[:, 7:8], ALU.subtract, ALU.mult)
            op1("pool", "tensor_tensor", [r_z, r_lngb], [r_z], z[:], z[:], lngb[:, 0:512], ALU.mult)
            vln, r_vln = vln_r.next()
            op1("pool", "tensor_tensor", [r_z, r_lngb], [r_vln], vln[:], z[:], lngb[:, 512:1024], ALU.add)
            if SUB == 3:
                return
            pm, r_pm = ps_next()
            mms = []
            for h in range(8):
                o = pm[:, h * 64:(h + 1) * 64]
                mms.append((o, sguw_bf[:, h * 128:(h + 1) * 128], vln[:, h * 64:(h + 1) * 64], True, False))
                mms.append((o, brow_bf[0:1, BSGU + h * 128:BSGU + (h + 1) * 128], ones_bf[0:1, 0:64], False, True))
            pe_mm(mms, [r_vln, r_w, r_brow, r_const], [r_pm])
            sgo, r_sgo = sgo_r.next()
            op1("dve", "tensor_tensor", [r_pm, r_u], [r_sgo], sgo[:], pm, u[:], ALU.mult)
            T["sgo"] = (sgo, r_sgo)
            tiles[i] = T

        SCALE = 64 ** -0.5

        def stageB(i):
            T = tiles.pop(i)
            xt, r_xt = T["xt"]
            qT, r_qT = T["qT"]
            sgo, r_sgo = T["sgo"]
            ao, r_ao = ao_r.next()
            for g in range(2):
                klist = []
                if i > 0:
                    klist.append(("p", kT2r[(i - 1) % 4], vaugr[(i - 1) % 4], None))
                klist.append(("o", kT2r[i % 4], vaugr[i % 4], None))
                if i < NT - 1:
                    klist.append(("n", kT2r[(i + 1) % 4], vaugr[(i + 1) % 4], None))
                klist.append(("c", (kcT2, r_kc), (vcaug, r_vc), 0))
                klist.append(("c", (kcT2, r_kc), (vcaug, r_vc), 1))
                plist = []
                for (kind, (kt, r_kt), (vt, r_vt), ci) in klist:
                    pss, r_pss = ps_pair()
                    mms = []
                    for h4 in range(4):
                        h = 4 * g + h4
                        a, hf = h // 2, h % 2
                        if kind == "c":
                            ks = kt[hf * 64:(hf + 1) * 64, g, ci * 128:(ci + 1) * 128]
                        else:
                            ks = kt[hf * 64:(hf + 1) * 64, g, :]
                        oc = hf * 512 + (h4 // 2) * 128
                        mms.append((pss[:, oc:oc + 128], ks, qT[hf * 64:(hf + 1) * 64, a, :], True, True))
                    pe_mm(mms, [r_kt, r_qT], r_pss)
                    pp, r_pp = p_r.next()
                    op1("act", "activation", r_pss, [r_pp], pp[:].rearrange("p (b c) -> p b c", b=2),
                        pss[:].rearrange("p (b c) -> p b c", b=2)[:, :, 0:256], AF.Exp, scale=SCALE)
                    if kind in ("p", "n"):
                        mk = mprev if kind == "p" else mnext
                        op1("pool", "tensor_tensor", [r_pp, r_const], [r_pp],
                            pp[:].rearrange("p (h q) -> p h q", h=4), pp[:].rearrange("p (h q) -> p h q", h=4),
                            mk.unsqueeze(1).to_broadcast([128, 4, 128]), ALU.mult)
                    if kind == "c":
                        vs = vt[:, ci, g, 0:65]
                    else:
                        vs = vt[:, g, 0:65]
                    plist.append((pp, r_pp, vs, r_vt))
                if SUB == 21:
                    return
                po, r_po = ps_next()
                mms = []
                rd = []
                for h4 in range(4):
                    for k, (pp, r_pp, vs, r_vt) in enumerate(plist):
                        pc_ = (h4 % 2) * 256 + (h4 // 2) * 128
                        mms.append((po[:, h4 * 66:h4 * 66 + 65], pp[:, pc_:pc_ + 128], vs,
                                    k == 0, k == len(plist) - 1))
                        rd += [r_pp, r_vt]
                pe_mm(mms, rd, [r_po])
                stt, r_stt = st_r.next()
                po3 = po[:, 0:264].rearrange("p (h c) -> p h c", h=4)
                op1("dve", "tensor_tensor", [r_po, r_const], [r_stt], stt[:, 0:4], po3[:, :, 64], esink[:, 4 * g:4 * g + 4],
                    ALU.add)
                op1("dve", "reciprocal", [r_stt], [r_stt], stt[:, 4:8], stt[:, 0:4])
                op1("dve", "tensor_tensor", [r_po, r_stt], [r_ao],
                    ao[:, g * 256:(g + 1) * 256].rearrange("p (h c) -> p h c", h=4), po3[:, :, 0:64],
                    stt[:, 4:8].unsqueeze(2).to_broadcast([128, 4, 64]), ALU.mult)
                if SUB == 22:
                    return
            if SUB == 23:
                return
            stt, r_stt = st_r.next()
            op1("act", "activation", [r_ao], [r_junk, r_stt], junk[:, 0:512], ao[:], AF.Square, accum_out=stt[:, 0:1])
            op1("act", "activation", [r_sgo], [r_junk, r_stt], junk[:, 512:1024], sgo[:], AF.Square,
                accum_out=stt[:, 1:2])
            rstd_from(stt[:, 0:2], stt[:, 2:4], 1.0 / 512, r_stt, r_stt)
            on, r_on = on_r.next()
            op1("dve", "tensor_scalar", [r_ao, r_stt], [r_on], on[:, 0:512], ao[:], stt[:, 2:3], None, ALU.mult)
            op1("pool", "tensor_scalar", [r_sgo, r_stt], [r_on], on[:, 512:1024], sgo[:], stt[:, 3:4], None, ALU.mult)
            pt, r_pt = ptr.next()
            pe_tr([(pt[:, j, :], on[:, j * 128:(j + 1) * 128]) for j in range(8)], [r_on, r_const], [r_pt])
            oT, r_oT = oT_r.next()
            op1("dve", "tensor_tensor", [r_pt, r_const], [r_oT], oT[:], pt[:],
                colp[:, 8:16].unsqueeze(2).to_broadcast([128, 8, 128]), ALU.mult)
            if SUB == 24:
                return
            pmix, r_pmix = ps_pair()
            mms = []
            for hh in range(2):
                o = pmix[:, hh * 512:(hh + 1) * 512]
                for j in range(8):
                    mms.append((o, oT[:, j, :], w_out_bf[:, j, hh * 512:(hh + 1) * 512], j == 0, False))
                mms.append((o, ones_bf[0:1, :], brow_bf[0:1, BOUT + hh * 512:BOUT + (hh + 1) * 512], False, True))
            pe_mm(mms, [r_oT, r_w, r_brow, r_const], r_pmix)
            stt, r_stt = st_r.next()
            op1("act", "activation", r_pmix, [r_junk, r_stt], junk[:], pmix[:], AF.Square, accum_out=stt[:, 0:1])
            rstd_from(stt[:, 0:1], stt[:, 1:2], 1.0 / 1024, r_stt, r_stt)
            tm, r_tm = tm_r.next()
            op1("dve", "scalar_tensor_tensor", r_pmix + [r_stt, r_MR], [r_tm], tm[:], pmix[:], stt[:, 1:2], MR[:, 0, :],
                ALU.mult, ALU.mult)
            xm, r_xm = xm_r.next()
            kx = (xm_r.i - 1) % 2
            op1("pool", "tensor_tensor", [r_tm, r_xt], [r_xm], xm[:], tm[:], xt[:], ALU.add)
            dma("sp", xmid_d[i * 128:(i + 1) * 128, :], xm[:], [r_xm], [r_xmd], d_xm[kx])
            if SUB == 25:
                return
            op1("act", "activation", [r_xm], [r_junk, r_stt], junk[:], xm[:], AF.Square, accum_out=stt[:, 2:3])
            rstd_from(stt[:, 2:3], stt[:, 3:4], 1.0 / 1024, r_stt, r_stt)
            h2f, r_h2f = h2f_r.next()
            op1("dve", "scalar_tensor_tensor", [r_xm, r_stt, r_MR], [r_h2f], h2f[:], xm[:], stt[:, 3:4], MR[:, 2, :],
                ALU.mult, ALU.mult)
            h2b, r_h2b = h2b_r.next()
            kh = (h2b_r.i - 1) % 2
            op1("pool", "tensor_tensor", [r_h2f, r_MR], [r_h2b], h2b[:], h2f[:], MR[:, 1, :], ALU.add)
            dma("sp", h2_d[i * 128:(i + 1) * 128, :], h2b[:], [r_h2b], [r_h2d], d_h2[kh])
            if SUB == 26:
                return
            pt, r_pt = ptr.next()
            pe_tr([(pt[:, j, :], h2b[:, j * 128:(j + 1) * 128]) for j in range(8)], [r_h2b, r_const], [r_pt])
            h2T, r_h2T = h2T_r.next()
            op1("act", "activation", [r_pt], [r_h2T], h2T[:], pt[:], AF.Copy)
            pl, r_pl = ps_next()
            mms = [(pl[:, 0:32], h2T[:, j, :], wr_bf[:, j, :], j == 0, False) for j in range(8)]
            mms.append((pl[:, 0:32], ones_bf[0:1, :], brow_bf[0:1, BRT:BRT + 32], False, True))
            pe_mm(mms, [r_h2T, r_w, r_brow, r_const], [r_pl])
            lg, r_lg = lg_r.next()
            op1("dve", "tensor_copy", [r_pl], [r_lg], lg[:], pl[:, 0:32])
            t8, r_t8 = t8_r.next()
            op1("dve", "max", [r_lg], [r_t8], t8[:], lg[:])
            if SUB == 27:
                return
            mk, r_mk = mk_r.next()
            op1("dve", "tensor_scalar", [r_lg, r_t8], [r_mk], mk[:], lg[:], t8[:, 3:4], None, ALU.is_ge)
            mkb, r_mkb = mkb_r.next()
            op1("dve", "tensor_copy", [r_mk], [r_mkb], mkb[:], mk[:])
            ex, r_ex = ex_r.next()
            op1("dve", "tensor_scalar", [r_t8], [r_ex], ex[:, 0:4], t8[:, 0:4], t8[:, 0:1], None, ALU.subtract)
            op1("act", "activation", [r_ex], [r_ex], ex[:, 4:8], ex[:, 0:4], AF.Exp)
            op1("dve", "tensor_reduce", [r_ex], [r_ex], ex[:, 8:9], ex[:, 4:8], AX.X, ALU.add)
            op1("dve", "reciprocal", [r_ex], [r_ex], ex[:, 9:10], ex[:, 8:9])
            op1("dve", "tensor_scalar", [r_ex], [r_route], gk_all[:, i * 4:(i + 1) * 4], ex[:, 4:8], ex[:, 9:10], None,
                ALU.mult)
            oh, r_oh = oh_r.next()
            for k in range(4):
                op1("dve", "tensor_scalar", [r_lg, r_t8], [r_oh], oh[:, k, :], lg[:], t8[:, k:k + 1], None, ALU.is_equal)
            pc2, r_pc2 = ps_next()
            pe_mm([(pc2[:, 0:32], Utri, mkb[:], True, True), (pc2[:, 32:64], ones_bf, mkb[:], True, True)],
                  [r_mkb, r_const], [r_pc2])
            Dm, r_Dm = Dm_r.next()
            op1("dve", "tensor_tensor", [r_pc2, r_run], [r_Dm], Dm[:], pc2[:, 0:32], runc[:], ALU.add)
            op1("dve", "tensor_tensor", [r_pc2, r_run], [r_run], runc[:], runc[:], pc2[:, 32:64], ALU.add)
            tq, r_tq = tq_r.next()
            op1("dve", "tensor_tensor", [r_oh, r_Dm], [r_tq], tq[:], oh[:], Dm[:].unsqueeze(1).to_broadcast([128, 4, 32]),
                ALU.mult)
            op1("dve", "tensor_reduce", [r_tq], [r_route], rank_all[:, i * 4:(i + 1) * 4], tq[:], AX.X, ALU.add)
            op1("dve", "tensor_tensor", [r_oh, r_const], [r_tq], tq[:], oh[:],
                iota_e.unsqueeze(1).to_broadcast([128, 4, 32]), ALU.mult)
            op1("dve", "tensor_reduce", [r_tq], [r_route], ek_all[:, i * 4:(i + 1) * 4], tq[:], AX.X, ALU.add)

        def all_p1_regs():
            rr_ = [r_junk, r_rA, r_rB, r_kc, r_vc, r_w, r_brow, r_lngb, r_AB, r_route, r_run, r_xmd, r_h2d]
            for ring in (xt_r, rp_r, xs_r, hT_r, qk_r, qT_r, u_r, gv_r, z_r, vln_r, sgo_r, p_r, ao_r, on_r, oT_r, tm_r,
                         xm_r, h2f_r, h2b_r, h2T_r, st_r, bn_r, lg_r, t8_r, mk_r, mkb_r, ex_r, oh_r, Dm_r, tq_r, ptr):
                rr_ += [r for (_, r) in ring.items]
            rr_ += [r for (_, r) in kT2r] + [r for (_, r) in vaugr] + [r for (_, _, r) in pslots]
            return rr_

        def finish_dbg():
            o = S.op("sp", None, reads=all_p1_regs())
            o.deps.update(z.idx for z in zero_ops)
            S.run()
            st1.close()
            st_mix.close()
            return nc

        stageA(0)
        if stop_after == 10:
            return finish_dbg()
        for i in range(1, NT):
            stageA(i)
            if stop_after == 11:
                return finish_dbg()
            stageB(i - 1)
            if stop_after == 12:
                return finish_dbg()
        stageB(NT - 1)

        allr1 = [r_junk, r_rA, r_rB, r_kc, r_vc, r_w, r_brow, r_lngb, r_AB]
        for ring in (xt_r, rp_r, xs_r, hT_r, qk_r, qT_r, u_r, gv_r, z_r, vln_r, sgo_r, p_r, ao_r, on_r, oT_r, tm_r,
                     xm_r, h2f_r, h2b_r, h2T_r, st_r, bn_r, lg_r, t8_r, mk_r, mkb_r, ex_r, oh_r, Dm_r, tq_r, ptr):
            allr1 += [r for (_, r) in ring.items]
        allr1 += [r for (_, r) in kT2r] + [r for (_, r) in vaugr] + [r for (_, _, r) in pslots]
        for e in Sched.ENGS:
            S.op(e, None, reads=allr1)
        if stop_after == 1:
            d_dbg = S.dsem("dbg")
            rr = [Reg("dbgo") for _ in range(4)]
            n4 = NT * 4
            dma("sp", dbg_route[:, 0:n4], rank_all[:], [r_route], [rr[0]], d_dbg)
            dma("sp", dbg_route[:, n4:2 * n4], ek_all[:], [r_route], [rr[1]], d_dbg)
            dma("sp", dbg_route[:, 2 * n4:3 * n4], gk_all[:], [r_route], [rr[2]], d_dbg)
            dma("sp", dbg_route[:, 3 * n4:3 * n4 + 32], runc[:], [r_run], [rr[3]], d_dbg)
            d_dbg.seal()
            o = S.op("sp", None, reads=rr + [r_xmd, r_h2d])
            o.deps.update(z.idx for z in zero_ops)
            S.run()
            st1.close()
            st_mix.close()
            return nc
        st1.close()
        st_mix.close()

        st2 = ExitStack()
        ci_ = sbuf(st2, "ci_", [128, 32], I32)
        pf = sbuf(st2, "pf", [128, 32], F32)
        cs0 = sbuf(st2, "cs0", [128, 32], F32)
        cs1 = sbuf(st2, "cs1", [128, 32], F32)
        pst = sbuf(st2, "pst", [128, 32], F32)
        cmp8 = sbuf(st2, "cmp8", [128, 32, 8], F32)
        cmpb = sbuf(st2, "cmpb", [128, NB, 32], F32)
        ebf = sbuf(st2, "ebf", [128, NB], F32)
        samef = sbuf(st2, "samef", [128, NB], F32)
        ohb = sbuf(st2, "ohb", [128, NT * 4, 32], F32)
        psel = sbuf(st2, "psel", [128, NT * 4], F32)
        r_b = Reg("book")
        LOG2R = R.bit_length() - 1
        op1("dve", "tensor_tensor", [r_run, r_const], [r_b], cmp8[:], runc[:].unsqueeze(2).to_broadcast([128, 32, 8]),
            thr8.unsqueeze(1).to_broadcast([128, 32, 8]), ALU.is_gt)
        op1("dve", "tensor_reduce", [r_b], [r_b], pf[:], cmp8[:], AX.X, ALU.add)
        op1("dve", "tensor_scalar", [r_b], [r_b], pf[:], pf[:], float(R), None, ALU.mult)
        op1("dve", "tensor_copy", [r_b], [r_b], cs0[:], pf[:])
        cur, nxt = cs0, cs1
        for s in (1, 2, 4, 8, 16):
            op1("dve", "tensor_copy", [r_b], [r_b], nxt[:, 0:s], cur[:, 0:s])
            op1("dve", "tensor_tensor", [r_b], [r_b], nxt[:, s:32], cur[:, s:32], cur[:, 0:32 - s], ALU.add)
            cur, nxt = nxt, cur
        pend = cur
        op1("dve", "tensor_tensor", [r_b], [r_b], pst[:], pend[:], pf[:], ALU.subtract)
        op1("dve", "tensor_tensor", [r_b, r_const], [r_b], cmpb[:], pend[:].unsqueeze(1).to_broadcast([128, NB, 32]),
            bstart.unsqueeze(2).to_broadcast([128, NB, 32]), ALU.is_le)
        op1("dve", "tensor_reduce", [r_b], [r_b], ebf[:], cmpb[:], AX.X, ALU.add)
        op1("dve", "tensor_scalar", [r_b], [r_b], ebf[:], ebf[:], 31.0, None, ALU.min)
        op1("dve", "tensor_tensor", [r_b], [r_b], samef[:, 2:NB], ebf[:, 2:NB], ebf[:, 0:NB - 2], ALU.is_equal)
        op1("dve", "tensor_scalar", [r_b, r_const], [r_b], ebf[:], ebf[:], 128.0, pidx, ALU.mult, ALU.add)
        op1("dve", "scalar_tensor_tensor", [r_b], [r_b], ebf[:, 2:NB], samef[:, 2:NB], 1.0e6, ebf[:, 2:NB],
            ALU.mult, ALU.add)
        op1("dve", "tensor_copy", [r_b], [r_eblk], widx[:], ebf[:])
        op1("dve", "tensor_tensor", [r_route, r_const], [r_b], ohb[:],
            ek_all[:].unsqueeze(2).to_broadcast([128, NT * 4, 32]),
            iota_e.unsqueeze(1).to_broadcast([128, NT * 4, 32]), ALU.is_equal)
        op1("dve", "tensor_tensor", [r_b], [r_b], ohb[:], ohb[:], pst[:].unsqueeze(1).to_broadcast([128, NT * 4, 32]),
            ALU.mult)
        op1("dve", "tensor_reduce", [r_b], [r_b], psel[:], ohb[:], AX.X, ALU.add)
        op1("dve", "tensor_tensor", [r_b, r_route], [r_b], psel[:], psel[:], rank_all[:], ALU.add)
        op1("dve", "tensor_copy", [r_b], [r_dest], dest_i[:], psel[:])

        h2l_r = mkring(st2, "h2l", [128, 1024], BF16, 3)
        d_h2l = [S.dsem("h2l%d" % k) for k in range(3)]
        d_sc = [S.dsem("scatter%d" % k) for k in range(3)]
        for i in range(NT):
            hl, r_hl = h2l_r.next()
            dma("sp", hl[:], h2_d[i * 128:(i + 1) * 128, :], [r_h2d], [r_hl], d_h2l[i % 3])
            for k in range(4):
                c = i * 4 + k
                o = S.op("pool", (lambda e, hl=hl, c=c: e.indirect_dma_start(
                    out=xs_d, out_offset=bass.IndirectOffsetOnAxis(ap=dest_i[:, c:c + 1], axis=0),
                    in_=hl[:], in_offset=None)),
                    [r_hl, r_dest], [Reg("xs_sc")], dsem=d_sc[i % 3])
                o.deps.update(z.idx for z in zero_ops)
        scatter_ops = []
        for d in d_sc:
            scatter_ops += d.ops
        if stop_after == 2:
            d_dbg = S.dsem("dbg")
            rr = [Reg("dbgo") for _ in range(2)]
            dma("sp", dbg_book[:, 0:NT * 4], dest_i[:], [r_dest], [rr[0]], d_dbg)
            dma("sp", dbg_book[:, NT * 4:NT * 4 + NB], widx[:], [r_eblk], [rr[1]], d_dbg)
            d_dbg.seal()
            o = S.op("sp", None, reads=rr)
            o.deps.update(z.idx for z in scatter_ops)
            S.run()
            st2.close()
            return nc
        for e in Sched.ENGS:
            S.op(e, None, reads=[r_b] + [r for (_, r) in h2l_r.items])
        st2.close()

        st3 = ExitStack()
        wgu_r = mkring(st3, "wgu", [128, 8, 2048], BF16, 2)
        wd_r = mkring(st3, "wd", [128, 8, 1024], BF16, 2)
        bg_r = mkring(st3, "bg", [128, 16], F32, 2)
        d_wgu = [S.dsem("wgu0"), S.dsem("wgu1")]
        d_wd = [S.dsem("wd0"), S.dsem("wd1")]
        d_bg = [S.dsem("bg0"), S.dsem("bg1")]
        xb_r = mkring(st3, "xb", [128, RG, 1024], BF16, 2)
        d_xb = [S.dsem("xb0"), S.dsem("xb1")]
        xbT_r = mkring(st3, "xbT", [128, 8, R], BF16, 2)
        aT_r = mkring(st3, "aT", [128, 8, R], BF16, 2)
        gc_r = mkring(st3, "gc", [128, R], F32, 2)
        uc_r = mkring(st3, "uc", [128, R], F32, 2)
        sg_r = mkring(st3, "sg", [128, R], F32, 2)
        yo_r = mkring(st3, "yo", [128, 1024], F32, 3)
        d_yo = [S.dsem("yo%d" % k) for k in range(3)]
        r_ys = Reg("ys_d")
        breg = {}

        def wbound(e):
            if "r" not in breg:
                breg["r"] = e.to_reg(4095)
            return breg["r"]
        wg_regs = [[Reg("wgk") for _ in range(8)] for _ in range(2)]
        wd_regs = [[Reg("wdk") for _ in range(8)] for _ in range(2)]

        for b in range(NB):
            kb = b % 2
            wg, r_wg = wgu_r.next()
            wdn, r_wdn = wd_r.next()
            bg, r_bg = bg_r.next()
            r_wgk = wg_regs[kb]
            r_wdk = wd_regs[kb]

            for k in range(8):
                S.op("pool", (lambda e, b=b, wg=wg, k=k: e.indirect_dma_start(
                    out=wg[:, k, :], out_offset=None, in_=wgu[k],
                    in_offset=bass.IndirectOffsetOnAxis(ap=widx[:, b:b + 1], axis=0),
                    bounds_check=wbound(e), oob_is_err=False)), [r_eblk], [r_wgk[k]], dsem=d_wgu[kb])
            for k in range(8):
                S.op("pool", (lambda e, b=b, wdn=wdn, k=k: e.indirect_dma_start(
                    out=wdn[:, k, :], out_offset=None, in_=wd[k],
                    in_offset=bass.IndirectOffsetOnAxis(ap=widx[:, b:b + 1], axis=0),
                    bounds_check=wbound(e), oob_is_err=False)), [r_eblk], [r_wdk[k]], dsem=d_wd[kb])
            S.op("pool", (lambda e, b=b, bg=bg: e.indirect_dma_start(
                out=bg[:], out_offset=None, in_=bgu,
                in_offset=bass.IndirectOffsetOnAxis(ap=widx[:, b:b + 1], axis=0),
                    bounds_check=wbound(e), oob_is_err=False)), [r_eblk], [r_bg], dsem=d_bg[kb])
            xb, r_xb = xb_r.next()
            o = S.op("sp", (lambda e, xb=xb, b=b: e.dma_start(
                out=xb[:], in_=xs_d[b * R:(b + 1) * R, :].rearrange("(g p) n -> p g n", p=128))),
                [], [r_xb], dsem=d_xb[kb])
            o.deps.update(z.idx for z in scatter_ops)
            xbT, r_xbT = xbT_r.next()
            for rg in range(RG):
                pt, r_pt = ptr.next()
                pe_tr([(pt[:, j, :], xb[:, rg, j * 128:(j + 1) * 128]) for j in range(8)], [r_xb, r_const], [r_pt])
                op1("act", "activation", [r_pt], [r_xbT], xbT[:, :, rg * 128:(rg + 1) * 128], pt[:], AF.Copy)
            aT, r_aT = aT_r.next()
            for j in range(8):
                pg, r_pg = ps_next()
                pu, r_pu = ps_next()
                pe_mm([(pg[:, 0:R], wg[:, k, j * 128:(j + 1) * 128], xbT[:, k, :], k == 0, k == 7) for k in range(8)],
                      r_wgk + [r_xbT], [r_pg])
                pe_mm([(pu[:, 0:R], wg[:, k, 1024 + j * 128:1024 + (j + 1) * 128], xbT[:, k, :], k == 0, k == 7)
                       for k in range(8)], r_wgk + [r_xbT], [r_pu])
                gc, r_gc = gc_r.next()
                uc, r_uc = uc_r.next()
                sg, r_sg = sg_r.next()
                op1("dve", "tensor_scalar", [r_pg, r_bg], [r_gc], gc[:], pg[:, 0:R], bg[:, j:j + 1], 7.0, ALU.add, ALU.min)
                op1("act", "activation", [r_pu, r_bg], [r_uc], uc[:], pu[:, 0:R], AF.Identity, bias=bg[:, 8 + j:9 + j])
                op1("act", "activation", [r_gc], [r_sg], sg[:], gc[:], AF.Sigmoid, scale=1.702)
                op1("dve", "tensor_scalar", [r_uc], [r_uc], uc[:], uc[:], 7.0, -7.0, ALU.min, ALU.max)
                op1("dve", "tensor_tensor", [r_gc, r_sg], [r_sg], sg[:], gc[:], sg[:], ALU.mult)
                op1("dve", "scalar_tensor_tensor", [r_uc, r_sg], [r_aT], aT[:, j, :], uc[:], 1.0, sg[:], ALU.add, ALU.mult)
            for rg in range(RG):
                yo, r_yo = yo_r.next()
                ky = (yo_r.i - 1) % 3
                for hh in range(2):
                    py, r_py = ps_next()
                    pe_mm([(py, aT[:, k, rg * 128:(rg + 1) * 128], wdn[:, k, hh * 512:(hh + 1) * 512], k == 0, k == 7)
                           for k in range(8)], [r_aT] + r_wdk, [r_py])
                    op1("act", "activation", [r_py], [r_yo], yo[:, hh * 512:(hh + 1) * 512], py, AF.Copy)
                dma("sp", ys_d[b * R + rg * 128:b * R + (rg + 1) * 128, :], yo[:], [r_yo], [Reg("ys_w")], d_yo[ky])
        ys_ops = []
        for d in d_yo:
            ys_ops += d.ops
        allr3 = []
        for ring in (wgu_r, wd_r, bg_r, xb_r, xbT_r, aT_r, gc_r, uc_r, sg_r, yo_r, ptr):
            allr3 += [r for (_, r) in ring.items]
        allr3 += [r for (_, _, r) in pslots]
        for kk in range(2):
            allr3 += wg_regs[kk] + wd_regs[kk]
        for e in Sched.ENGS:
            o = S.op(e, None, reads=allr3)
            o.deps.update(z.idx for z in ys_ops)
        st3.close()

        st4 = ExitStack()
        yg_r = mkring(st4, "yg", [128, 4, 1024], F32, 2)
        d_yg = [S.dsem("yg0"), S.dsem("yg1")]
        xl_r = mkring(st4, "xl", [128, 1024], F32, 2)
        d_xl = [S.dsem("xl0"), S.dsem("xl1")]
        acc_r = mkring(st4, "acc", [128, 1024], F32, 2)
        ob_r = mkring(st4, "ob", [128, 1024], F32, 2)
        G_r = mkring(st4, "G", [128, 32], F32, 2)
        Gb_r = mkring(st4, "Gb", [128, 32], BF16, 2)
        GT_r = mkring(st4, "GT", [32, 128], BF16, 2)
        toh_r = mkring(st4, "toh", [128, 32], F32, 2)
        st4_r = mkring(st4, "st4", [128, 4], F32, 2)
        junk4 = sbuf(st4, "junk4", [128, 1024], BF16)
        r_junk4 = Reg("junk4")
        out_regs = []
        yg_regs = [[Reg("ygk") for _ in range(4)] for _ in range(2)]
        for i in range(NT):
            yg, r_yg0 = yg_r.next()
            ky = i % 2
            r_ygk = yg_regs[ky]
            r_yg = r_ygk[0]
            for k in range(4):
                c = i * 4 + k
                S.op("pool", (lambda e, yg=yg, k=k, c=c: e.indirect_dma_start(
                    out=yg[:, k, :], out_offset=None, in_=ys_d,
                    in_offset=bass.IndirectOffsetOnAxis(ap=dest_i[:, c:c + 1], axis=0))), [r_dest, r_ys], [r_ygk[k]],
                    dsem=d_yg[ky])
            xl, r_xl = xl_r.next()
            dma("sp", xl[:], xmid_d[i * 128:(i + 1) * 128, :], [r_xmd], [r_xl], d_xl[ky])
            G, r_G = G_r.next()
            toh, r_toh = toh_r.next()
            for k in range(4):
                c = i * 4 + k
                op1("dve", "tensor_scalar", [r_route, r_const], [r_toh], toh[:], iota_e, ek_all[:, c:c + 1],
                    None, ALU.is_equal)
                op1("dve", "tensor_scalar", [r_route, r_toh], [r_toh], toh[:], toh[:], gk_all[:, c:c + 1],
                    None, ALU.mult)
                if k == 0:
                    op1("dve", "tensor_copy", [r_toh], [r_G], G[:], toh[:])
                else:
                    op1("dve", "tensor_tensor", [r_toh, r_G], [r_G], G[:], G[:], toh[:], ALU.add)
            Gb, r_Gb = Gb_r.next()
            op1("dve", "tensor_copy", [r_G], [r_Gb], Gb[:], G[:])
            pt, r_pt = ptr.next()
            pe_tr([(pt[0:32, 0, :], Gb[:])], [r_Gb, r_const], [r_pt])
            GT, r_GT = GT_r.next()
            op1("act", "activation", [r_pt], [r_GT], GT[:], pt[0:32, 0, :], AF.Copy)
            pbd, r_pbd = ps_pair()
            pe_mm([(pbd[:, hh * 512:(hh + 1) * 512], GT[:], bdown_bf[:, hh * 512:(hh + 1) * 512], True, True)
                   for hh in range(2)], [r_GT, r_bdown], r_pbd)
            acc, r_acc = acc_r.next()
            c0 = i * 4
            op1("dve", "scalar_tensor_tensor", r_ygk + [r_route] + r_pbd, [r_acc], acc[:], yg[:, 0, :],
                gk_all[:, c0:c0 + 1], pbd[:], ALU.mult, ALU.add)
            for k in (1, 2, 3):
                eng = "dve"
                op1(eng, "scalar_tensor_tensor", r_ygk + [r_route, r_acc], [r_acc], acc[:], yg[:, k, :],
                    gk_all[:, c0 + k:c0 + k + 1], acc[:], ALU.mult, ALU.add)
            s4, r_s4 = st4_r.next()
            op1("act", "activation", [r_acc], [r_junk4, r_s4], junk4[:], acc[:], AF.Square, accum_out=s4[:, 0:1])
            rstd_from(s4[:, 0:1], s4[:, 1:2], 1.0 / 1024, r_s4, r_s4)
            ob, r_ob = ob_r.next()
            op1("dve", "scalar_tensor_tensor", [r_acc, r_s4, r_MR], [r_ob], ob[:], acc[:], s4[:, 1:2], MR[:, 3, :],
                ALU.mult, ALU.mult)
            op1("pool", "tensor_tensor", [r_ob, r_xl], [r_ob], ob[:], ob[:], xl[:], ALU.add)
            ro = Reg("out")
            dma("sp", out[i * 128:(i + 1) * 128, :], ob[:], [r_ob], [ro], d_out[i % 2])
            out_regs.append(ro)
        S.op("sp", None, reads=out_regs)
        S.run()
        st4.close()
    return nc


def _host_consts():
    ident = np.eye(128, dtype=np.float32)
    U = np.triu(np.ones((128, 128), np.float32), k=1)
    ones = np.ones((128, 128), np.float32)
    jj = np.arange(128)[:, None]
    ii = np.arange(128)[None, :]
    mprev = (jj >= ii).astype(np.float32)
    mnext = (jj <= ii).astype(np.float32)
    cbf = np.concatenate([ident, U, ones, mprev, mnext], axis=1)
    cf32 = np.concatenate([np.tile(np.arange(32, dtype=np.float32), (128, 1)),
                           np.tile(np.arange(NB, dtype=np.float32) * R, (128, 1)),
                           np.arange(128, dtype=np.float32)[:, None],
                           np.tile(np.arange(8, dtype=np.float32) * R, (128, 1))], axis=1)
    t = np.arange(4096)
    pos_row = (t // 64).astype(np.float32)
    pos_col = (t % 64).astype(np.float32)
    inv_freq = (np.float32(10000.0) ** (-np.arange(16, dtype=np.float32) / np.float32(16))).astype(np.float32)
    ar = pos_row[:, None] * inv_freq
    ac = pos_col[:, None] * inv_freq
    cr, sr, cc_, sc_ = np.cos(ar), np.sin(ar), np.cos(ac), np.sin(ac)
    rope = np.concatenate([cr, cr, cc_, cc_, -sr, sr, -sc_, sc_], axis=1).astype(np.float32)
    return np.ascontiguousarray(cbf), np.ascontiguousarray(cf32), np.ascontiguousarray(rope)


_CACHE = {}


def _prep(x, c, ctx, c_ctx, w_ada, b_ada, g_pre_mix, g_post_mix, g_pre_ffn, g_post_ffn,
          w_in, b_in, attn_sink, sgu_ln_g, sgu_ln_b, sgu_w, sgu_b, g_attn_out, g_sgu_out,
          w_out, b_out, w_router, b_router, w_gate_up, b_gate_up, w_down, b_down):
    f = lambda a: np.ascontiguousarray(np.asarray(a, dtype=np.float32))
    x, c, ctx, c_ctx = f(x), f(c), f(ctx), f(c_ctx)
    cbf, cf32, rope = _host_consts()
    perm = np.concatenate([np.arange(0, 512), np.arange(768, 1280), np.arange(1280, 1792), np.arange(512, 768)])
    w_in_p = f(f(w_in)[0][:, perm])
    b_in_p = f(b_in)[0][perm]
    col = lambda v: f(v).reshape(8, 128).T
    colpack = f(np.concatenate([col(f(g_pre_mix)[0]),
                                col(np.concatenate([f(g_attn_out)[0], f(g_sgu_out)[0]]))], axis=1))
    rowpack = f(np.concatenate([f(g_post_mix)[0], f(g_pre_ffn)[0], f(g_post_ffn)[0], f(sgu_ln_g)[0],
                                f(sgu_ln_b)[0], f(attn_sink)[0]])[None, :])
    browf = f(np.concatenate([b_in_p, f(b_out)[0], f(sgu_b)[0].reshape(-1), f(b_router)[0]])[None, :])
    sguwT = f(np.transpose(f(sgu_w)[0], (2, 0, 1)).reshape(128, 1024))
    wgu3 = f(w_gate_up)[0]
    wd3 = f(w_down)[0]
    bgu = f(f(b_gate_up)[0].reshape(32, 2, 8, 128).transpose(0, 3, 1, 2).reshape(32 * 128, 16))
    shared = {
        "w_ada": f(w_ada)[0], "b_ada": f(b_ada), "colpack": colpack, "rowpack": rowpack, "browf": browf,
        "w_in": w_in_p, "w_out": f(w_out)[0], "w_r": f(w_router)[0], "sguwT": sguwT, "b_down": f(b_down)[0],
        "bgu": bgu, "cbf": cbf, "cf32": cf32, "rope": rope,
    }
    for k in range(8):
        shared["wgu%d" % k] = f(wgu3[:, k * 128:(k + 1) * 128, :].reshape(32 * 128, 2048))
        shared["wd%d" % k] = f(wd3[:, k * 128:(k + 1) * 128, :].reshape(32 * 128, 1024))
    in_maps = []
    for b in range(8):
        m = dict(shared)
        m["x"] = x[b]
        m["ctx"] = ctx[b]
        m["cc"] = f(np.stack([c[b].reshape(8, 128).T, c_ctx.reshape(8, 128).T], axis=2).reshape(128, 16))
        in_maps.append(m)
    return in_maps


def kernel(**inputs):
    if "nc" not in _CACHE:
        _CACHE["nc"] = build_program()
    nc = _CACHE["nc"]
    in_maps = _prep(**inputs)
    res = run_bass_kernel_spmd(nc, in_maps, core_ids=list(range(8)))
    return np.stack([np.asarray(r["out"], dtype=np.float32) for r in res.results], axis=0)
```
